# Optimizing a Trainium2 kernel written in Bass

```python
import jax
import jax.numpy as jnp
from jax import lax
import numpy as np

D_MODEL = 1024
BATCH = 4
SEQ = 8192
DEPTH = 1

N_HEADS = 8
HEAD_DIM = D_MODEL // N_HEADS
ATTN_WIDTH = N_HEADS * HEAD_DIM
ROT_DIM = HEAD_DIM // 4
ROPE_THETA = 500000.0
MOBA_BLOCK = 256
MOBA_TOPK = 3
Q_CHUNK = 32
LRU_WIDTH = D_MODEL
LRU_BLOCKS = 4
LRU_BLOCK_WIDTH = LRU_WIDTH // LRU_BLOCKS
CONV_WIDTH = 4
LRU_C = 8.0
D_FF = 4 * D_MODEL
NORM_EPS = 1e-6
MASK_VALUE = -1e30
SPLITS = (ATTN_WIDTH, 2 * ATTN_WIDTH, 3 * ATTN_WIDTH,
          3 * ATTN_WIDTH + LRU_WIDTH, 3 * ATTN_WIDTH + 2 * LRU_WIDTH,
          3 * ATTN_WIDTH + 2 * LRU_WIDTH + D_MODEL)
IN_COLS = 3 * ATTN_WIDTH + 2 * LRU_WIDTH + 2 * D_MODEL

kernel_name = "hybrid_moba_rglru_sqrelu_layer"


def rms_norm(x, gain):
    xf = x.astype(jnp.float32)
    y = xf * lax.rsqrt(jnp.mean(xf * xf, axis=-1, keepdims=True) + NORM_EPS)
    return (y * gain.astype(jnp.float32)).astype(x.dtype)


def rope_tables(seq_len):
    inv_freq = ROPE_THETA ** (-jnp.arange(0, ROT_DIM, 2, dtype=jnp.float32) / ROT_DIM)
    ang = jnp.arange(seq_len, dtype=jnp.float32)[:, None] * inv_freq[None, :]
    return jnp.cos(ang), jnp.sin(ang)


def partial_rope(x, cos, sin):
    half = ROT_DIM // 2
    x1 = x[..., :half].astype(jnp.float32)
    x2 = x[..., half:ROT_DIM].astype(jnp.float32)
    rot = jnp.concatenate([x1 * cos - x2 * sin, x2 * cos + x1 * sin], axis=-1).astype(x.dtype)
    return jnp.concatenate([rot, x[..., ROT_DIM:]], axis=-1)


def moba_attention(q, k, v):
    b, h, s, dh = q.shape
    n_blocks = -(-s // MOBA_BLOCK)
    pad = n_blocks * MOBA_BLOCK - s
    kp = jnp.pad(k, ((0, 0), (0, 0), (0, pad), (0, 0)))
    vp = jnp.pad(v, ((0, 0), (0, 0), (0, pad), (0, 0)))
    kb = kp.reshape(b, h, n_blocks, MOBA_BLOCK, dh)
    vb = vp.reshape(b, h, n_blocks, MOBA_BLOCK, dh)
    k_mean = jnp.mean(kb.astype(jnp.float32), axis=3)
    top = min(MOBA_TOPK, n_blocks)
    scale = dh ** -0.5
    n_chunks = s // Q_CHUNK
    q_chunks = jnp.moveaxis(q.reshape(b, h, n_chunks, Q_CHUNK, dh), 2, 0)
    block_ids = jnp.arange(n_blocks)
    gather_blocks = jax.vmap(jax.vmap(lambda blocks, idx: blocks[idx]))

    def one_chunk(args):
        c, qc = args
        q_start = c * Q_CHUNK
        q_pos = q_start + jnp.arange(Q_CHUNK)
        q_block = q_start // MOBA_BLOCK
        gate = jnp.einsum("bhqd,bhnd->bhqn", qc.astype(jnp.float32), k_mean)
        gate = jnp.where(block_ids < q_block, gate, -jnp.inf)
        _, sel = lax.top_k(gate, top)
        sel_ok = jnp.arange(top) < q_block
        k_sel = gather_blocks(kb, sel)
        v_sel = gather_blocks(vb, sel)
        s_sel = jnp.einsum("bhqd,bhqjkd->bhqjk", qc, k_sel).astype(jnp.float32) * scale
        s_sel = jnp.where(sel_ok[:, None], s_sel, MASK_VALUE)
        s_sel = s_sel.reshape(b, h, Q_CHUNK, top * MOBA_BLOCK)
        k_own = lax.dynamic_slice_in_dim(kp, q_block * MOBA_BLOCK, MOBA_BLOCK, axis=2)
        v_own = lax.dynamic_slice_in_dim(vp, q_block * MOBA_BLOCK, MOBA_BLOCK, axis=2)
        key_pos = q_block * MOBA_BLOCK + jnp.arange(MOBA_BLOCK)
        s_own = jnp.einsum("bhqd,bhkd->bhqk", qc, k_own).astype(jnp.float32) * scale
        s_own = jnp.where(key_pos[None, :] <= q_pos[:, None], s_own, MASK_VALUE)
        p = jax.nn.softmax(jnp.concatenate([s_sel, s_own], axis=-1), axis=-1).astype(v.dtype)
        p_sel = p[..., :top * MOBA_BLOCK].reshape(b, h, Q_CHUNK, top, MOBA_BLOCK)
        p_own = p[..., top * MOBA_BLOCK:]
        return (jnp.einsum("bhqjk,bhqjkd->bhqd", p_sel, v_sel)
                + jnp.einsum("bhqk,bhkd->bhqd", p_own, v_own))

    out = lax.map(one_chunk, (jnp.arange(n_chunks, dtype=jnp.int32), q_chunks))
    return jnp.moveaxis(out, 0, 2).reshape(b, h, s, dh)


def causal_depthwise_conv(x, w, bias):
    s = x.shape[1]
    xp = jnp.pad(x, ((0, 0), (CONV_WIDTH - 1, 0), (0, 0)))
    y = bias
    for j in range(CONV_WIDTH):
        y = y + w[j] * xp[:, j:j + s]
    return y


def block_diag_linear(x, w, bias):
    b, s, _ = x.shape
    xg = x.reshape(b, s, LRU_BLOCKS, LRU_BLOCK_WIDTH)
    return jnp.einsum("bsgi,gij->bsgj", xg, w).reshape(b, s, LRU_WIDTH) + bias


def _linear_combine(left, right):
    a_l, b_l = left
    a_r, b_r = right
    return a_l * a_r, a_r * b_l + b_r


def rg_lru(x, w_a, b_a, w_i, b_i, lam):
    r = jax.nn.sigmoid(block_diag_linear(x, w_a, b_a).astype(jnp.float32))
    i = jax.nn.sigmoid(block_diag_linear(x, w_i, b_i).astype(jnp.float32))
    log_a = -LRU_C * r * jax.nn.softplus(-lam.astype(jnp.float32))
    a = jnp.exp(log_a)
    mult = jnp.sqrt(-jnp.expm1(2.0 * log_a))
    bx = mult * (i * x.astype(jnp.float32))
    _, h = lax.associative_scan(_linear_combine, (a, bx), axis=1)
    return h.astype(x.dtype)


def hybrid_mixer(u, w_in, conv_w, conv_b, w_rg_a, b_rg_a, w_rg_i, b_rg_i, lru_lambda,
                 w_branch_attn, w_branch_lru, w_out):
    b, s, _ = u.shape
    proj = jnp.einsum("bsd,dc->bsc", u, w_in)
    q, k, v, x_lru, g_lru, gate_attn, gate_lru = jnp.split(proj, list(SPLITS), axis=-1)

    def to_heads(t):
        return t.reshape(b, s, N_HEADS, HEAD_DIM).transpose(0, 2, 1, 3)

    cos, sin = rope_tables(s)
    qh = partial_rope(to_heads(q), cos, sin)
    kh = partial_rope(to_heads(k), cos, sin)
    attn = moba_attention(qh, kh, to_heads(v))
    attn = attn.transpose(0, 2, 1, 3).reshape(b, s, ATTN_WIDTH)
    y_attn = attn @ w_branch_attn

    xc = causal_depthwise_conv(x_lru, conv_w, conv_b)
    hl = rg_lru(xc, w_rg_a, b_rg_a, w_rg_i, b_rg_i, lru_lambda)
    y_lru = (hl * jax.nn.gelu(g_lru)) @ w_branch_lru

    merged = jax.nn.sigmoid(gate_attn) * y_attn + jax.nn.sigmoid(gate_lru) * y_lru
    return merged @ w_out


def setup_inputs(seed: int = 0) -> dict:
    key = jax.random.key(seed)
    ks = jax.random.split(key, 18)
    f32 = jnp.float32

    def nrm(k, shape, scale):
        return jax.random.normal(k, shape, f32) * scale

    def gain(k):
        return 1.0 + 0.05 * jax.random.normal(k, (DEPTH, D_MODEL), f32)

    a_init = jax.random.uniform(ks[10], (DEPTH, LRU_WIDTH), f32, 0.9, 0.999)
    return {
        "x": nrm(ks[0], (BATCH, SEQ, D_MODEL), 1.0),
        "attn_pre_norm": gain(ks[1]),
        "attn_post_norm": gain(ks[2]),
        "w_in": nrm(ks[3], (DEPTH, D_MODEL, IN_COLS), D_MODEL ** -0.5),
        "conv_w": nrm(ks[4], (DEPTH, CONV_WIDTH, LRU_WIDTH), CONV_WIDTH ** -0.5),
        "conv_b": nrm(ks[5], (DEPTH, LRU_WIDTH), 0.01),
        "w_rg_a": nrm(ks[6], (DEPTH, LRU_BLOCKS, LRU_BLOCK_WIDTH, LRU_BLOCK_WIDTH), LRU_BLOCK_WIDTH ** -0.5),
        "b_rg_a": nrm(ks[7], (DEPTH, LRU_WIDTH), 0.01),
        "w_rg_i": nrm(ks[8], (DEPTH, LRU_BLOCKS, LRU_BLOCK_WIDTH, LRU_BLOCK_WIDTH), LRU_BLOCK_WIDTH ** -0.5),
        "b_rg_i": nrm(ks[9], (DEPTH, LRU_WIDTH), 0.01),
        "lru_lambda": jnp.log(a_init) - jnp.log1p(-a_init),
        "w_branch_attn": nrm(ks[11], (DEPTH, ATTN_WIDTH, D_MODEL), ATTN_WIDTH ** -0.5),
        "w_branch_lru": nrm(ks[12], (DEPTH, LRU_WIDTH, D_MODEL), LRU_WIDTH ** -0.5),
        "w_out": nrm(ks[13], (DEPTH, D_MODEL, D_MODEL), D_MODEL ** -0.5),
        "mlp_pre_norm": gain(ks[14]),
        "mlp_post_norm": gain(ks[15]),
        "w_mlp_up": nrm(ks[16], (DEPTH, D_MODEL, D_FF), D_MODEL ** -0.5),
        "w_mlp_down": nrm(ks[17], (DEPTH, D_FF, D_MODEL), D_FF ** -0.5),
    }


def reference(x, attn_pre_norm, attn_post_norm, w_in, conv_w, conv_b, w_rg_a, b_rg_a,
              w_rg_i, b_rg_i, lru_lambda, w_branch_attn, w_branch_lru, w_out,
              mlp_pre_norm, mlp_post_norm, w_mlp_up, w_mlp_down):
    h = x
    for l in range(DEPTH):
        u = rms_norm(h, attn_pre_norm[l])
        mix = hybrid_mixer(u, w_in[l], conv_w[l], conv_b[l], w_rg_a[l], b_rg_a[l],
                           w_rg_i[l], b_rg_i[l], lru_lambda[l],
                           w_branch_attn[l], w_branch_lru[l], w_out[l])
        h = h + rms_norm(mix, attn_post_norm[l])
        u = rms_norm(h, mlp_pre_norm[l])
        m = jnp.square(jax.nn.relu(u @ w_mlp_up[l])) @ w_mlp_down[l]
        h = h + rms_norm(m, mlp_post_norm[l])
    return h
```

```python
import numpy as np
import concourse.bass as bass
import concourse.mybir as mybir
from concourse.bass_utils import run_bass_kernel_spmd

F32 = mybir.dt.float32
BF16 = mybir.dt.bfloat16
AF = mybir.ActivationFunctionType
ALU = mybir.AluOpType
AX = mybir.AxisListType

SAME_ENGINE_SYNC = True
D = 1024
S = 8192
SO = 4096
NH = 8
NBLK = 32
ARENA_WORDS = 52000
SCALE = 128 ** -0.5
EPS = 1e-6


class Prog:
    ENG = ("pe", "act", "dve", "pool", "sp")

    def __init__(self, nc):
        self.nc = nc
        self.streams = {k: [] for k in self.ENG}
        self.esem = {k: nc.alloc_semaphore(name="es_" + k) for k in self.ENG}
        self.ecount = {k: 0 for k in self.ENG}
        self.obs = {k: {} for k in self.ENG}
        self.res = {}
        self.dsem = {}
        self.last_dma = {}

    def _deps(self, reads, writes):
        toks = []
        for r in reads:
            st = self.res.get(r)
            if st is not None and st[0] is not None:
                toks.append(st[0])
        for w in writes:
            st = self.res.get(w)
            if st is not None:
                if st[0] is not None:
                    toks.append(st[0])
                toks.extend(st[1])
        return toks

    def _record(self, tok, reads, writes):
        for r in reads:
            st = self.res.setdefault(r, [None, []])
            st[1].append(tok)
        for w in writes:
            self.res[w] = [tok, []]

    def _waits(self, eng, toks):
        obs = self.obs[eng]
        need = {}
        for (sem, val, src, snap) in toks:
            if src == eng and (eng == "pe" or not SAME_ENGINE_SYNC):
                continue
            if obs.get(id(sem), 0) >= val:
                continue
            cur = need.get(id(sem))
            if cur is None or cur[1] < val:
                need[id(sem)] = (sem, val, snap)
        out = []
        for k, (sem, val, snap) in need.items():
            if obs.get(k, 0) >= val:
                continue
            out.append((sem, val))
            obs[k] = val
            for kk, vv in snap.items():
                if obs.get(kk, 0) < vv:
                    obs[kk] = vv
        return out

    def op(self, eng, name, *args, reads=(), writes=(), **kw):
        fn = (name, args, kw)
        psr = [r for r in reads if isinstance(r, tuple) and r[0] == "ps"]
        if psr:
            reads = [r for r in reads if not (isinstance(r, tuple) and r[0] == "ps")]
            writes = list(writes) + [r for r in psr if r not in writes]
        toks = self._deps(reads, writes)
        waits = self._waits(eng, toks)
        self.ecount[eng] += 1
        sem = self.esem[eng]
        tok = (sem, self.ecount[eng], eng, dict(self.obs[eng]))
        self.streams[eng].append((waits, fn, sem, 1))
        self._record(tok, reads, writes)

    def dma(self, queue, semkey, reads=(), writes=(), **kw):
        fn = ("dma_start", (), kw)
        if semkey not in self.dsem:
            self.dsem[semkey] = [self.nc.alloc_semaphore(name="ds_%d" % len(self.dsem)), 0, None]
        ent = self.dsem[semkey]
        toks = self._deps(reads, writes)
        if ent[2] is not None:
            toks.append(ent[2])
        waits = self._waits(queue, toks)
        ent[1] += 16
        tok = (ent[0], ent[1], "dma", dict(self.obs[queue]))
        ent[2] = tok
        self.last_dma[semkey] = tok
        self.streams[queue].append((waits, fn, ent[0], 16))
        self._record(tok, reads, writes)

    def barrier(self):
        toks = [(self.esem[k], self.ecount[k]) for k in self.ENG if self.ecount[k] > 0]
        toks += [(t[0], t[1]) for t in self.last_dma.values()]
        for e in self.ENG:
            obs = self.obs[e]
            waits = []
            for (sem, val) in toks:
                if obs.get(id(sem), 0) < val:
                    waits.append((sem, val))
                    obs[id(sem)] = val
            self.streams[e].append((waits, None, None, 0))
        self.res = {}

    def emit(self):
        nc = self.nc
        streams = self.streams

        def run(e, lst):
            for (waits, fn, sem, inc) in lst:
                for (s, v) in waits:
                    e.wait_ge(s, v)
                if fn is not None:
                    getattr(e, fn[0])(*fn[1], **fn[2]).then_inc(sem, inc)

        with nc.Block() as block:
            @block.tensor
            def _(e):
                run(e, streams["pe"])

            @block.scalar
            def _(e):
                run(e, streams["act"])

            @block.vector
            def _(e):
                run(e, streams["dve"])

            @block.gpsimd
            def _(e):
                run(e, streams["pool"])

            @block.sync
            def _(e):
                run(e, streams["sp"])


class Arena:
    def __init__(self, nc, nwords):
        self.t = nc.alloc_sbuf_tensor("arena", [128, nwords], F32)
        self.n = nwords
        self.top = 0

    def mark(self):
        return self.top

    def release(self, m):
        self.top = m

    def alloc(self, nelem, dtype=F32):
        words = nelem if dtype == F32 else (nelem + 1) // 2
        words = (words + 7) // 8 * 8
        a = self.top
        self.top += words
        assert self.top <= self.n, ("SBUF arena overflow", self.top, self.n)
        ap = self.t[:, a:a + words]
        if dtype != F32:
            ap = ap.bitcast(dtype)
        return ap[:, 0:nelem]


class Rot:
    def __init__(self, items):
        self.items = items
        self.i = 0

    def next(self):
        it = self.items[self.i % len(self.items)]
        self.i += 1
        return it


def v3(ap, a):
    return ap.rearrange("p (a b) -> p a b", a=a)


def build_program(debug=False, stop=None):
    nc = bass.Bass("TRN2", target_bir_lowering=False)

    def din(name, shape, dt=F32):
        return nc.dram_tensor(name, list(shape), dt, kind="ExternalInput").ap()

    xa = din("xa", [S, D])
    xo = din("xo", [SO, D])
    w_in = din("w_in", [D, 7168])
    w_rga = din("w_rga", [4, 256, 256])
    w_rgi = din("w_rgi", [4, 256, 256])
    w_ba = din("w_ba", [D, D])
    w_bl = din("w_bl", [D, D])
    w_out = din("w_out", [D, D])
    w_up = din("w_up", [D, 4096])
    w_down = din("w_down", [4096, D])
    vecs = din("vecs", [128, 64])
    grep = din("grep", [128, 4 * D])
    ropek = din("ropek", [2, 32, S])
    ropeq = din("ropeq", [2, 32, SO])
    gmask_d = din("gmask", [128, 16 * 32])
    cmask_d = din("cmask", [128, 2 * 2 * 256])
    psel_d = din("psel", [128, 2])
    out_d = nc.dram_tensor("out", [SO, D], F32, kind="ExternalOutput").ap()

    def dscr(name, shape, dt):
        return nc.dram_tensor(name, list(shape), dt, kind="ExternalOutput" if debug else "Internal").ap()

    Ks = dscr("Ks", [NH, 128, S], BF16)
    Vs = dscr("Vs", [NH, 128, 64 * 130], BF16)
    Qs = dscr("Qs", [NH, 128, SO], BF16)
    Hs = dscr("Hs", [8, 128, SO], BF16)
    YL = dscr("YL", [8, 128, SO], BF16)
    H1 = dscr("H1", [SO, D], F32)

    P = Prog(nc)
    A = Arena(nc, ARENA_WORDS)
    psb = [nc.alloc_psum_tensor("psb%d" % i, [128, 512], F32) for i in range(8)]

    def ps(i):
        return psb[i][:, :], ("ps", i)

    identf = A.alloc(128)
    ident = A.alloc(128, BF16)
    vec = A.alloc(64)
    c1 = A.alloc(8)
    c2 = A.alloc(8)
    tmp8 = [A.alloc(8) for _ in range(4)]
    psel = A.alloc(2)
    ksum = A.alloc(NH * NBLK)
    ksum_hi = A.alloc(NH * NBLK, BF16)
    ksum_lo = A.alloc(NH * NBLK, BF16)
    carryx = A.alloc(8 * 3)
    carryh = A.alloc(8)
    pmat = A.alloc(128, BF16)
    pmatf = A.alloc(128)

    def vcol(base, c):
        return vec[:, base + c:base + c + 1]

    P.dma("sp", "c_vec", out=vec, in_=vecs, writes=["vec"])
    P.dma("sp", "c_psel", out=psel, in_=psel_d, writes=["psel"])
    P.op("pool", "memset", identf, 0.0, writes=["identf"])
    P.op("pool", "affine_select", out=identf, in_=identf, pattern=[[-1, 128]], compare_op=ALU.not_equal,
                                           fill=1.0, base=0, channel_multiplier=1,
         reads=["identf"], writes=["identf"])
    P.op("dve", "tensor_copy", out=ident, in_=identf, reads=["identf"], writes=["ident"])
    P.op("pool", "memset", pmatf, 0.0, writes=["pmatf"])
    P.op("pool", "affine_select", out=pmatf[:, 0:16], in_=pmatf[:, 0:16], pattern=[[-1, 16]],
                                           compare_op=ALU.not_equal, fill=1.0, base=-16, channel_multiplier=1,
         reads=["pmatf"], writes=["pmatf"])
    P.op("pool", "affine_select", out=pmatf[:, 16:32], in_=pmatf[:, 16:32], pattern=[[-1, 16]],
                                           compare_op=ALU.not_equal, fill=1.0, base=0, channel_multiplier=1,
         reads=["pmatf"], writes=["pmatf"])
    P.op("dve", "tensor_copy", out=pmat, in_=pmatf, reads=["pmatf"], writes=["pmat"])
    P.op("pool", "memset", ksum, 0.0, writes=["ksum"])
    P.op("pool", "memset", carryx, 0.0, writes=["carryx"])
    P.op("pool", "memset", carryh, 0.0, writes=["carryh"])
    lam = vec[:, 56:64]
    t_abs, t_e, t_l, t_r = tmp8
    P.op("dve", "tensor_scalar", out=t_r, in0=lam, scalar1=-1.0, scalar2=None, op0=ALU.mult,
         reads=["vec"], writes=["t_r"])
    P.op("dve", "tensor_tensor", out=t_abs, in0=lam, in1=t_r, op=ALU.max,
         reads=["vec", "t_r"], writes=["t_abs"])
    P.op("act", "activation", out=t_e, in_=t_abs, func=AF.Exp, scale=-1.0, reads=["t_abs"], writes=["t_e"])
    P.op("act", "activation", out=t_l, in_=t_e, func=AF.Ln, bias=1.0, reads=["t_e"], writes=["t_l"])
    P.op("dve", "tensor_scalar", out=t_r, in0=t_r, scalar1=0.0, scalar2=None, op0=ALU.max,
         reads=["t_r"], writes=["t_r"])
    P.op("dve", "tensor_tensor", out=t_r, in0=t_r, in1=t_l, op=ALU.add, reads=["t_r", "t_l"], writes=["t_r"])
    P.op("dve", "tensor_scalar", out=c1, in0=t_r, scalar1=-8.0, scalar2=None, op0=ALU.mult,
         reads=["t_r"], writes=["c1"])
    P.op("dve", "tensor_scalar", out=c2, in0=t_r, scalar1=-16.0, scalar2=None, op0=ALU.mult,
         reads=["t_r"], writes=["c2"])

    base_mark = A.mark()

    wcnt = [0]

    def load_w(dst3, src, key=None, semkey=None):
        wcnt[0] += 1
        P.dma("pool", "wl%d" % (wcnt[0] % 2), out=dst3, in_=src, writes=[key])

    def norm_tile(xt, xkey, gain_rep, gkey, xs, xskey, junk, ss_rot, act_xs=False):
        ss, sskey = ss_rot.next()
        P.op("act", "activation", out=junk, in_=xt, func=AF.Square, accum_out=ss[:, 0:1],
             reads=[xkey], writes=["junk", sskey])
        P.op("dve", "tensor_scalar", out=ss[:, 1:2], in0=ss[:, 0:1], scalar1=1.0 / D, scalar2=EPS,
                                              op0=ALU.mult, op1=ALU.add, reads=[sskey], writes=[sskey])
        P.op("act", "activation", out=ss[:, 2:3], in_=ss[:, 1:2], func=AF.Sqrt, reads=[sskey], writes=[sskey])
        P.op("dve", "reciprocal", out=ss[:, 3:4], in_=ss[:, 2:3], reads=[sskey], writes=[sskey])
        P.op("dve", "scalar_tensor_tensor", out=xs, in0=xt, scalar=ss[:, 3:4], in1=gain_rep,
                                                     op0=ALU.mult, op1=ALU.mult,
             reads=[xkey, sskey, gkey], writes=[xskey])
        return ss, sskey

    def transpose_tile(xs, xskey, psT, pskey, uT3, ukey, col0, evac_eng):
        pT = psT.bitcast(BF16)
        for c in range(8):
            P.op("pe", "transpose", out=pT[:, c * 128:(c + 1) * 128], in_=xs[:, c * 128:(c + 1) * 128],
                                                  identity=ident, reads=[xskey, "ident"], writes=[pskey])
        src = v3(pT, 8)
        dst = uT3[:, :, col0:col0 + 128]
        if evac_eng == "act":
            P.op("act", "activation", out=dst, in_=src, func=AF.Copy, reads=[pskey], writes=[ukey])
        else:
            P.op("dve", "tensor_copy", out=dst, in_=src, reads=[pskey], writes=[ukey])

    def rope_evac(pk, pkkey, n, C32, S32, rkey, kT, kTkey, t1, t2, tkey, swp, swpkey, evac_eng="act"):
        if evac_eng == "act":
            P.op("act", "activation", out=kT, in_=pk, func=AF.Copy, reads=[pkkey], writes=[kTkey])
        else:
            P.op("dve", "tensor_copy", out=kT, in_=pk, reads=[pkkey], writes=[kTkey])
        if not DBG.get('Krope', 1):
            return
        if DBG.get('Kt1', 1):
            P.op("dve", "tensor_tensor", out=t1[0:32, :], in0=pk[0:32, :], in1=(C32 if DBG.get('Kc32', 1) else t2[0:32, :]), op=ALU.mult,
                 reads=[pkkey, rkey] + ([kTkey] if DBG.get('Kser', 0) else []), writes=[tkey + "1"])
        if DBG.get('Ksw', 1):
            P.op("pe", "matmul", swp[:, 0:n], lhsT=pmat, rhs=kT, start=True, stop=True,
                 reads=[kTkey, "pmat"], writes=[swpkey])
        if not DBG.get('Krope2', 1):
            return
        P.op("dve", "tensor_tensor", out=t2[0:32, :], in0=swp[0:32, 0:n], in1=S32, op=ALU.mult,
             reads=[swpkey, rkey], writes=[tkey + "2"])
        P.op("pool", "tensor_tensor", out=kT[0:32, :], in0=t1[0:32, :], in1=t2[0:32, :], op=ALU.add,
             reads=[tkey + "1", tkey + "2"], writes=[kTkey])

    NG = S // 512
    wkvx = A.alloc(8 * 3072, BF16)
    wkvx3 = v3(wkvx, 8)
    wrg = A.alloc(2 * 8 * 256, BF16)
    wrg4 = wrg.rearrange("p (a c j) -> p a c j", a=2, c=8)
    g_pre = A.alloc(D)
    xts = [(A.alloc(D), ("xt", j)) for j in range(4)]
    xs_rot = Rot([(A.alloc(D, BF16), ("xs", i)) for i in range(2)])
    junk = A.alloc(D, BF16)
    ss_rot = Rot([(A.alloc(4), ("ss", i)) for i in range(4)])
    uT_rot = Rot([(A.alloc(8 * 512, BF16), ("uT", i)) for i in range(2)])
    kT_rot = Rot([(A.alloc(512, BF16), ("kT", i)) for i in range(3)])
    t_rot = Rot([((A.alloc(512), A.alloc(512)), "tr%d" % i) for i in range(2)])
    rope_rot = Rot([(A.alloc(2 * 512), ("rope", i)) for i in range(2)])
    vst = A.alloc(8 * 4 * 130, BF16)
    vst4 = vst.rearrange("p (h j e) -> p h j e", h=8, j=4)
    xl_rot = Rot([(A.alloc(515), ("xl", i)) for i in range(2)])
    xc_all = A.alloc(8 * 512)
    xc3 = v3(xc_all, 8)
    xcb = A.alloc(8 * 512, BF16)
    xcb3 = v3(xcb, 8)
    lw_rot = Rot([(tuple(A.alloc(512) for _ in range(5)), "lw%d" % i) for i in range(2)])
    hsel_rot = Rot([(A.alloc(8 * 256, BF16), ("hsel", i)) for i in range(2)])
    hsel_tmp = A.alloc(256)

    for s_ in range(3):
        load_w(wkvx3[:, :, s_ * 1024:(s_ + 1) * 1024],
               w_in[:, 1024 + s_ * 1024:2048 + s_ * 1024].rearrange("(c p) n -> p c n", p=128),
               key=("wkvx", s_), semkey="w%d" % s_)
    load_w(wrg4[:, 0], w_rga.rearrange("g (k p) j -> p (g k) j", p=128), key="wrg_a", semkey="w3")
    load_w(wrg4[:, 1], w_rgi.rearrange("g (k p) j -> p (g k) j", p=128), key="wrg_i", semkey="w4")
    P.dma("sp", "c_gain", out=g_pre, in_=grep[:, 0:D], writes=["g_pre"])
    P.op("pool", "memset", vst4[:, :, :, 128:130], 1.0, writes=["vst"])

    def load_x_1a(g):
        for j in range(4):
            xt, xkey = xts[j]
            r0 = g * 512 + j * 128
            P.dma("sp", "x%d" % j, out=xt, in_=xa[r0:r0 + 128, :], writes=[xkey])

    load_x_1a(0)
    mm_banks = Rot([4, 5, 6, 7])

    def normT_1a(g):
        uT, ukey = uT_rot.next()
        uT3 = v3(uT, 8)
        for j in range(4):
            xt, xkey = xts[j]
            xs, xskey = xs_rot.next()
            norm_tile(xt, xkey, g_pre, "g_pre", xs, xskey, junk, ss_rot)
            psT, pskey = ps(j % 2)
            transpose_tile(xs, xskey, psT, pskey, uT3, ukey, j * 128, "act")
        if g + 1 < NG:
            load_x_1a(g + 1)
        return uT3, ukey

    NG1 = DBG.get('ng', NG)
    cur = normT_1a(0)
    for g in range(NG1):
        t0 = g * 512
        uT3, ukey = cur
        rp, rpkey = rope_rot.next()
        rp3 = v3(rp, 2)
        P.dma("sp", "rope%d" % (g % 2),
              out=rp3[0:32, :, :], in_=ropek[:, :, t0:t0 + 512].rearrange("a r t -> r a t"), writes=[rpkey])
        C32, S32 = rp3[0:32, 0, :], rp3[0:32, 1, :]
        for c in range(8):
            b = mm_banks.next()
            px, pxkey = ps(b)
            for k in range(8):
                P.op("pe", "matmul",
                     px, lhsT=wkvx3[:, k, 2048 + c * 128:2048 + (c + 1) * 128], rhs=uT3[:, k, :],
                     start=(k == 0), stop=(k == 7), reads=[ukey, ("wkvx", 2)], writes=[pxkey])
            xl, xlkey = xl_rot.next()
            P.op("pool", "tensor_copy", out=xl[:, 0:3], in_=carryx[:, 3 * c:3 * c + 3],
                 reads=["carryx"], writes=[xlkey])
            P.op("act", "activation", out=xl[:, 3:515], in_=px, func=AF.Copy,
                 reads=[pxkey], writes=[xlkey])
            P.op("pool", "tensor_copy", out=carryx[:, 3 * c:3 * c + 3], in_=xl[:, 512:515],
                 reads=[xlkey], writes=["carryx"])
            xcc = xc3[:, c, :]
            P.op("dve", "tensor_scalar",
                 out=xcc, in0=xl[:, 0:512], scalar1=vcol(0, c), scalar2=vcol(32, c), op0=ALU.mult, op1=ALU.add,
                 reads=[xlkey, "vec"], writes=[("xc", c)])
            for jj in range(1, 4):
                P.op("dve", "scalar_tensor_tensor",
                     out=xcc, in0=xl[:, jj:jj + 512], scalar=vcol(8 * jj, c), in1=xcc, op0=ALU.mult, op1=ALU.add,
                     reads=[xlkey, "vec", ("xc", c)], writes=[("xc", c)])
            P.op("pool", "tensor_copy", out=xcb3[:, c, :], in_=xcc,
                 reads=[("xc", c)], writes=[("xcb", c)])

        def k_part2(h, kT, kTkey, t1, t2, tkey, swp, swpkey):
            P.op("pe", "matmul", swp[:, 0:512], lhsT=pmat, rhs=kT, start=True, stop=True,
                 reads=[kTkey, "pmat"], writes=[swpkey])
            P.op("dve", "tensor_tensor", out=t2[0:32, :], in0=swp[0:32, 0:512], in1=S32, op=ALU.mult,
                 reads=[swpkey, rpkey], writes=[tkey + "2"])
            P.op("pool", "tensor_tensor", out=kT[0:32, :], in0=t1[0:32, :], in1=t2[0:32, :], op=ALU.add,
                 reads=[tkey + "1", tkey + "2"], writes=[kTkey])
            P.op("dve", "tensor_reduce",
                 out=ksum[:, h * 32 + 2 * g:h * 32 + 2 * g + 2], in_=v3(kT, 2), axis=AX.X, op=ALU.add,
                 reads=[kTkey], writes=["ksum"])
            P.dma("sp", "kst%d" % ((g * NH + h) % 3),
                  out=Ks[h, :, t0:t0 + 512], in_=kT, reads=[kTkey], writes=[("Ks", h)])

        pend = None
        for h in range(NH):
            b = mm_banks.next()
            pk, pkkey = ps(b)
            for c in range(8):
                P.op("pe", "matmul", pk, lhsT=wkvx3[:, c, h * 128:(h + 1) * 128], rhs=uT3[:, c, :],
                     start=(c == 0), stop=(c == 7), reads=[ukey, ("wkvx", 0)], writes=[pkkey])
            kT, kTkey = kT_rot.next()
            (t1, t2), tkey = t_rot.next()
            swp, swpkey = ps(2 + (h % 2))
            P.op("act", "activation", out=kT, in_=pk, func=AF.Copy, reads=[pkkey], writes=[kTkey])
            P.op("dve", "tensor_tensor", out=t1[0:32, :], in0=pk[0:32, :], in1=C32, op=ALU.mult,
                 reads=[pkkey, rpkey], writes=[tkey + "1"])
            if pend is not None:
                k_part2(*pend)
            pend = (h, kT, kTkey, t1, t2, tkey, swp, swpkey)
        k_part2(*pend)

        for j in range(4):
            for hh in range(2):
                b = mm_banks.next()
                pv, pvkey = ps(b)
                for c in range(8):
                    P.op("pe", "matmul",
                         pv, lhsT=uT3[:, c, j * 128:(j + 1) * 128], rhs=wkvx3[:, c, 1024 + hh * 512:1024 + (hh + 1) * 512],
                         start=(c == 0), stop=(c == 7), reads=[ukey, ("wkvx", 1)], writes=[pvkey])
                P.op("act", "activation",
                     out=vst4[:, hh * 4:(hh + 1) * 4, j, 0:128], in_=v3(pv, 4), func=AF.Copy,
                     reads=[pvkey], writes=["vst"])
        P.dma("sp", "vst",
              out=Vs.rearrange("h p (j e) -> p h j e", e=130)[:, :, 4 * g:4 * g + 4, :], in_=vst4,
              reads=["vst"], writes=["Vs"])

        if g + 1 < NG1:
            cur = normT_1a(g + 1)

        hsel, hselkey = hsel_rot.next()
        hsel3 = v3(hsel, 8)
        for oc in range(8):
            gi, jc = oc // 2, oc % 2
            (r_, i_, a_, m_, h_), lwkey = lw_rot.next()
            br = mm_banks.next()
            pr, prkey = ps(br)
            bi = mm_banks.next()
            pi, pikey = ps(bi)
            for which, pp, ppkey in ((0, pr, prkey), (1, pi, pikey)):
                for kc in range(2):
                    P.op("pe", "matmul",
                         pp, lhsT=wrg4[:, which, gi * 2 + kc, jc * 128:(jc + 1) * 128], rhs=xcb3[:, gi * 2 + kc, :],
                         start=(kc == 0), stop=(kc == 1),
                         reads=[("xcb", gi * 2), ("xcb", gi * 2 + 1), "wrg_a", "wrg_i"], writes=[ppkey])
            P.op("act", "activation", out=r_, in_=pr, func=AF.Sigmoid, bias=vcol(40, oc),
                 reads=[prkey, "vec"], writes=[lwkey + "r"])
            P.op("act", "activation", out=i_, in_=pi, func=AF.Sigmoid, bias=vcol(48, oc),
                 reads=[pikey, "vec"], writes=[lwkey + "i"])
            P.op("act", "activation", out=a_, in_=r_, func=AF.Exp, scale=c1[:, oc:oc + 1],
                 reads=[lwkey + "r", "c1"], writes=[lwkey + "a"])
            P.op("act", "activation", out=m_, in_=r_, func=AF.Exp, scale=c2[:, oc:oc + 1],
                 reads=[lwkey + "r", "c2"], writes=[lwkey + "m"])
            P.op("act", "activation", out=m_, in_=m_, func=AF.Sqrt, scale=-1.0, bias=1.0,
                 reads=[lwkey + "m"], writes=[lwkey + "m"])
            P.op("pool", "tensor_tensor", out=i_, in0=i_, in1=xc3[:, oc, :], op=ALU.mult,
                 reads=[lwkey + "i", ("xc", oc)], writes=[lwkey + "i"])
            P.op("pool", "tensor_tensor", out=i_, in0=i_, in1=m_, op=ALU.mult,
                 reads=[lwkey + "i", lwkey + "m"], writes=[lwkey + "i"])
            P.op("dve", "tensor_tensor_scan",
                 out=h_, data0=a_, data1=i_, initial=carryh[:, oc:oc + 1], op0=ALU.mult, op1=ALU.add,
                 reads=[lwkey + "a", lwkey + "i", "carryh"], writes=[lwkey + "h"])
            P.op("dve", "tensor_copy", out=carryh[:, oc:oc + 1], in_=h_[:, 511:512],
                 reads=[lwkey + "h"], writes=["carryh"])
            P.op("pool", "tensor_scalar", out=hsel_tmp, in0=h_[:, 256:512], scalar1=psel[:, 1:2],
                 scalar2=None, op0=ALU.mult,
                 reads=[lwkey + "h", "psel"], writes=["hsel_tmp"])
            P.op("dve", "scalar_tensor_tensor",
                 out=hsel3[:, oc, :], in0=h_[:, 0:256], scalar=psel[:, 0:1], in1=hsel_tmp, op0=ALU.mult, op1=ALU.add,
                 reads=[lwkey + "h", "psel", "hsel_tmp"], writes=[hselkey])
        P.dma("sp", "hst%d" % (g % 2),
              out=Hs[:, :, g * 256:(g + 1) * 256].rearrange("c p t -> p c t"), in_=hsel3,
              reads=[hselkey], writes=["Hs"])

    P.op("dve", "tensor_copy", out=ksum_hi, in_=ksum, reads=["ksum"], writes=["ksum_hi"])
    P.op("dve", "tensor_tensor", out=ksum, in0=ksum, in1=ksum_hi, op=ALU.subtract,
         reads=["ksum", "ksum_hi"], writes=["ksum"])
    P.op("dve", "tensor_copy", out=ksum_lo, in_=ksum, reads=["ksum"], writes=["ksum_lo"])

    if stop == "1a":
        P.barrier()
        P.emit()
        return nc
    P.barrier()
    A.release(base_mark)
    NGO = SO // 512
    wq = A.alloc(8 * 1024, BF16)
    wq3 = v3(wq, 8)
    wgl = A.alloc(8 * 1024, BF16)
    wgl3 = v3(wgl, 8)
    wgt = A.alloc(8 * 1024, BF16)
    wgt3 = v3(wgt, 8)
    wbl = A.alloc(8 * 1024, BF16)
    wbl3 = v3(wbl, 8)
    g_pre = A.alloc(D)
    xts = [(A.alloc(D), ("xt", j)) for j in range(4)]
    xs_rot = Rot([(A.alloc(D, BF16), ("xs", i)) for i in range(2)])
    junk = A.alloc(D, BF16)
    ss_rot = Rot([(A.alloc(4), ("ss", i)) for i in range(4)])
    uT_rot = Rot([(A.alloc(8 * 512, BF16), ("uT", i)) for i in range(2)])
    kT_rot = Rot([(A.alloc(512, BF16), ("kT", i)) for i in range(3)])
    t_rot = Rot([((A.alloc(512), A.alloc(512)), "tr%d" % i) for i in range(2)])
    rope_rot = Rot([(A.alloc(2 * 512), ("rope", i)) for i in range(2)])
    hs_rot = Rot([(A.alloc(8 * 512, BF16), ("hs", i)) for i in range(2)])
    gw_rot = Rot([(tuple(A.alloc(512) for _ in range(3)), "gw%d" % i) for i in range(2)])
    hg_rot = Rot([(A.alloc(8 * 512, BF16), ("hg", i)) for i in range(2)])
    sg_rot = Rot([(A.alloc(512), ("sg", i)) for i in range(2)])
    yl_rot = Rot([(A.alloc(8 * 512, BF16), ("yl", i)) for i in range(2)])

    load_w(wq3, w_in[:, 0:1024].rearrange("(c p) n -> p c n", p=128), key="wq", semkey="w0")
    load_w(wgl3, w_in[:, 4096:5120].rearrange("(c p) n -> p c n", p=128), key="wgl", semkey="w1")
    load_w(wgt3, w_in[:, 6144:7168].rearrange("(c p) n -> p c n", p=128), key="wgt", semkey="w2")
    load_w(wbl3, w_bl.rearrange("(c p) n -> p c n", p=128), key="wbl", semkey="w3")
    P.dma("sp", "c_gain", out=g_pre, in_=grep[:, 0:D], writes=["g_pre"])

    def load_x_own(g, ntile):
        for j in range(ntile):
            xt, xkey = xts[j]
            r0 = g * ntile * 128 + j * 128
            P.dma("sp", "x%d" % j, out=xt, in_=xo[r0:r0 + 128, :], writes=[xkey])

    load_x_own(0, 4)
    for g in range(NGO):
        t0 = g * 512
        rp, rpkey = rope_rot.next()
        rp3 = v3(rp, 2)
        P.dma("sp", "rope%d" % (g % 2),
            out=rp3[0:32, :, :], in_=ropeq[:, :, t0:t0 + 512].rearrange("a r t -> r a t"), writes=[rpkey])
        hs, hskey = hs_rot.next()
        hs3 = v3(hs, 8)
        P.dma("sp", "hld%d" % (g % 2),
            out=hs3, in_=Hs[:, :, t0:t0 + 512].rearrange("c p t -> p c t"), reads=["Hs"], writes=[hskey])
        uT, ukey = uT_rot.next()
        uT3 = v3(uT, 8)
        for j in range(4):
            xt, xkey = xts[j]
            xs, xskey = xs_rot.next()
            norm_tile(xt, xkey, g_pre, "g_pre", xs, xskey, junk, ss_rot)
            psT, pskey = ps(j % 2)
            transpose_tile(xs, xskey, psT, pskey, uT3, ukey, j * 128, "act")
        if g + 1 < NGO:
            load_x_own(g + 1, 4)
        for h in range(NH):
            b = mm_banks.next()
            pk, pkkey = ps(b)
            for c in range(8):
                P.op("pe", "matmul", pk, lhsT=wq3[:, c, h * 128:(h + 1) * 128], rhs=uT3[:, c, :],
                                                               start=(c == 0), stop=(c == 7),
                     reads=[ukey, "wq"], writes=[pkkey])
            kT, kTkey = kT_rot.next()
            (t1, t2), tkey = t_rot.next()
            swp, swpkey = ps(2 + (h % 2))
            rope_evac(pk, pkkey, 512, rp3[0:32, 0, :], rp3[0:32, 1, :], rpkey, kT, kTkey, t1, t2, tkey, swp, swpkey)
            P.dma("sp", "kst%d" % ((g * NH + h) % 3),
                out=Qs[h, :, t0:t0 + 512], in_=kT, reads=[kTkey], writes=[("Qs", h)])
        hg, hgkey = hg_rot.next()
        hg3 = v3(hg, 8)
        for oc in range(8):
            b = mm_banks.next()
            pg, pgkey = ps(b)
            for c in range(8):
                P.op("pe", "matmul", pg, lhsT=wgl3[:, c, oc * 128:(oc + 1) * 128], rhs=uT3[:, c, :],
                                                                 start=(c == 0), stop=(c == 7),
                     reads=[ukey, "wgl"], writes=[pgkey])
            (gx, gq, gs), gwkey = gw_rot.next()
            P.op("act", "activation", out=gx, in_=pg, func=AF.Copy, reads=[pgkey], writes=[gwkey + "x"])
            P.op("act", "activation", out=gq, in_=pg, func=AF.Square, reads=[pgkey], writes=[gwkey + "q"])
            P.op("dve", "tensor_scalar", out=gq, in0=gq, scalar1=0.044715, scalar2=1.0, op0=ALU.mult, op1=ALU.add,
                 reads=[gwkey + "q"], writes=[gwkey + "q"])
            P.op("pool", "tensor_tensor", out=gq, in0=gq, in1=gx, op=ALU.mult,
                 reads=[gwkey + "q", gwkey + "x"], writes=[gwkey + "q"])
            P.op("act", "activation", out=gs, in_=gq, func=AF.Sigmoid, scale=1.5957691216057308,
                 reads=[gwkey + "q"], writes=[gwkey + "s"])
            P.op("pool", "tensor_tensor", out=gs, in0=gs, in1=gx, op=ALU.mult,
                 reads=[gwkey + "s", gwkey + "x"], writes=[gwkey + "s"])
            P.op("dve", "tensor_tensor", out=hg3[:, oc, :], in0=gs, in1=hs3[:, oc, :], op=ALU.mult,
                 reads=[gwkey + "s", hskey], writes=[hgkey])
        yl, ylkey = yl_rot.next()
        yl3 = v3(yl, 8)
        for oc in range(8):
            b = mm_banks.next()
            pg, pgkey = ps(b)
            for c in range(8):
                P.op("pe", "matmul", pg, lhsT=wgt3[:, c, oc * 128:(oc + 1) * 128], rhs=uT3[:, c, :],
                                                                 start=(c == 0), stop=(c == 7),
                     reads=[ukey, "wgt"], writes=[pgkey])
            sg, sgkey = sg_rot.next()
            P.op("act", "activation", out=sg, in_=pg, func=AF.Sigmoid, reads=[pgkey], writes=[sgkey])
            b2 = mm_banks.next()
            py, pykey = ps(b2)
            for c in range(8):
                P.op("pe", "matmul", py, lhsT=wbl3[:, c, oc * 128:(oc + 1) * 128], rhs=hg3[:, c, :],
                                                                 start=(c == 0), stop=(c == 7),
                     reads=[hgkey, "wbl"], writes=[pykey])
            P.op("dve", "tensor_tensor", out=yl3[:, oc, :], in0=py, in1=sg, op=ALU.mult,
                 reads=[pykey, sgkey], writes=[ylkey])
        P.dma("sp", "yst%d" % (g % 2),
            out=YL[:, :, t0:t0 + 512].rearrange("c p t -> p c t"), in_=yl3, reads=[ylkey], writes=["YL"])

    if stop == "1b":
        P.barrier()
        P.emit()
        return nc
    P.barrier()
    A.release(base_mark)
    attnT = A.alloc(NH * SO, BF16)
    attnT3 = v3(attnT, NH)
    p2_mark = A.mark()
    kv_rot = Rot([((A.alloc(S, BF16), A.alloc(64 * 130, BF16), A.alloc(SO, BF16)), "kv%d" % i) for i in range(2)])
    gmask = A.alloc(16 * 32)
    cmaskf = A.alloc(1024)
    cmask = A.alloc(1024, BF16)
    cmask3 = v3(cmask, 2)
    pt_rot = Rot([(A.alloc(512, BF16), ("pt", i)) for i in range(3)])
    acc_rot = Rot([(A.alloc(2 * 130), ("acc", i)) for i in range(2)])
    sel_rot = Rot([(A.alloc(2 * 32), ("sel", i)) for i in range(2)])
    g1_rot = Rot([(A.alloc(32 + 8 + 1), ("g1", i)) for i in range(4)])
    on_rot = Rot([(A.alloc(2 * 128, BF16), ("on", i)) for i in range(2)])
    rinv = A.alloc(2)

    P.dma("sp", "c_gmask", out=gmask, in_=gmask_d, writes=["gmask"])
    P.dma("sp", "c_cmask", out=cmaskf, in_=cmask_d, writes=["cmaskf"])
    P.op("dve", "tensor_copy", out=cmask, in_=cmaskf, reads=["cmaskf"], writes=["cmask"])

    def load_head(h):
        (KT, VV, QT), kvkey = kv_rot.next()
        P.dma("sp", kvkey + "k", out=KT, in_=Ks[h], reads=[("Ks", h)], writes=[kvkey + "k"])
        P.dma("sp", kvkey + "v", out=VV, in_=Vs[h], reads=["Vs"], writes=[kvkey + "v"])
        P.dma("sp", kvkey + "q", out=QT, in_=Qs[h], reads=[("Qs", h)], writes=[kvkey + "q"])
        return (KT, v3(VV, 64), QT), kvkey

    s_banks = Rot([0, 1, 2])
    o_banks = Rot([3, 4])
    nxt = load_head(0)
    for h in range(NH):
        (KT, VV3, QT), kvkey = nxt
        if h + 1 < NH:
            nxt = load_head(h + 1)
        units = [(i, j) for i in range(16) for j in range(2 * i + 2)]
        state = {}

        def emit_S(u):
            i, j = units[u]
            b = s_banks.next()
            sp_, spkey = ps(b)
            for kt in range(2):
                P.op("pe", "matmul",
                    sp_[:, kt * 256:(kt + 1) * 256], lhsT=KT[:, (2 * j + kt) * 128:(2 * j + kt + 1) * 128],
                    rhs=QT[:, i * 256:(i + 1) * 256], start=True, stop=True,
                    reads=[kvkey + "k", kvkey + "q"], writes=[spkey])
            state[u] = (sp_, spkey)

        def emit_gate(i):
            pgt, pgtkey = ps(5)
            acc, acckey = acc_rot.next()
            sel, selkey = sel_rot.next()
            for qt in range(2):
                qcols = QT[:, i * 256 + qt * 128:i * 256 + (qt + 1) * 128]
                P.op("pe", "matmul", pgt[:, qt * 32:(qt + 1) * 32], lhsT=qcols,
                                                                  rhs=ksum_hi[:, h * 32:(h + 1) * 32], start=True, stop=False,
                     reads=[kvkey + "q", "ksum_hi"], writes=[pgtkey])
                P.op("pe", "matmul", pgt[:, qt * 32:(qt + 1) * 32], lhsT=qcols,
                                                                  rhs=ksum_lo[:, h * 32:(h + 1) * 32], start=False, stop=True,
                     reads=[kvkey + "q", "ksum_lo"], writes=[pgtkey])
            for qt in range(2):
                g1, g1key = g1_rot.next()
                P.op("dve", "tensor_tensor", out=g1[:, 0:32], in0=pgt[:, qt * 32:(qt + 1) * 32],
                                                                    in1=gmask[:, i * 32:(i + 1) * 32], op=ALU.add,
                     reads=[pgtkey, "gmask"], writes=[g1key])
                P.op("dve", "max", out=g1[:, 32:40], in_=g1[:, 0:32], reads=[g1key], writes=[g1key])
                P.op("dve", "tensor_scalar", out=g1[:, 40:41], in0=g1[:, 35:36], scalar1=-1e29, scalar2=None,
                                                             op0=ALU.max, reads=[g1key], writes=[g1key])
                P.op("dve", "tensor_scalar", out=sel[:, qt * 32:(qt + 1) * 32], in0=g1[:, 0:32],
                                                                             scalar1=g1[:, 40:41], scalar2=None, op0=ALU.is_ge,
                     reads=[g1key], writes=[selkey])
            return acc, acckey, sel, selkey

        blk = {}

        def emit_rest(u):
            i, j = units[u]
            sp_, spkey = state.pop(u)
            acc, acckey, sel, selkey = blk[i]
            pt, ptkey = pt_rot.next()
            P.op("act", "activation", out=pt, in_=sp_, func=AF.Exp, scale=SCALE, reads=[spkey], writes=[ptkey])
            if j >= 2 * i:
                P.op("pool", "tensor_tensor", out=pt, in0=pt, in1=cmask3[:, j - 2 * i, :], op=ALU.mult,
                     reads=[ptkey, "cmask"], writes=[ptkey])
            b = o_banks.next()
            po, pokey = ps(b)
            po3 = po[:, 0:260].rearrange("p (a b) -> p a b", a=2)
            for qt in range(2):
                for kt in range(2):
                    P.op("pe", "matmul",
                        po3[:, qt, :], lhsT=pt[:, kt * 256 + qt * 128:kt * 256 + (qt + 1) * 128], rhs=VV3[:, 2 * j + kt, :],
                        start=(kt == 0), stop=(kt == 1), reads=[ptkey, kvkey + "v"], writes=[pokey])
            acc3 = v3(acc, 2)
            for qt in range(2):
                sc = sel[:, qt * 32 + j:qt * 32 + j + 1]
                if j == 0:
                    P.op("dve", "tensor_scalar", out=acc3[:, qt, :], in0=po3[:, qt, :], scalar1=sc,
                                                                        scalar2=None, op0=ALU.mult,
                         reads=[pokey, selkey], writes=[acckey])
                else:
                    P.op("dve", "scalar_tensor_tensor", out=acc3[:, qt, :], in0=po3[:, qt, :], scalar=sc,
                                                                               in1=acc3[:, qt, :], op0=ALU.mult, op1=ALU.add,
                         reads=[pokey, selkey, acckey], writes=[acckey])

        pending = []

        def emit_norm(i):
            acc, acckey, sel, selkey = blk[i]
            acc3 = v3(acc, 2)
            on, onkey = on_rot.next()
            on3 = v3(on, 2)
            P.op("dve", "reciprocal", out=rinv, in_=acc3[:, :, 128], reads=[acckey], writes=["rinv"])
            for qt in range(2):
                P.op("dve", "tensor_scalar", out=on3[:, qt, :], in0=acc3[:, qt, 0:128], scalar1=rinv[:, qt:qt + 1],
                                                             scalar2=None, op0=ALU.mult,
                     reads=[acckey, "rinv"], writes=[onkey])
            pending.append((i, on3, onkey))

        def emit_fin():
            i, on3, onkey = pending.pop(0)
            pT_, pTkey = ps(6)
            pTb = pT_.bitcast(BF16)
            for qt in range(2):
                P.op("pe", "transpose", out=pTb[:, qt * 128:(qt + 1) * 128], in_=on3[:, qt, :], identity=ident,
                     reads=[onkey, "ident"], writes=[pTkey])
            P.op("dve", "tensor_copy", out=attnT3[:, h, i * 256:(i + 1) * 256], in_=pTb[:, 0:256],
                 reads=[pTkey], writes=["attnT"])

        U = len(units)
        blk[0] = emit_gate(0)
        emit_S(0)
        for u in range(U):
            i, j = units[u]
            if u + 1 < U:
                ni, nj = units[u + 1]
                if nj == 0:
                    blk[ni] = emit_gate(ni)
                emit_S(u + 1)
            emit_rest(u)
            if j == 1 and pending:
                emit_fin()
            if j == 2 * i + 1:
                emit_norm(i)
        while pending:
            emit_fin()

    if debug:
        ATT = nc.dram_tensor("ATT", [NH, 128, SO], BF16, kind="ExternalOutput").ap()
        P.dma("sp", "dbg_att", out=ATT.rearrange("h p t -> p h t"), in_=attnT3, reads=["attnT"], writes=["ATT"])
    if stop == "2":
        P.barrier()
        P.emit()
        return nc
    P.barrier()
    A.release(p2_mark)
    NG3 = SO // 256
    wga = A.alloc(8 * 1024, BF16)
    wga3 = v3(wga, 8)
    wba = A.alloc(8 * 1024, BF16)
    wba3 = v3(wba, 8)
    wo = A.alloc(8 * 1024, BF16)
    wo3 = v3(wo, 8)
    g_pre = A.alloc(D)
    g_post = A.alloc(D)
    xts = [(A.alloc(D), ("xt", j)) for j in range(2)]
    xs_rot = Rot([(A.alloc(D, BF16), ("xs", i)) for i in range(2)])
    junk = A.alloc(D, BF16)
    ss_rot = Rot([(A.alloc(4), ("ss", i)) for i in range(4)])
    uT_rot = Rot([(A.alloc(8 * 256, BF16), ("uT", i)) for i in range(2)])
    sg_rot = Rot([(A.alloc(256), ("sg", i)) for i in range(2)])
    yld_rot = Rot([(A.alloc(8 * 256, BF16), ("yld", i)) for i in range(2)])
    mT_rot = Rot([(A.alloc(8 * 256, BF16), ("mT", i)) for i in range(2)])
    mix_rot = Rot([(A.alloc(D), ("mix", i)) for i in range(2)])

    load_w(wga3, w_in[:, 5120:6144].rearrange("(c p) n -> p c n", p=128), key="wga", semkey="w0")
    load_w(wba3, w_ba.rearrange("(c p) n -> p c n", p=128), key="wba", semkey="w1")
    load_w(wo3, w_out.rearrange("(c p) n -> p c n", p=128), key="wo", semkey="w2")
    P.dma("sp", "c_gain", out=g_pre, in_=grep[:, 0:D], writes=["g_pre"])
    P.dma("sp", "c_gain2", out=g_post, in_=grep[:, D:2 * D], writes=["g_post"])

    load_x_own(0, 2)
    for g in range(NG3):
        t0 = g * 256
        yld, yldkey = yld_rot.next()
        yld3 = v3(yld, 8)
        P.dma("sp", "yld%d" % (g % 2),
            out=yld3, in_=YL[:, :, t0:t0 + 256].rearrange("c p t -> p c t"), reads=["YL"], writes=[yldkey])
        uT, ukey = uT_rot.next()
        uT3 = v3(uT, 8)
        mix_list = []
        for j in range(2):
            xt, xkey = xts[j]
            xs, xskey = xs_rot.next()
            norm_tile(xt, xkey, g_pre, "g_pre", xs, xskey, junk, ss_rot)
            psT, pskey = ps(j % 2)
            transpose_tile(xs, xskey, psT, pskey, uT3, ukey, j * 128, "act")
        mT, mTkey = mT_rot.next()
        mT3 = v3(mT, 8)
        for oc in range(8):
            b = mm_banks.next()
            pg, pgkey = ps(b)
            for c in range(8):
                P.op("pe", "matmul", pg[:, 0:256], lhsT=wga3[:, c, oc * 128:(oc + 1) * 128], rhs=uT3[:, c, :],
                                                                 start=(c == 0), stop=(c == 7),
                     reads=[ukey, "wga"], writes=[pgkey])
            sg, sgkey = sg_rot.next()
            P.op("act", "activation", out=sg, in_=pg[:, 0:256], func=AF.Sigmoid, reads=[pgkey], writes=[sgkey])
            b2 = mm_banks.next()
            py, pykey = ps(b2)
            for c in range(8):
                P.op("pe", "matmul", py[:, 0:256], lhsT=wba3[:, c, oc * 128:(oc + 1) * 128],
                                                                 rhs=attnT3[:, c, t0:t0 + 256], start=(c == 0), stop=(c == 7),
                     reads=["attnT", "wba"], writes=[pykey])
            P.op("dve", "tensor_tensor", out=sg, in0=py[:, 0:256], in1=sg, op=ALU.mult,
                 reads=[pykey, sgkey], writes=[sgkey])
            P.op("pool", "tensor_tensor", out=mT3[:, oc, :], in0=sg, in1=yld3[:, oc, :], op=ALU.add,
                 reads=[sgkey, yldkey], writes=[mTkey])
        for j in range(2):
            xt, xkey = xts[j]
            mix, mixkey = mix_rot.next()
            for hh in range(2):
                b = mm_banks.next()
                pm, pmkey = ps(b)
                for c in range(8):
                    P.op("pe", "matmul", pm, lhsT=mT3[:, c, j * 128:(j + 1) * 128],
                                                                          rhs=wo3[:, c, hh * 512:(hh + 1) * 512],
                                                                          start=(c == 0), stop=(c == 7),
                         reads=[mTkey, "wo"], writes=[pmkey])
                P.op("act", "activation", out=mix[:, hh * 512:(hh + 1) * 512], in_=pm, func=AF.Copy,
                     reads=[pmkey], writes=[mixkey])
            ss, sskey = ss_rot.next()
            P.op("act", "activation", out=junk, in_=mix, func=AF.Square, accum_out=ss[:, 0:1],
                 reads=[mixkey], writes=["junk", sskey])
            P.op("dve", "tensor_scalar", out=ss[:, 1:2], in0=ss[:, 0:1], scalar1=1.0 / D, scalar2=EPS,
                                                         op0=ALU.mult, op1=ALU.add, reads=[sskey], writes=[sskey])
            P.op("act", "activation", out=ss[:, 2:3], in_=ss[:, 1:2], func=AF.Sqrt, reads=[sskey], writes=[sskey])
            P.op("dve", "reciprocal", out=ss[:, 3:4], in_=ss[:, 2:3], reads=[sskey], writes=[sskey])
            P.op("dve", "scalar_tensor_tensor", out=mix, in0=mix, scalar=ss[:, 3:4], in1=g_post,
                                                                         op0=ALU.mult, op1=ALU.mult,
                 reads=[mixkey, sskey, "g_post"], writes=[mixkey])
            P.op("pool", "tensor_tensor", out=mix, in0=mix, in1=xt, op=ALU.add,
                 reads=[mixkey, xkey], writes=[mixkey])
            r0 = t0 + j * 128
            P.dma("sp", "h1st%d" % j, out=H1[r0:r0 + 128, :], in_=mix,
                  reads=[mixkey], writes=["H1"])
        if g + 1 < NG3:
            load_x_own(g + 1, 2)

    if stop == "3a":
        P.barrier()
        P.emit()
        return nc
    P.barrier()
    A.release(base_mark)
    wup = A.alloc(8 * 4096, BF16)
    wup3 = v3(wup, 8)
    wdn = A.alloc(32 * 1024, BF16)
    wdn3 = v3(wdn, 32)
    g_pre = A.alloc(D)
    g_post = A.alloc(D)
    xts = [(A.alloc(D), ("xt", j)) for j in range(2)]
    xs_rot = Rot([(A.alloc(D, BF16), ("xs", i)) for i in range(1)])
    junk = A.alloc(D, BF16)
    ss_rot = Rot([(A.alloc(4), ("ss", i)) for i in range(4)])
    uT_rot = Rot([(A.alloc(8 * 256, BF16), ("uT", i)) for i in range(1)])
    aT = A.alloc(32 * 256, BF16)
    aT3 = v3(aT, 32)
    rl_rot = Rot([(A.alloc(256), ("rl", i)) for i in range(2)])
    mo_rot = Rot([(A.alloc(D), ("mo", i)) for i in range(1)])

    for s_ in range(4):
        load_w(wup3[:, :, s_ * 1024:(s_ + 1) * 1024], w_up[:, s_ * 1024:(s_ + 1) * 1024].rearrange("(c p) n -> p c n", p=128),
               key=("wup", s_), semkey="w%d" % s_)
    for s_ in range(4):
        load_w(wdn3[:, s_ * 8:(s_ + 1) * 8, :], w_down[s_ * 1024:(s_ + 1) * 1024, :].rearrange("(c p) n -> p c n", p=128),
               key=("wdn", s_), semkey="w%d" % (4 + s_) if s_ < 1 else "w%d" % s_)
    P.dma("sp", "c_gain", out=g_pre, in_=grep[:, 2 * D:3 * D], writes=["g_pre"])
    P.dma("sp", "c_gain2", out=g_post, in_=grep[:, 3 * D:4 * D], writes=["g_post"])

    def load_h1(g):
        for j in range(2):
            xt, xkey = xts[j]
            r0 = g * 256 + j * 128
            P.dma("sp", "x%d" % j, out=xt, in_=H1[r0:r0 + 128, :],
                  reads=["H1"], writes=[xkey])

    load_h1(0)
    for g in range(NG3):
        t0 = g * 256
        uT, ukey = uT_rot.next()
        uT3 = v3(uT, 8)
        for j in range(2):
            xt, xkey = xts[j]
            xs, xskey = xs_rot.next()
            norm_tile(xt, xkey, g_pre, "g_pre", xs, xskey, junk, ss_rot)
            psT, pskey = ps(j % 2)
            transpose_tile(xs, xskey, psT, pskey, uT3, ukey, j * 128, "act")
        for fc in range(32):
            b = mm_banks.next()
            pu, pukey = ps(b)
            for c in range(8):
                P.op("pe", "matmul", pu[:, 0:256], lhsT=wup3[:, c, fc * 128:(fc + 1) * 128], rhs=uT3[:, c, :],
                                                                 start=(c == 0), stop=(c == 7),
                     reads=[ukey, ("wup", fc // 8)], writes=[pukey])
            rl, rlkey = rl_rot.next()
            P.op("act", "activation", out=rl, in_=pu[:, 0:256], func=AF.Relu, reads=[pukey], writes=[rlkey])
            eng = "dve" if fc % 2 == 0 else "pool"
            P.op(eng, "tensor_tensor", out=aT3[:, fc, :], in0=rl, in1=rl, op=ALU.mult,
                 reads=[rlkey], writes=[("aT", fc)])
        for j in range(2):
            xt, xkey = xts[j]
            mo, mokey = mo_rot.next()
            for hh in range(2):
                b = mm_banks.next()
                pm, pmkey = ps(b)
                for fc in range(32):
                    P.op("pe", "matmul", pm, lhsT=aT3[:, fc, j * 128:(j + 1) * 128],
                                                                            rhs=wdn3[:, fc, hh * 512:(hh + 1) * 512],
                                                                            start=(fc == 0), stop=(fc == 31),
                         reads=[("aT", fc), ("wdn", fc // 8)], writes=[pmkey])
                P.op("act", "activation", out=mo[:, hh * 512:(hh + 1) * 512], in_=pm, func=AF.Copy,
                     reads=[pmkey], writes=[mokey])
            ss, sskey = ss_rot.next()
            P.op("act", "activation", out=junk, in_=mo, func=AF.Square, accum_out=ss[:, 0:1],
                 reads=[mokey], writes=["junk", sskey])
            P.op("dve", "tensor_scalar", out=ss[:, 1:2], in0=ss[:, 0:1], scalar1=1.0 / D, scalar2=EPS,
                                                         op0=ALU.mult, op1=ALU.add, reads=[sskey], writes=[sskey])
            P.op("act", "activation", out=ss[:, 2:3], in_=ss[:, 1:2], func=AF.Sqrt, reads=[sskey], writes=[sskey])
            P.op("dve", "reciprocal", out=ss[:, 3:4], in_=ss[:, 2:3], reads=[sskey], writes=[sskey])
            P.op("dve", "scalar_tensor_tensor", out=mo, in0=mo, scalar=ss[:, 3:4], in1=g_post,
                                                                       op0=ALU.mult, op1=ALU.mult,
                 reads=[mokey, sskey, "g_post"], writes=[mokey])
            P.op("pool", "tensor_tensor", out=mo, in0=mo, in1=xt, op=ALU.add,
                 reads=[mokey, xkey], writes=[mokey])
            r0 = t0 + j * 128
            P.dma("sp", "ost", out=out_d[r0:r0 + 128, :], in_=mo,
                  reads=[mokey], writes=["out"])
        if g + 1 < NG3:
            load_h1(g + 1)

    P.barrier()
    P.emit()
    return nc


def _fm(v):
    return np.ascontiguousarray(np.asarray(v, np.float32).reshape(8, 128).T)


def _rope_tables(pos):
    inv_freq = (np.float32(500000.0) ** (-(np.arange(0, 32, 2, dtype=np.float32)) / np.float32(32))).astype(np.float32)
    ang = (pos.astype(np.float32)[:, None] * inv_freq[None, :]).astype(np.float32)
    cos = np.cos(ang.astype(np.float64)).astype(np.float32).T
    sin = np.sin(ang.astype(np.float64)).astype(np.float32).T
    c32 = np.concatenate([cos, cos], axis=0)
    s32 = np.concatenate([-sin, sin], axis=0)
    return np.ascontiguousarray(np.stack([c32, s32], axis=0))


_NC_CACHE = {}
DBG = {}


def kernel(x, attn_pre_norm, attn_post_norm, w_in, conv_w, conv_b, w_rg_a, b_rg_a, w_rg_i, b_rg_i, lru_lambda,
           w_branch_attn, w_branch_lru, w_out, mlp_pre_norm, mlp_post_norm, w_mlp_up, w_mlp_down):
    x = np.asarray(x, np.float32)
    f = lambda a: np.ascontiguousarray(np.asarray(a, np.float32))
    vec_cols = [_fm(conv_w[0][j]) for j in range(4)] + [_fm(conv_b[0]), _fm(b_rg_a[0]), _fm(b_rg_i[0]), _fm(lru_lambda[0])]
    vecs = np.ascontiguousarray(np.concatenate(vec_cols, axis=1))
    grep = np.ascontiguousarray(np.broadcast_to(np.concatenate(
        [f(attn_pre_norm[0]), f(attn_post_norm[0]), f(mlp_pre_norm[0]), f(mlp_post_norm[0])])[None, :], (128, 4096)))
    ropek = _rope_tables(np.arange(S))
    shared = {
        "w_in": f(w_in[0]), "w_rga": f(w_rg_a[0]), "w_rgi": f(w_rg_i[0]), "w_ba": f(w_branch_attn[0]),
        "w_bl": f(w_branch_lru[0]), "w_out": f(w_out[0]), "w_up": f(w_mlp_up[0]), "w_down": f(w_mlp_down[0]),
        "vecs": vecs, "grep": grep, "ropek": ropek,
    }
    tri = (np.arange(256)[:, None] <= np.arange(256)[None, :]).astype(np.float32)
    in_maps = []
    own_rows = []
    for c in range(8):
        b, p = c // 2, c % 2
        blocks = np.arange(16) * 2 + p
        rows = (blocks[:, None] * 256 + np.arange(256)[None, :]).reshape(-1)
        own_rows.append((b, rows))
        gm = np.zeros((16, 32), np.float32)
        for i in range(16):
            gb = 2 * i + p
            gm[i, gb] = 1e30
            gm[i, gb + 1:] = -1e30
        gmask = np.ascontiguousarray(np.broadcast_to(gm.reshape(1, 512), (128, 512)))
        ma = tri if p == 0 else np.ones((256, 256), np.float32)
        mb = np.zeros((256, 256), np.float32) if p == 0 else tri
        cm = np.stack([ma.reshape(2, 128, 256).transpose(1, 0, 2), mb.reshape(2, 128, 256).transpose(1, 0, 2)], axis=1)
        cmask = np.ascontiguousarray(cm.reshape(128, 1024))
        psel = np.ascontiguousarray(np.broadcast_to(np.array([1.0 - p, float(p)], np.float32)[None, :], (128, 2)))
        m = dict(shared)
        m.update({"xa": np.ascontiguousarray(x[b]), "xo": np.ascontiguousarray(x[b][rows]),
                  "ropeq": _rope_tables(rows), "gmask": gmask, "cmask": cmask, "psel": psel})
        in_maps.append(m)
    if _NC_CACHE.get("prep_only"):
        return in_maps, own_rows
    if "nc" not in _NC_CACHE:
        _NC_CACHE["nc"] = build_program()
    res = run_bass_kernel_spmd(_NC_CACHE["nc"], in_maps, core_ids=list(range(8)))
    out = np.empty((4, S, D), np.float32)
    for c in range(8):
        b, rows = own_rows[c]
        out[b, rows] = res.results[c]["out"]
    return out
```

```python
import numpy as np
import concourse.bass as bass
import concourse.mybir as mybir
from concourse.bass_utils import run_bass_kernel_spmd

F32 = mybir.dt.float32
BF16 = mybir.dt.bfloat16
AF = mybir.ActivationFunctionType
ALU = mybir.AluOpType
AX = mybir.AxisListType

SAME_ENGINE_SYNC = True
D = 1024
S = 8192
SO = 4096
NH = 8
NBLK = 32
ARENA_WORDS = 52000
SCALE = 128 ** -0.5
EPS = 1e-6


class Prog:
    ENG = ("pe", "act", "dve", "pool", "sp")

    def __init__(self, nc):
        self.nc = nc
        self.streams = {k: [] for k in self.ENG}
        self.esem = {k: nc.alloc_semaphore(name="es_" + k) for k in self.ENG}
        self.ecount = {k: 0 for k in self.ENG}
        self.obs = {k: {} for k in self.ENG}
        self.res = {}
        self.dsem = {}
        self.last_dma = {}

    def _deps(self, reads, writes):
        toks = []
        for r in reads:
            st = self.res.get(r)
            if st is not None and st[0] is not None:
                toks.append(st[0])
        for w in writes:
            st = self.res.get(w)
            if st is not None:
                if st[0] is not None:
                    toks.append(st[0])
                toks.extend(st[1])
        return toks

    def _record(self, tok, reads, writes):
        for r in reads:
            st = self.res.setdefault(r, [None, []])
            st[1].append(tok)
        for w in writes:
            self.res[w] = [tok, []]

    def _waits(self, eng, toks):
        obs = self.obs[eng]
        need = {}
        for (sem, val, src, snap) in toks:
            if src == eng and (eng == "pe" or not SAME_ENGINE_SYNC):
                continue
            if obs.get(id(sem), 0) >= val:
                continue
            cur = need.get(id(sem))
            if cur is None or cur[1] < val:
                need[id(sem)] = (sem, val, snap)
        out = []
        for k, (sem, val, snap) in need.items():
            if obs.get(k, 0) >= val:
                continue
            out.append((sem, val))
            obs[k] = val
            for kk, vv in snap.items():
                if obs.get(kk, 0) < vv:
                    obs[kk] = vv
        return out

    def op(self, eng, name, *args, reads=(), writes=(), **kw):
        fn = (name, args, kw)
        psr = [r for r in reads if isinstance(r, tuple) and r[0] == "ps"]
        if psr:
            reads = [r for r in reads if not (isinstance(r, tuple) and r[0] == "ps")]
            writes = list(writes) + [r for r in psr if r not in writes]
        toks = self._deps(reads, writes)
        waits = self._waits(eng, toks)
        self.ecount[eng] += 1
        sem = self.esem[eng]
        tok = (sem, self.ecount[eng], eng, dict(self.obs[eng]))
        self.streams[eng].append((waits, fn, sem, 1))
        self._record(tok, reads, writes)

    def dma(self, queue, semkey, reads=(), writes=(), **kw):
        fn = ("dma_start", (), kw)
        if semkey not in self.dsem:
            self.dsem[semkey] = [self.nc.alloc_semaphore(name="ds_%d" % len(self.dsem)), 0, None]
        ent = self.dsem[semkey]
        toks = self._deps(reads, writes)
        if ent[2] is not None:
            toks.append(ent[2])
        waits = self._waits(queue, toks)
        ent[1] += 16
        tok = (ent[0], ent[1], "dma", dict(self.obs[queue]))
        ent[2] = tok
        self.last_dma[semkey] = tok
        self.streams[queue].append((waits, fn, ent[0], 16))
        self._record(tok, reads, writes)

    def barrier(self):
        toks = [(self.esem[k], self.ecount[k]) for k in self.ENG if self.ecount[k] > 0]
        toks += [(t[0], t[1]) for t in self.last_dma.values()]
        for e in self.ENG:
            obs = self.obs[e]
            waits = []
            for (sem, val) in toks:
                if obs.get(id(sem), 0) < val:
                    waits.append((sem, val))
                    obs[id(sem)] = val
            self.streams[e].append((waits, None, None, 0))
        self.res = {}

    def emit(self):
        nc = self.nc
        streams = self.streams

        def run(e, lst):
            for (waits, fn, sem, inc) in lst:
                for (s, v) in waits:
                    e.wait_ge(s, v)
                if fn is not None:
                    getattr(e, fn[0])(*fn[1], **fn[2]).then_inc(sem, inc)

        with nc.Block() as block:
            @block.tensor
            def _(e):
                run(e, streams["pe"])

            @block.scalar
            def _(e):
                run(e, streams["act"])

            @block.vector
            def _(e):
                run(e, streams["dve"])

            @block.gpsimd
            def _(e):
                run(e, streams["pool"])

            @block.sync
            def _(e):
                run(e, streams["sp"])


class Arena:
    def __init__(self, nc, nwords):
        self.t = nc.alloc_sbuf_tensor("arena", [128, nwords], F32)
        self.n = nwords
        self.top = 0

    def mark(self):
        return self.top

    def release(self, m):
        self.top = m

    def alloc(self, nelem, dtype=F32):
        words = nelem if dtype == F32 else (nelem + 1) // 2
        words = (words + 7) // 8 * 8
        a = self.top
        self.top += words
        assert self.top <= self.n, ("SBUF arena overflow", self.top, self.n)
        ap = self.t[:, a:a + words]
        if dtype != F32:
            ap = ap.bitcast(dtype)
        return ap[:, 0:nelem]


class Rot:
    def __init__(self, items):
        self.items = items
        self.i = 0

    def next(self):
        it = self.items[self.i % len(self.items)]
        self.i += 1
        return it


def v3(ap, a):
    return ap.rearrange("p (a b) -> p a b", a=a)


def build_program(debug=False, stop=None):
    nc = bass.Bass("TRN2", target_bir_lowering=False)

    def din(name, shape, dt=F32):
        return nc.dram_tensor(name, list(shape), dt, kind="ExternalInput").ap()

    xa = din("xa", [S, D])
    xo = din("xo", [SO, D])
    w_in = din("w_in", [D, 7168])
    w_rga = din("w_rga", [4, 256, 256])
    w_rgi = din("w_rgi", [4, 256, 256])
    w_ba = din("w_ba", [D, D])
    w_bl = din("w_bl", [D, D])
    w_out = din("w_out", [D, D])
    w_up = din("w_up", [D, 4096])
    w_down = din("w_down", [4096, D])
    vecs = din("vecs", [128, 64])
    grep = din("grep", [128, 4 * D])
    ropek = din("ropek", [2, 32, S])
    ropeq = din("ropeq", [2, 32, SO])
    gmask_d = din("gmask", [128, 16 * 32])
    cmask_d = din("cmask", [128, 2 * 2 * 256])
    psel_d = din("psel", [128, 2])
    out_d = nc.dram_tensor("out", [SO, D], F32, kind="ExternalOutput").ap()

    def dscr(name, shape, dt):
        return nc.dram_tensor(name, list(shape), dt, kind="ExternalOutput" if debug else "Internal").ap()

    Ks = dscr("Ks", [NH, 128, S], BF16)
    Vs = dscr("Vs", [NH, 128, 64 * 130], BF16)
    Qs = dscr("Qs", [NH, 128, SO], BF16)
    Hs = dscr("Hs", [8, 128, SO], BF16)
    YL = dscr("YL", [8, 128, SO], BF16)
    H1 = dscr("H1", [SO, D], F32)

    P = Prog(nc)
    A = Arena(nc, ARENA_WORDS)
    psb = [nc.alloc_psum_tensor("psb%d" % i, [128, 512], F32) for i in range(8)]

    def ps(i):
        return psb[i][:, :], ("ps", i)

    identf = A.alloc(128)
    ident = A.alloc(128, BF16)
    vec = A.alloc(64)
    c1 = A.alloc(8)
    c2 = A.alloc(8)
    tmp8 = [A.alloc(8) for _ in range(4)]
    psel = A.alloc(2)
    ksum = A.alloc(NH * NBLK)
    ksum_hi = A.alloc(NH * NBLK, BF16)
    ksum_lo = A.alloc(NH * NBLK, BF16)
    carryx = A.alloc(8 * 3)
    carryh = A.alloc(8)
    pmat = A.alloc(128, BF16)
    pmatf = A.alloc(128)

    def vcol(base, c):
        return vec[:, base + c:base + c + 1]

    P.dma("sp", "c_vec", out=vec, in_=vecs, writes=["vec"])
    P.dma("sp", "c_psel", out=psel, in_=psel_d, writes=["psel"])
    P.op("pool", "memset", identf, 0.0, writes=["identf"])
    P.op("pool", "affine_select", out=identf, in_=identf, pattern=[[-1, 128]], compare_op=ALU.not_equal,
                                           fill=1.0, base=0, channel_multiplier=1,
         reads=["identf"], writes=["identf"])
    P.op("dve", "tensor_copy", out=ident, in_=identf, reads=["identf"], writes=["ident"])
    P.op("pool", "memset", pmatf, 0.0, writes=["pmatf"])
    P.op("pool", "affine_select", out=pmatf[:, 0:16], in_=pmatf[:, 0:16], pattern=[[-1, 16]],
                                           compare_op=ALU.not_equal, fill=1.0, base=-16, channel_multiplier=1,
         reads=["pmatf"], writes=["pmatf"])
    P.op("pool", "affine_select", out=pmatf[:, 16:32], in_=pmatf[:, 16:32], pattern=[[-1, 16]],
                                           compare_op=ALU.not_equal, fill=1.0, base=0, channel_multiplier=1,
         reads=["pmatf"], writes=["pmatf"])
    P.op("dve", "tensor_copy", out=pmat, in_=pmatf, reads=["pmatf"], writes=["pmat"])
    P.op("pool", "memset", ksum, 0.0, writes=["ksum"])
    P.op("pool", "memset", carryx, 0.0, writes=["carryx"])
    P.op("pool", "memset", carryh, 0.0, writes=["carryh"])
    lam = vec[:, 56:64]
    t_abs, t_e, t_l, t_r = tmp8
    P.op("dve", "tensor_scalar", out=t_r, in0=lam, scalar1=-1.0, scalar2=None, op0=ALU.mult,
         reads=["vec"], writes=["t_r"])
    P.op("dve", "tensor_tensor", out=t_abs, in0=lam, in1=t_r, op=ALU.max,
         reads=["vec", "t_r"], writes=["t_abs"])
    P.op("act", "activation", out=t_e, in_=t_abs, func=AF.Exp, scale=-1.0, reads=["t_abs"], writes=["t_e"])
    P.op("act", "activation", out=t_l, in_=t_e, func=AF.Ln, bias=1.0, reads=["t_e"], writes=["t_l"])
    P.op("dve", "tensor_scalar", out=t_r, in0=t_r, scalar1=0.0, scalar2=None, op0=ALU.max,
         reads=["t_r"], writes=["t_r"])
    P.op("dve", "tensor_tensor", out=t_r, in0=t_r, in1=t_l, op=ALU.add, reads=["t_r", "t_l"], writes=["t_r"])
    P.op("dve", "tensor_scalar", out=c1, in0=t_r, scalar1=-8.0, scalar2=None, op0=ALU.mult,
         reads=["t_r"], writes=["c1"])
    P.op("dve", "tensor_scalar", out=c2, in0=t_r, scalar1=-16.0, scalar2=None, op0=ALU.mult,
         reads=["t_r"], writes=["c2"])

    base_mark = A.mark()

    wcnt = [0]

    def load_w(dst3, src, key=None, semkey=None):
        wcnt[0] += 1
        P.dma("pool", "wl%d" % (wcnt[0] % 2), out=dst3, in_=src, writes=[key])

    def norm_tile(xt, xkey, gain_rep, gkey, xs, xskey, junk, ss_rot, act_xs=False):
        ss, sskey = ss_rot.next()
        P.op("act", "activation", out=junk, in_=xt, func=AF.Square, accum_out=ss[:, 0:1],
             reads=[xkey], writes=["junk", sskey])
        P.op("dve", "tensor_scalar", out=ss[:, 1:2], in0=ss[:, 0:1], scalar1=1.0 / D, scalar2=EPS,
                                              op0=ALU.mult, op1=ALU.add, reads=[sskey], writes=[sskey])
        P.op("act", "activation", out=ss[:, 2:3], in_=ss[:, 1:2], func=AF.Sqrt, reads=[sskey], writes=[sskey])
        P.op("dve", "reciprocal", out=ss[:, 3:4], in_=ss[:, 2:3], reads=[sskey], writes=[sskey])
        P.op("dve", "scalar_tensor_tensor", out=xs, in0=xt, scalar=ss[:, 3:4], in1=gain_rep,
                                                     op0=ALU.mult, op1=ALU.mult,
             reads=[xkey, sskey, gkey], writes=[xskey])
        return ss, sskey

    def transpose_tile(xs, xskey, psT, pskey, uT3, ukey, col0, evac_eng):
        pT = psT.bitcast(BF16)
        for c in range(8):
            P.op("pe", "transpose", out=pT[:, c * 128:(c + 1) * 128], in_=xs[:, c * 128:(c + 1) * 128],
                                                  identity=ident, reads=[xskey, "ident"], writes=[pskey])
        src = v3(pT, 8)
        dst = uT3[:, :, col0:col0 + 128]
        if evac_eng == "act":
            P.op("act", "activation", out=dst, in_=src, func=AF.Copy, reads=[pskey], writes=[ukey])
        else:
            P.op("dve", "tensor_copy", out=dst, in_=src, reads=[pskey], writes=[ukey])

    def rope_evac(pk, pkkey, n, C32, S32, rkey, kT, kTkey, t1, t2, tkey, swp, swpkey, evac_eng="act"):
        if evac_eng == "act":
            P.op("act", "activation", out=kT, in_=pk, func=AF.Copy, reads=[pkkey], writes=[kTkey])
        else:
            P.op("dve", "tensor_copy", out=kT, in_=pk, reads=[pkkey], writes=[kTkey])
        if not DBG.get('Krope', 1):
            return
        if DBG.get('Kt1', 1):
            P.op("dve", "tensor_tensor", out=t1[0:32, :], in0=pk[0:32, :], in1=(C32 if DBG.get('Kc32', 1) else t2[0:32, :]), op=ALU.mult,
                 reads=[pkkey, rkey] + ([kTkey] if DBG.get('Kser', 0) else []), writes=[tkey + "1"])
        if DBG.get('Ksw', 1):
            P.op("pe", "matmul", swp[:, 0:n], lhsT=pmat, rhs=kT, start=True, stop=True,
                 reads=[kTkey, "pmat"], writes=[swpkey])
        if not DBG.get('Krope2', 1):
            return
        P.op("dve", "tensor_tensor", out=t2[0:32, :], in0=swp[0:32, 0:n], in1=S32, op=ALU.mult,
             reads=[swpkey, rkey], writes=[tkey + "2"])
        P.op("pool", "tensor_tensor", out=kT[0:32, :], in0=t1[0:32, :], in1=t2[0:32, :], op=ALU.add,
             reads=[tkey + "1", tkey + "2"], writes=[kTkey])

    NG = S // 512
    wkvx = A.alloc(8 * 3072, BF16)
    wkvx3 = v3(wkvx, 8)
    wrg = A.alloc(2 * 8 * 256, BF16)
    wrg4 = wrg.rearrange("p (a c j) -> p a c j", a=2, c=8)
    g_pre = A.alloc(D)
    xts = [(A.alloc(D), ("xt", j)) for j in range(4)]
    xs_rot = Rot([(A.alloc(D, BF16), ("xs", i)) for i in range(2)])
    junk = A.alloc(D, BF16)
    ss_rot = Rot([(A.alloc(4), ("ss", i)) for i in range(4)])
    uT_rot = Rot([(A.alloc(8 * 512, BF16), ("uT", i)) for i in range(2)])
    kT_rot = Rot([(A.alloc(512, BF16), ("kT", i)) for i in range(3)])
    t_rot = Rot([((A.alloc(512), A.alloc(512)), "tr%d" % i) for i in range(2)])
    rope_rot = Rot([(A.alloc(2 * 512), ("rope", i)) for i in range(2)])
    vst = A.alloc(8 * 4 * 130, BF16)
    vst4 = vst.rearrange("p (h j e) -> p h j e", h=8, j=4)
    xl_rot = Rot([(A.alloc(515), ("xl", i)) for i in range(2)])
    xc_all = A.alloc(8 * 512)
    xc3 = v3(xc_all, 8)
    xcb = A.alloc(8 * 512, BF16)
    xcb3 = v3(xcb, 8)
    lw_rot = Rot([(tuple(A.alloc(512) for _ in range(5)), "lw%d" % i) for i in range(2)])
    hsel_rot = Rot([(A.alloc(8 * 256, BF16), ("hsel", i)) for i in range(2)])
    hsel_tmp = A.alloc(256)

    for s_ in range(3):
        load_w(wkvx3[:, :, s_ * 1024:(s_ + 1) * 1024],
               w_in[:, 1024 + s_ * 1024:2048 + s_ * 1024].rearrange("(c p) n -> p c n", p=128),
               key=("wkvx", s_), semkey="w%d" % s_)
    load_w(wrg4[:, 0], w_rga.rearrange("g (k p) j -> p (g k) j", p=128), key="wrg_a", semkey="w3")
    load_w(wrg4[:, 1], w_rgi.rearrange("g (k p) j -> p (g k) j", p=128), key="wrg_i", semkey="w4")
    P.dma("sp", "c_gain", out=g_pre, in_=grep[:, 0:D], writes=["g_pre"])
    P.op("pool", "memset", vst4[:, :, :, 128:130], 1.0, writes=["vst"])

    def load_x_1a(g):
        for j in range(4):
            xt, xkey = xts[j]
            r0 = g * 512 + j * 128
            P.dma("sp", "x%d" % j, out=xt, in_=xa[r0:r0 + 128, :], writes=[xkey])

    load_x_1a(0)
    mm_banks = Rot([4, 5, 6, 7])

    xs4 = [(A.alloc(D, BF16), ("xs4", i)) for i in range(4)]

    def norm_part_1a(g):
        for j in range(4):
            xt, xkey = xts[j]
            xs, xskey = xs4[j]
            norm_tile(xt, xkey, g_pre, "g_pre", xs, xskey, junk, ss_rot)
        if g + 1 < NG:
            load_x_1a(g + 1)

    def tr_part_1a(g):
        uT, ukey = uT_rot.next()
        uT3 = v3(uT, 8)
        for j in range(4):
            xs, xskey = xs4[j]
            psT, pskey = ps(j % 2)
            transpose_tile(xs, xskey, psT, pskey, uT3, ukey, j * 128, "act")
        return uT3, ukey

    NG1 = DBG.get('ng', NG)
    norm_part_1a(0)
    cur = tr_part_1a(0)
    for g in range(NG1):
        t0 = g * 512
        uT3, ukey = cur
        rp, rpkey = rope_rot.next()
        rp3 = v3(rp, 2)
        P.dma("sp", "rope%d" % (g % 2),
              out=rp3[0:32, :, :], in_=ropek[:, :, t0:t0 + 512].rearrange("a r t -> r a t"), writes=[rpkey])
        C32, S32 = rp3[0:32, 0, :], rp3[0:32, 1, :]
        for c in range(8):
            b = mm_banks.next()
            px, pxkey = ps(b)
            for k in range(8):
                P.op("pe", "matmul",
                     px, lhsT=wkvx3[:, k, 2048 + c * 128:2048 + (c + 1) * 128], rhs=uT3[:, k, :],
                     start=(k == 0), stop=(k == 7), reads=[ukey, ("wkvx", 2)], writes=[pxkey])
            xl, xlkey = xl_rot.next()
            P.op("pool", "tensor_copy", out=xl[:, 0:3], in_=carryx[:, 3 * c:3 * c + 3],
                 reads=["carryx"], writes=[xlkey])
            P.op("act", "activation", out=xl[:, 3:515], in_=px, func=AF.Copy,
                 reads=[pxkey], writes=[xlkey])
            P.op("pool", "tensor_copy", out=carryx[:, 3 * c:3 * c + 3], in_=xl[:, 512:515],
                 reads=[xlkey], writes=["carryx"])
            xcc = xc3[:, c, :]
            P.op("dve", "tensor_scalar",
                 out=xcc, in0=xl[:, 0:512], scalar1=vcol(0, c), scalar2=vcol(32, c), op0=ALU.mult, op1=ALU.add,
                 reads=[xlkey, "vec"], writes=[("xc", c)])
            for jj in range(1, 4):
                P.op("dve", "scalar_tensor_tensor",
                     out=xcc, in0=xl[:, jj:jj + 512], scalar=vcol(8 * jj, c), in1=xcc, op0=ALU.mult, op1=ALU.add,
                     reads=[xlkey, "vec", ("xc", c)], writes=[("xc", c)])
            P.op("pool", "tensor_copy", out=xcb3[:, c, :], in_=xcc,
                 reads=[("xc", c)], writes=[("xcb", c)])

        if g + 1 < NG1:
            norm_part_1a(g + 1)

        def k_part2(h, kT, kTkey, t1, t2, tkey, swp, swpkey):
            P.op("pe", "matmul", swp[:, 0:512], lhsT=pmat, rhs=kT, start=True, stop=True,
                 reads=[kTkey, "pmat"], writes=[swpkey])
            P.op("dve", "tensor_tensor", out=t2[0:32, :], in0=swp[0:32, 0:512], in1=S32, op=ALU.mult,
                 reads=[swpkey, rpkey], writes=[tkey + "2"])
            P.op("pool", "tensor_tensor", out=kT[0:32, :], in0=t1[0:32, :], in1=t2[0:32, :], op=ALU.add,
                 reads=[tkey + "1", tkey + "2"], writes=[kTkey])
            P.op("dve", "tensor_reduce",
                 out=ksum[:, h * 32 + 2 * g:h * 32 + 2 * g + 2], in_=v3(kT, 2), axis=AX.X, op=ALU.add,
                 reads=[kTkey], writes=["ksum"])
            P.dma("sp", "kst%d" % ((g * NH + h) % 3),
                  out=Ks[h, :, t0:t0 + 512], in_=kT, reads=[kTkey], writes=[("Ks", h)])

        pend = None
        for h in range(NH):
            b = mm_banks.next()
            pk, pkkey = ps(b)
            for c in range(8):
                P.op("pe", "matmul", pk, lhsT=wkvx3[:, c, h * 128:(h + 1) * 128], rhs=uT3[:, c, :],
                     start=(c == 0), stop=(c == 7), reads=[ukey, ("wkvx", 0)], writes=[pkkey])
            kT, kTkey = kT_rot.next()
            (t1, t2), tkey = t_rot.next()
            swp, swpkey = ps(2 + (h % 2))
            P.op("act", "activation", out=kT, in_=pk, func=AF.Copy, reads=[pkkey], writes=[kTkey])
            P.op("dve", "tensor_tensor", out=t1[0:32, :], in0=pk[0:32, :], in1=C32, op=ALU.mult,
                 reads=[pkkey, rpkey], writes=[tkey + "1"])
            if pend is not None:
                k_part2(*pend)
            pend = (h, kT, kTkey, t1, t2, tkey, swp, swpkey)
        k_part2(*pend)

        for j in range(4):
            for hh in range(2):
                b = mm_banks.next()
                pv, pvkey = ps(b)
                for c in range(8):
                    P.op("pe", "matmul",
                         pv, lhsT=uT3[:, c, j * 128:(j + 1) * 128], rhs=wkvx3[:, c, 1024 + hh * 512:1024 + (hh + 1) * 512],
                         start=(c == 0), stop=(c == 7), reads=[ukey, ("wkvx", 1)], writes=[pvkey])
                P.op("act", "activation",
                     out=vst4[:, hh * 4:(hh + 1) * 4, j, 0:128], in_=v3(pv, 4), func=AF.Copy,
                     reads=[pvkey], writes=["vst"])
        P.dma("sp", "vst",
              out=Vs.rearrange("h p (j e) -> p h j e", e=130)[:, :, 4 * g:4 * g + 4, :], in_=vst4,
              reads=["vst"], writes=["Vs"])

        if g + 1 < NG1:
            cur = tr_part_1a(g + 1)

        hsel, hselkey = hsel_rot.next()
        hsel3 = v3(hsel, 8)
        for op_ in range(0, 8, 2):
            pair = []
            for oc in (op_, op_ + 1):
                gi, jc = oc // 2, oc % 2
                (r_, i_, a_, m_, h_), lwkey = lw_rot.next()
                br = mm_banks.next()
                pr, prkey = ps(br)
                bi = mm_banks.next()
                pi, pikey = ps(bi)
                for which, pp, ppkey in ((0, pr, prkey), (1, pi, pikey)):
                    for kc in range(2):
                        P.op("pe", "matmul",
                             pp, lhsT=wrg4[:, which, gi * 2 + kc, jc * 128:(jc + 1) * 128], rhs=xcb3[:, gi * 2 + kc, :],
                             start=(kc == 0), stop=(kc == 1),
                             reads=[("xcb", gi * 2), ("xcb", gi * 2 + 1), "wrg_a", "wrg_i"], writes=[ppkey])
                pair.append((oc, r_, i_, a_, m_, h_, lwkey, pr, prkey, pi, pikey))
            for (oc, r_, i_, a_, m_, h_, lwkey, pr, prkey, pi, pikey) in pair:
                P.op("act", "activation", out=r_, in_=pr, func=AF.Sigmoid, bias=vcol(40, oc),
                     reads=[prkey, "vec"], writes=[lwkey + "r"])
                P.op("act", "activation", out=i_, in_=pi, func=AF.Sigmoid, bias=vcol(48, oc),
                     reads=[pikey, "vec"], writes=[lwkey + "i"])
            for (oc, r_, i_, a_, m_, h_, lwkey, pr, prkey, pi, pikey) in pair:
                P.op("act", "activation", out=a_, in_=r_, func=AF.Exp, scale=c1[:, oc:oc + 1],
                     reads=[lwkey + "r", "c1"], writes=[lwkey + "a"])
                P.op("act", "activation", out=m_, in_=r_, func=AF.Exp, scale=c2[:, oc:oc + 1],
                     reads=[lwkey + "r", "c2"], writes=[lwkey + "m"])
            for (oc, r_, i_, a_, m_, h_, lwkey, pr, prkey, pi, pikey) in pair:
                P.op("act", "activation", out=m_, in_=m_, func=AF.Sqrt, scale=-1.0, bias=1.0,
                     reads=[lwkey + "m"], writes=[lwkey + "m"])
            for (oc, r_, i_, a_, m_, h_, lwkey, pr, prkey, pi, pikey) in pair:
                P.op("pool", "tensor_tensor", out=i_, in0=i_, in1=xc3[:, oc, :], op=ALU.mult,
                     reads=[lwkey + "i", ("xc", oc)], writes=[lwkey + "i"])
                P.op("pool", "tensor_tensor", out=i_, in0=i_, in1=m_, op=ALU.mult,
                     reads=[lwkey + "i", lwkey + "m"], writes=[lwkey + "i"])
                P.op("dve", "tensor_tensor_scan",
                     out=h_, data0=a_, data1=i_, initial=carryh[:, oc:oc + 1], op0=ALU.mult, op1=ALU.add,
                     reads=[lwkey + "a", lwkey + "i", "carryh"], writes=[lwkey + "h"])
                P.op("dve", "tensor_copy", out=carryh[:, oc:oc + 1], in_=h_[:, 511:512],
                     reads=[lwkey + "h"], writes=["carryh"])
                P.op("pool", "tensor_scalar", out=hsel_tmp, in0=h_[:, 256:512], scalar1=psel[:, 1:2],
                     scalar2=None, op0=ALU.mult,
                     reads=[lwkey + "h", "psel"], writes=["hsel_tmp"])
                P.op("dve", "scalar_tensor_tensor",
                     out=hsel3[:, oc, :], in0=h_[:, 0:256], scalar=psel[:, 0:1], in1=hsel_tmp, op0=ALU.mult, op1=ALU.add,
                     reads=[lwkey + "h", "psel", "hsel_tmp"], writes=[hselkey])
        P.dma("sp", "hst%d" % (g % 2),
              out=Hs[:, :, g * 256:(g + 1) * 256].rearrange("c p t -> p c t"), in_=hsel3,
              reads=[hselkey], writes=["Hs"])

    P.op("dve", "tensor_copy", out=ksum_hi, in_=ksum, reads=["ksum"], writes=["ksum_hi"])
    P.op("dve", "tensor_tensor", out=ksum, in0=ksum, in1=ksum_hi, op=ALU.subtract,
         reads=["ksum", "ksum_hi"], writes=["ksum"])
    P.op("dve", "tensor_copy", out=ksum_lo, in_=ksum, reads=["ksum"], writes=["ksum_lo"])

    if stop == "1a":
        P.barrier()
        P.emit()
        return nc
    P.barrier()
    A.release(base_mark)
    NGO = SO // 512
    wq = A.alloc(8 * 1024, BF16)
    wq3 = v3(wq, 8)
    wgl = A.alloc(8 * 1024, BF16)
    wgl3 = v3(wgl, 8)
    wgt = A.alloc(8 * 1024, BF16)
    wgt3 = v3(wgt, 8)
    wbl = A.alloc(8 * 1024, BF16)
    wbl3 = v3(wbl, 8)
    g_pre = A.alloc(D)
    xts = [(A.alloc(D), ("xt", j)) for j in range(4)]
    xs_rot = Rot([(A.alloc(D, BF16), ("xs", i)) for i in range(2)])
    junk = A.alloc(D, BF16)
    ss_rot = Rot([(A.alloc(4), ("ss", i)) for i in range(4)])
    uT_rot = Rot([(A.alloc(8 * 512, BF16), ("uT", i)) for i in range(2)])
    kT_rot = Rot([(A.alloc(512, BF16), ("kT", i)) for i in range(3)])
    t_rot = Rot([((A.alloc(512), A.alloc(512)), "tr%d" % i) for i in range(2)])
    rope_rot = Rot([(A.alloc(2 * 512), ("rope", i)) for i in range(2)])
    hs_rot = Rot([(A.alloc(8 * 512, BF16), ("hs", i)) for i in range(2)])
    gw_rot = Rot([(tuple(A.alloc(512) for _ in range(3)), "gw%d" % i) for i in range(2)])
    hg_rot = Rot([(A.alloc(8 * 512, BF16), ("hg", i)) for i in range(2)])
    sg_rot = Rot([(A.alloc(512), ("sg", i)) for i in range(2)])
    yl_rot = Rot([(A.alloc(8 * 512, BF16), ("yl", i)) for i in range(2)])

    load_w(wq3, w_in[:, 0:1024].rearrange("(c p) n -> p c n", p=128), key="wq", semkey="w0")
    load_w(wgl3, w_in[:, 4096:5120].rearrange("(c p) n -> p c n", p=128), key="wgl", semkey="w1")
    load_w(wgt3, w_in[:, 6144:7168].rearrange("(c p) n -> p c n", p=128), key="wgt", semkey="w2")
    load_w(wbl3, w_bl.rearrange("(c p) n -> p c n", p=128), key="wbl", semkey="w3")
    P.dma("sp", "c_gain", out=g_pre, in_=grep[:, 0:D], writes=["g_pre"])

    def load_x_own(g, ntile):
        for j in range(ntile):
            xt, xkey = xts[j]
            r0 = g * ntile * 128 + j * 128
            P.dma("sp", "x%d" % j, out=xt, in_=xo[r0:r0 + 128, :], writes=[xkey])

    load_x_own(0, 4)
    for g in range(NGO):
        t0 = g * 512
        rp, rpkey = rope_rot.next()
        rp3 = v3(rp, 2)
        P.dma("sp", "rope%d" % (g % 2),
            out=rp3[0:32, :, :], in_=ropeq[:, :, t0:t0 + 512].rearrange("a r t -> r a t"), writes=[rpkey])
        hs, hskey = hs_rot.next()
        hs3 = v3(hs, 8)
        P.dma("sp", "hld%d" % (g % 2),
            out=hs3, in_=Hs[:, :, t0:t0 + 512].rearrange("c p t -> p c t"), reads=["Hs"], writes=[hskey])
        uT, ukey = uT_rot.next()
        uT3 = v3(uT, 8)
        for j in range(4):
            xt, xkey = xts[j]
            xs, xskey = xs_rot.next()
            norm_tile(xt, xkey, g_pre, "g_pre", xs, xskey, junk, ss_rot)
            psT, pskey = ps(j % 2)
            transpose_tile(xs, xskey, psT, pskey, uT3, ukey, j * 128, "act")
        if g + 1 < NGO:
            load_x_own(g + 1, 4)
        for h in range(NH):
            b = mm_banks.next()
            pk, pkkey = ps(b)
            for c in range(8):
                P.op("pe", "matmul", pk, lhsT=wq3[:, c, h * 128:(h + 1) * 128], rhs=uT3[:, c, :],
                                                               start=(c == 0), stop=(c == 7),
                     reads=[ukey, "wq"], writes=[pkkey])
            kT, kTkey = kT_rot.next()
            (t1, t2), tkey = t_rot.next()
            swp, swpkey = ps(2 + (h % 2))
            rope_evac(pk, pkkey, 512, rp3[0:32, 0, :], rp3[0:32, 1, :], rpkey, kT, kTkey, t1, t2, tkey, swp, swpkey)
            P.dma("sp", "kst%d" % ((g * NH + h) % 3),
                out=Qs[h, :, t0:t0 + 512], in_=kT, reads=[kTkey], writes=[("Qs", h)])
        hg, hgkey = hg_rot.next()
        hg3 = v3(hg, 8)
        for oc in range(8):
            b = mm_banks.next()
            pg, pgkey = ps(b)
            for c in range(8):
                P.op("pe", "matmul", pg, lhsT=wgl3[:, c, oc * 128:(oc + 1) * 128], rhs=uT3[:, c, :],
                                                                 start=(c == 0), stop=(c == 7),
                     reads=[ukey, "wgl"], writes=[pgkey])
            (gx, gq, gs), gwkey = gw_rot.next()
            P.op("act", "activation", out=gx, in_=pg, func=AF.Copy, reads=[pgkey], writes=[gwkey + "x"])
            P.op("act", "activation", out=gq, in_=pg, func=AF.Square, reads=[pgkey], writes=[gwkey + "q"])
            P.op("dve", "tensor_scalar", out=gq, in0=gq, scalar1=0.044715, scalar2=1.0, op0=ALU.mult, op1=ALU.add,
                 reads=[gwkey + "q"], writes=[gwkey + "q"])
            P.op("pool", "tensor_tensor", out=gq, in0=gq, in1=gx, op=ALU.mult,
                 reads=[gwkey + "q", gwkey + "x"], writes=[gwkey + "q"])
            P.op("act", "activation", out=gs, in_=gq, func=AF.Sigmoid, scale=1.5957691216057308,
                 reads=[gwkey + "q"], writes=[gwkey + "s"])
            P.op("pool", "tensor_tensor", out=gs, in0=gs, in1=gx, op=ALU.mult,
                 reads=[gwkey + "s", gwkey + "x"], writes=[gwkey + "s"])
            P.op("dve", "tensor_tensor", out=hg3[:, oc, :], in0=gs, in1=hs3[:, oc, :], op=ALU.mult,
                 reads=[gwkey + "s", hskey], writes=[hgkey])
        yl, ylkey = yl_rot.next()
        yl3 = v3(yl, 8)
        for oc in range(8):
            b = mm_banks.next()
            pg, pgkey = ps(b)
            for c in range(8):
                P.op("pe", "matmul", pg, lhsT=wgt3[:, c, oc * 128:(oc + 1) * 128], rhs=uT3[:, c, :],
                                                                 start=(c == 0), stop=(c == 7),
                     reads=[ukey, "wgt"], writes=[pgkey])
            sg, sgkey = sg_rot.next()
            P.op("act", "activation", out=sg, in_=pg, func=AF.Sigmoid, reads=[pgkey], writes=[sgkey])
            b2 = mm_banks.next()
            py, pykey = ps(b2)
            for c in range(8):
                P.op("pe", "matmul", py, lhsT=wbl3[:, c, oc * 128:(oc + 1) * 128], rhs=hg3[:, c, :],
                                                                 start=(c == 0), stop=(c == 7),
                     reads=[hgkey, "wbl"], writes=[pykey])
            P.op("dve", "tensor_tensor", out=yl3[:, oc, :], in0=py, in1=sg, op=ALU.mult,
                 reads=[pykey, sgkey], writes=[ylkey])
        P.dma("sp", "yst%d" % (g % 2),
            out=YL[:, :, t0:t0 + 512].rearrange("c p t -> p c t"), in_=yl3, reads=[ylkey], writes=["YL"])

    if stop == "1b":
        P.barrier()
        P.emit()
        return nc
    P.barrier()
    A.release(base_mark)
    attnT = A.alloc(NH * SO, BF16)
    attnT3 = v3(attnT, NH)
    p2_mark = A.mark()
    kv_rot = Rot([((A.alloc(S, BF16), A.alloc(64 * 130, BF16), A.alloc(SO, BF16)), "kv%d" % i) for i in range(2)])
    gmask = A.alloc(16 * 32)
    cmaskf = A.alloc(1024)
    cmask = A.alloc(1024, BF16)
    cmask3 = v3(cmask, 2)
    pt_rot = Rot([(A.alloc(512, BF16), ("pt", i)) for i in range(3)])
    acc_rot = Rot([(A.alloc(2 * 130), ("acc", i)) for i in range(2)])
    sel_rot = Rot([(A.alloc(2 * 32), ("sel", i)) for i in range(2)])
    g1_rot = Rot([(A.alloc(32 + 8 + 1), ("g1", i)) for i in range(4)])
    on_rot = Rot([(A.alloc(2 * 128, BF16), ("on", i)) for i in range(2)])
    rinv = A.alloc(2)

    P.dma("sp", "c_gmask", out=gmask, in_=gmask_d, writes=["gmask"])
    P.dma("sp", "c_cmask", out=cmaskf, in_=cmask_d, writes=["cmaskf"])
    P.op("dve", "tensor_copy", out=cmask, in_=cmaskf, reads=["cmaskf"], writes=["cmask"])

    def load_head(h):
        (KT, VV, QT), kvkey = kv_rot.next()
        P.dma("sp", kvkey + "k", out=KT, in_=Ks[h], reads=[("Ks", h)], writes=[kvkey + "k"])
        P.dma("sp", kvkey + "v", out=VV, in_=Vs[h], reads=["Vs"], writes=[kvkey + "v"])
        P.dma("sp", kvkey + "q", out=QT, in_=Qs[h], reads=[("Qs", h)], writes=[kvkey + "q"])
        return (KT, v3(VV, 64), QT), kvkey

    s_banks = Rot([0, 1, 2])
    o_banks = Rot([3, 4])
    nxt = load_head(0)
    for h in range(NH):
        (KT, VV3, QT), kvkey = nxt
        if h + 1 < NH:
            nxt = load_head(h + 1)
        units = [(i, j) for i in range(16) for j in range(2 * i + 2)]
        state = {}

        def emit_S(u):
            i, j = units[u]
            b = s_banks.next()
            sp_, spkey = ps(b)
            for kt in range(2):
                P.op("pe", "matmul",
                    sp_[:, kt * 256:(kt + 1) * 256], lhsT=KT[:, (2 * j + kt) * 128:(2 * j + kt + 1) * 128],
                    rhs=QT[:, i * 256:(i + 1) * 256], start=True, stop=True,
                    reads=[kvkey + "k", kvkey + "q"], writes=[spkey])
            state[u] = (sp_, spkey)

        def emit_gate(i):
            pgt, pgtkey = ps(5)
            acc, acckey = acc_rot.next()
            sel, selkey = sel_rot.next()
            for qt in range(2):
                qcols = QT[:, i * 256 + qt * 128:i * 256 + (qt + 1) * 128]
                P.op("pe", "matmul", pgt[:, qt * 32:(qt + 1) * 32], lhsT=qcols,
                                                                  rhs=ksum_hi[:, h * 32:(h + 1) * 32], start=True, stop=False,
                     reads=[kvkey + "q", "ksum_hi"], writes=[pgtkey])
                P.op("pe", "matmul", pgt[:, qt * 32:(qt + 1) * 32], lhsT=qcols,
                                                                  rhs=ksum_lo[:, h * 32:(h + 1) * 32], start=False, stop=True,
                     reads=[kvkey + "q", "ksum_lo"], writes=[pgtkey])
            for qt in range(2):
                g1, g1key = g1_rot.next()
                P.op("dve", "tensor_tensor", out=g1[:, 0:32], in0=pgt[:, qt * 32:(qt + 1) * 32],
                                                                    in1=gmask[:, i * 32:(i + 1) * 32], op=ALU.add,
                     reads=[pgtkey, "gmask"], writes=[g1key])
                P.op("dve", "max", out=g1[:, 32:40], in_=g1[:, 0:32], reads=[g1key], writes=[g1key])
                P.op("dve", "tensor_scalar", out=g1[:, 40:41], in0=g1[:, 35:36], scalar1=-1e29, scalar2=None,
                                                             op0=ALU.max, reads=[g1key], writes=[g1key])
                P.op("dve", "tensor_scalar", out=sel[:, qt * 32:(qt + 1) * 32], in0=g1[:, 0:32],
                                                                             scalar1=g1[:, 40:41], scalar2=None, op0=ALU.is_ge,
                     reads=[g1key], writes=[selkey])
            return acc, acckey, sel, selkey

        blk = {}

        def emit_rest(u):
            i, j = units[u]
            sp_, spkey = state.pop(u)
            acc, acckey, sel, selkey = blk[i]
            pt, ptkey = pt_rot.next()
            P.op("act", "activation", out=pt, in_=sp_, func=AF.Exp, scale=SCALE, reads=[spkey], writes=[ptkey])
            if j >= 2 * i:
                P.op("pool", "tensor_tensor", out=pt, in0=pt, in1=cmask3[:, j - 2 * i, :], op=ALU.mult,
                     reads=[ptkey, "cmask"], writes=[ptkey])
            b = o_banks.next()
            po, pokey = ps(b)
            po3 = po[:, 0:260].rearrange("p (a b) -> p a b", a=2)
            for qt in range(2):
                for kt in range(2):
                    P.op("pe", "matmul",
                        po3[:, qt, :], lhsT=pt[:, kt * 256 + qt * 128:kt * 256 + (qt + 1) * 128], rhs=VV3[:, 2 * j + kt, :],
                        start=(kt == 0), stop=(kt == 1), reads=[ptkey, kvkey + "v"], writes=[pokey])
            acc3 = v3(acc, 2)
            for qt in range(2):
                sc = sel[:, qt * 32 + j:qt * 32 + j + 1]
                if j == 0:
                    P.op("dve", "tensor_scalar", out=acc3[:, qt, :], in0=po3[:, qt, :], scalar1=sc,
                                                                        scalar2=None, op0=ALU.mult,
                         reads=[pokey, selkey], writes=[acckey])
                else:
                    P.op("dve", "scalar_tensor_tensor", out=acc3[:, qt, :], in0=po3[:, qt, :], scalar=sc,
                                                                               in1=acc3[:, qt, :], op0=ALU.mult, op1=ALU.add,
                         reads=[pokey, selkey, acckey], writes=[acckey])

        pending = []

        def emit_norm(i):
            acc, acckey, sel, selkey = blk[i]
            acc3 = v3(acc, 2)
            on, onkey = on_rot.next()
            on3 = v3(on, 2)
            P.op("dve", "reciprocal", out=rinv, in_=acc3[:, :, 128], reads=[acckey], writes=["rinv"])
            for qt in range(2):
                P.op("dve", "tensor_scalar", out=on3[:, qt, :], in0=acc3[:, qt, 0:128], scalar1=rinv[:, qt:qt + 1],
                                                             scalar2=None, op0=ALU.mult,
                     reads=[acckey, "rinv"], writes=[onkey])
            pending.append((i, on3, onkey))

        def emit_fin():
            i, on3, onkey = pending.pop(0)
            pT_, pTkey = ps(6)
            pTb = pT_.bitcast(BF16)
            for qt in range(2):
                P.op("pe", "transpose", out=pTb[:, qt * 128:(qt + 1) * 128], in_=on3[:, qt, :], identity=ident,
                     reads=[onkey, "ident"], writes=[pTkey])
            P.op("dve", "tensor_copy", out=attnT3[:, h, i * 256:(i + 1) * 256], in_=pTb[:, 0:256],
                 reads=[pTkey], writes=["attnT"])

        U = len(units)
        blk[0] = emit_gate(0)
        emit_S(0)
        for u in range(U):
            i, j = units[u]
            if u + 1 < U:
                ni, nj = units[u + 1]
                if nj == 0:
                    blk[ni] = emit_gate(ni)
                emit_S(u + 1)
            emit_rest(u)
            if j == 1 and pending:
                emit_fin()
            if j == 2 * i + 1:
                emit_norm(i)
        while pending:
            emit_fin()

    if debug:
        ATT = nc.dram_tensor("ATT", [NH, 128, SO], BF16, kind="ExternalOutput").ap()
        P.dma("sp", "dbg_att", out=ATT.rearrange("h p t -> p h t"), in_=attnT3, reads=["attnT"], writes=["ATT"])
    if stop == "2":
        P.barrier()
        P.emit()
        return nc
    P.barrier()
    A.release(p2_mark)
    NG3 = SO // 256
    wga = A.alloc(8 * 1024, BF16)
    wga3 = v3(wga, 8)
    wba = A.alloc(8 * 1024, BF16)
    wba3 = v3(wba, 8)
    wo = A.alloc(8 * 1024, BF16)
    wo3 = v3(wo, 8)
    g_pre = A.alloc(D)
    g_post = A.alloc(D)
    xts = [(A.alloc(D), ("xt", j)) for j in range(2)]
    xs_rot = Rot([(A.alloc(D, BF16), ("xs", i)) for i in range(2)])
    junk = A.alloc(D, BF16)
    ss_rot = Rot([(A.alloc(4), ("ss", i)) for i in range(4)])
    uT_rot = Rot([(A.alloc(8 * 256, BF16), ("uT", i)) for i in range(2)])
    sg_rot = Rot([(A.alloc(256), ("sg", i)) for i in range(2)])
    yld_rot = Rot([(A.alloc(8 * 256, BF16), ("yld", i)) for i in range(2)])
    mT_rot = Rot([(A.alloc(8 * 256, BF16), ("mT", i)) for i in range(2)])
    mix_rot = Rot([(A.alloc(D), ("mix", i)) for i in range(2)])

    load_w(wga3, w_in[:, 5120:6144].rearrange("(c p) n -> p c n", p=128), key="wga", semkey="w0")
    load_w(wba3, w_ba.rearrange("(c p) n -> p c n", p=128), key="wba", semkey="w1")
    load_w(wo3, w_out.rearrange("(c p) n -> p c n", p=128), key="wo", semkey="w2")
    P.dma("sp", "c_gain", out=g_pre, in_=grep[:, 0:D], writes=["g_pre"])
    P.dma("sp", "c_gain2", out=g_post, in_=grep[:, D:2 * D], writes=["g_post"])

    load_x_own(0, 2)
    for g in range(NG3):
        t0 = g * 256
        yld, yldkey = yld_rot.next()
        yld3 = v3(yld, 8)
        P.dma("sp", "yld%d" % (g % 2),
            out=yld3, in_=YL[:, :, t0:t0 + 256].rearrange("c p t -> p c t"), reads=["YL"], writes=[yldkey])
        uT, ukey = uT_rot.next()
        uT3 = v3(uT, 8)
        mix_list = []
        for j in range(2):
            xt, xkey = xts[j]
            xs, xskey = xs_rot.next()
            norm_tile(xt, xkey, g_pre, "g_pre", xs, xskey, junk, ss_rot)
            psT, pskey = ps(j % 2)
            transpose_tile(xs, xskey, psT, pskey, uT3, ukey, j * 128, "act")
        mT, mTkey = mT_rot.next()
        mT3 = v3(mT, 8)
        for oc in range(8):
            b = mm_banks.next()
            pg, pgkey = ps(b)
            for c in range(8):
                P.op("pe", "matmul", pg[:, 0:256], lhsT=wga3[:, c, oc * 128:(oc + 1) * 128], rhs=uT3[:, c, :],
                                                                 start=(c == 0), stop=(c == 7),
                     reads=[ukey, "wga"], writes=[pgkey])
            sg, sgkey = sg_rot.next()
            P.op("act", "activation", out=sg, in_=pg[:, 0:256], func=AF.Sigmoid, reads=[pgkey], writes=[sgkey])
            b2 = mm_banks.next()
            py, pykey = ps(b2)
            for c in range(8):
                P.op("pe", "matmul", py[:, 0:256], lhsT=wba3[:, c, oc * 128:(oc + 1) * 128],
                                                                 rhs=attnT3[:, c, t0:t0 + 256], start=(c == 0), stop=(c == 7),
                     reads=["attnT", "wba"], writes=[pykey])
            P.op("dve", "tensor_tensor", out=sg, in0=py[:, 0:256], in1=sg, op=ALU.mult,
                 reads=[pykey, sgkey], writes=[sgkey])
            P.op("pool", "tensor_tensor", out=mT3[:, oc, :], in0=sg, in1=yld3[:, oc, :], op=ALU.add,
                 reads=[sgkey, yldkey], writes=[mTkey])
        for j in range(2):
            xt, xkey = xts[j]
            mix, mixkey = mix_rot.next()
            for hh in range(2):
                b = mm_banks.next()
                pm, pmkey = ps(b)
                for c in range(8):
                    P.op("pe", "matmul", pm, lhsT=mT3[:, c, j * 128:(j + 1) * 128],
                                                                          rhs=wo3[:, c, hh * 512:(hh + 1) * 512],
                                                                          start=(c == 0), stop=(c == 7),
                         reads=[mTkey, "wo"], writes=[pmkey])
                P.op("act", "activation", out=mix[:, hh * 512:(hh + 1) * 512], in_=pm, func=AF.Copy,
                     reads=[pmkey], writes=[mixkey])
            ss, sskey = ss_rot.next()
            P.op("act", "activation", out=junk, in_=mix, func=AF.Square, accum_out=ss[:, 0:1],
                 reads=[mixkey], writes=["junk", sskey])
            P.op("dve", "tensor_scalar", out=ss[:, 1:2], in0=ss[:, 0:1], scalar1=1.0 / D, scalar2=EPS,
                                                         op0=ALU.mult, op1=ALU.add, reads=[sskey], writes=[sskey])
            P.op("act", "activation", out=ss[:, 2:3], in_=ss[:, 1:2], func=AF.Sqrt, reads=[sskey], writes=[sskey])
            P.op("dve", "reciprocal", out=ss[:, 3:4], in_=ss[:, 2:3], reads=[sskey], writes=[sskey])
            P.op("dve", "scalar_tensor_tensor", out=mix, in0=mix, scalar=ss[:, 3:4], in1=g_post,
                                                                         op0=ALU.mult, op1=ALU.mult,
                 reads=[mixkey, sskey, "g_post"], writes=[mixkey])
            P.op("pool", "tensor_tensor", out=mix, in0=mix, in1=xt, op=ALU.add,
                 reads=[mixkey, xkey], writes=[mixkey])
            r0 = t0 + j * 128
            P.dma("sp", "h1st%d" % j, out=H1[r0:r0 + 128, :], in_=mix,
                  reads=[mixkey], writes=["H1"])
        if g + 1 < NG3:
            load_x_own(g + 1, 2)

    if stop == "3a":
        P.barrier()
        P.emit()
        return nc
    P.barrier()
    A.release(base_mark)
    wup = A.alloc(8 * 4096, BF16)
    wup3 = v3(wup, 8)
    wdn = A.alloc(32 * 1024, BF16)
    wdn3 = v3(wdn, 32)
    g_pre = A.alloc(D)
    g_post = A.alloc(D)
    xts = [(A.alloc(D), ("xt", j)) for j in range(2)]
    xs_rot = Rot([(A.alloc(D, BF16), ("xs", i)) for i in range(1)])
    junk = A.alloc(D, BF16)
    ss_rot = Rot([(A.alloc(4), ("ss", i)) for i in range(4)])
    uT_rot = Rot([(A.alloc(8 * 256, BF16), ("uT", i)) for i in range(1)])
    aT = A.alloc(32 * 256, BF16)
    aT3 = v3(aT, 32)
    rl_rot = Rot([(A.alloc(256), ("rl", i)) for i in range(2)])
    mo_rot = Rot([(A.alloc(D), ("mo", i)) for i in range(1)])

    for s_ in range(4):
        load_w(wup3[:, :, s_ * 1024:(s_ + 1) * 1024], w_up[:, s_ * 1024:(s_ + 1) * 1024].rearrange("(c p) n -> p c n", p=128),
               key=("wup", s_), semkey="w%d" % s_)
    for s_ in range(4):
        load_w(wdn3[:, s_ * 8:(s_ + 1) * 8, :], w_down[s_ * 1024:(s_ + 1) * 1024, :].rearrange("(c p) n -> p c n", p=128),
               key=("wdn", s_), semkey="w%d" % (4 + s_) if s_ < 1 else "w%d" % s_)
    P.dma("sp", "c_gain", out=g_pre, in_=grep[:, 2 * D:3 * D], writes=["g_pre"])
    P.dma("sp", "c_gain2", out=g_post, in_=grep[:, 3 * D:4 * D], writes=["g_post"])

    def load_h1(g):
        for j in range(2):
            xt, xkey = xts[j]
            r0 = g * 256 + j * 128
            P.dma("sp", "x%d" % j, out=xt, in_=H1[r0:r0 + 128, :],
                  reads=["H1"], writes=[xkey])

    load_h1(0)
    for g in range(NG3):
        t0 = g * 256
        uT, ukey = uT_rot.next()
        uT3 = v3(uT, 8)
        for j in range(2):
            xt, xkey = xts[j]
            xs, xskey = xs_rot.next()
            norm_tile(xt, xkey, g_pre, "g_pre", xs, xskey, junk, ss_rot)
            psT, pskey = ps(j % 2)
            transpose_tile(xs, xskey, psT, pskey, uT3, ukey, j * 128, "act")
        for fc in range(32):
            b = mm_banks.next()
            pu, pukey = ps(b)
            for c in range(8):
                P.op("pe", "matmul", pu[:, 0:256], lhsT=wup3[:, c, fc * 128:(fc + 1) * 128], rhs=uT3[:, c, :],
                                                                 start=(c == 0), stop=(c == 7),
                     reads=[ukey, ("wup", fc // 8)], writes=[pukey])
            rl, rlkey = rl_rot.next()
            P.op("act", "activation", out=rl, in_=pu[:, 0:256], func=AF.Relu, reads=[pukey], writes=[rlkey])
            eng = "dve" if fc % 2 == 0 else "pool"
            P.op(eng, "tensor_tensor", out=aT3[:, fc, :], in0=rl, in1=rl, op=ALU.mult,
                 reads=[rlkey], writes=[("aT", fc)])
        for j in range(2):
            xt, xkey = xts[j]
            mo, mokey = mo_rot.next()
            for hh in range(2):
                b = mm_banks.next()
                pm, pmkey = ps(b)
                for fc in range(32):
                    P.op("pe", "matmul", pm, lhsT=aT3[:, fc, j * 128:(j + 1) * 128],
                                                                            rhs=wdn3[:, fc, hh * 512:(hh + 1) * 512],
                                                                            start=(fc == 0), stop=(fc == 31),
                         reads=[("aT", fc), ("wdn", fc // 8)], writes=[pmkey])
                P.op("act", "activation", out=mo[:, hh * 512:(hh + 1) * 512], in_=pm, func=AF.Copy,
                     reads=[pmkey], writes=[mokey])
            ss, sskey = ss_rot.next()
            P.op("act", "activation", out=junk, in_=mo, func=AF.Square, accum_out=ss[:, 0:1],
                 reads=[mokey], writes=["junk", sskey])
            P.op("dve", "tensor_scalar", out=ss[:, 1:2], in0=ss[:, 0:1], scalar1=1.0 / D, scalar2=EPS,
                                                         op0=ALU.mult, op1=ALU.add, reads=[sskey], writes=[sskey])
            P.op("act", "activation", out=ss[:, 2:3], in_=ss[:, 1:2], func=AF.Sqrt, reads=[sskey], writes=[sskey])
            P.op("dve", "reciprocal", out=ss[:, 3:4], in_=ss[:, 2:3], reads=[sskey], writes=[sskey])
            P.op("dve", "scalar_tensor_tensor", out=mo, in0=mo, scalar=ss[:, 3:4], in1=g_post,
                                                                       op0=ALU.mult, op1=ALU.mult,
                 reads=[mokey, sskey, "g_post"], writes=[mokey])
            P.op("pool", "tensor_tensor", out=mo, in0=mo, in1=xt, op=ALU.add,
                 reads=[mokey, xkey], writes=[mokey])
            r0 = t0 + j * 128
            P.dma("sp", "ost", out=out_d[r0:r0 + 128, :], in_=mo,
                  reads=[mokey], writes=["out"])
        if g + 1 < NG3:
            load_h1(g + 1)

    P.barrier()
    P.emit()
    return nc


def _fm(v):
    return np.ascontiguousarray(np.asarray(v, np.float32).reshape(8, 128).T)


def _rope_tables(pos):
    inv_freq = (np.float32(500000.0) ** (-(np.arange(0, 32, 2, dtype=np.float32)) / np.float32(32))).astype(np.float32)
    ang = (pos.astype(np.float32)[:, None] * inv_freq[None, :]).astype(np.float32)
    cos = np.cos(ang.astype(np.float64)).astype(np.float32).T
    sin = np.sin(ang.astype(np.float64)).astype(np.float32).T
    c32 = np.concatenate([cos, cos], axis=0)
    s32 = np.concatenate([-sin, sin], axis=0)
    return np.ascontiguousarray(np.stack([c32, s32], axis=0))


_NC_CACHE = {}
DBG = {}


def kernel(x, attn_pre_norm, attn_post_norm, w_in, conv_w, conv_b, w_rg_a, b_rg_a, w_rg_i, b_rg_i, lru_lambda,
           w_branch_attn, w_branch_lru, w_out, mlp_pre_norm, mlp_post_norm, w_mlp_up, w_mlp_down):
    x = np.asarray(x, np.float32)
    f = lambda a: np.ascontiguousarray(np.asarray(a, np.float32))
    vec_cols = [_fm(conv_w[0][j]) for j in range(4)] + [_fm(conv_b[0]), _fm(b_rg_a[0]), _fm(b_rg_i[0]), _fm(lru_lambda[0])]
    vecs = np.ascontiguousarray(np.concatenate(vec_cols, axis=1))
    grep = np.ascontiguousarray(np.broadcast_to(np.concatenate(
        [f(attn_pre_norm[0]), f(attn_post_norm[0]), f(mlp_pre_norm[0]), f(mlp_post_norm[0])])[None, :], (128, 4096)))
    ropek = _rope_tables(np.arange(S))
    shared = {
        "w_in": f(w_in[0]), "w_rga": f(w_rg_a[0]), "w_rgi": f(w_rg_i[0]), "w_ba": f(w_branch_attn[0]),
        "w_bl": f(w_branch_lru[0]), "w_out": f(w_out[0]), "w_up": f(w_mlp_up[0]), "w_down": f(w_mlp_down[0]),
        "vecs": vecs, "grep": grep, "ropek": ropek,
    }
    tri = (np.arange(256)[:, None] <= np.arange(256)[None, :]).astype(np.float32)
    in_maps = []
    own_rows = []
    for c in range(8):
        b, p = c // 2, c % 2
        blocks = np.arange(16) * 2 + p
        rows = (blocks[:, None] * 256 + np.arange(256)[None, :]).reshape(-1)
        own_rows.append((b, rows))
        gm = np.zeros((16, 32), np.float32)
        for i in range(16):
            gb = 2 * i + p
            gm[i, gb] = 1e30
            gm[i, gb + 1:] = -1e30
        gmask = np.ascontiguousarray(np.broadcast_to(gm.reshape(1, 512), (128, 512)))
        ma = tri if p == 0 else np.ones((256, 256), np.float32)
        mb = np.zeros((256, 256), np.float32) if p == 0 else tri
        cm = np.stack([ma.reshape(2, 128, 256).transpose(1, 0, 2), mb.reshape(2, 128, 256).transpose(1, 0, 2)], axis=1)
        cmask = np.ascontiguousarray(cm.reshape(128, 1024))
        psel = np.ascontiguousarray(np.broadcast_to(np.array([1.0 - p, float(p)], np.float32)[None, :], (128, 2)))
        m = dict(shared)
        m.update({"xa": np.ascontiguousarray(x[b]), "xo": np.ascontiguousarray(x[b][rows]),
                  "ropeq": _rope_tables(rows), "gmask": gmask, "cmask": cmask, "psel": psel})
        in_maps.append(m)
    if _NC_CACHE.get("prep_only"):
        return in_maps, own_rows
    if "nc" not in _NC_CACHE:
        _NC_CACHE["nc"] = build_program()
    res = run_bass_kernel_spmd(_NC_CACHE["nc"], in_maps, core_ids=list(range(8)))
    out = np.empty((4, S, D), np.float32)
    for c in range(8):
        b, rows = own_rows[c]
        out[b, rows] = res.results[c]["out"]
    return out
```

```python
import numpy as np
import concourse.bass as bass
import concourse.mybir as mybir
from concourse.bass_utils import run_bass_kernel_spmd

F32 = mybir.dt.float32
BF16 = mybir.dt.bfloat16
AF = mybir.ActivationFunctionType
ALU = mybir.AluOpType
AX = mybir.AxisListType

SAME_ENGINE_SYNC = True
D = 1024
S = 8192
SO = 4096
NH = 8
NBLK = 32
ARENA_WORDS = 53100
SCALE = 128 ** -0.5
EPS = 1e-6


class Prog:
    ENG = ("pe", "act", "dve", "pool", "sp")

    def __init__(self, nc):
        self.nc = nc
        self.streams = {k: [] for k in self.ENG}
        self.esem = {k: nc.alloc_semaphore(name="es_" + k) for k in self.ENG}
        self.ecount = {k: 0 for k in self.ENG}
        self.obs = {k: {} for k in self.ENG}
        self.res = {}
        self.dsem = {}
        self.last_dma = {}

    def _deps(self, reads, writes):
        toks = []
        for r in reads:
            st = self.res.get(r)
            if st is not None and st[0] is not None:
                toks.append(st[0])
        for w in writes:
            st = self.res.get(w)
            if st is not None:
                if st[0] is not None:
                    toks.append(st[0])
                toks.extend(st[1])
        return toks

    def _record(self, tok, reads, writes):
        for r in reads:
            st = self.res.setdefault(r, [None, []])
            st[1].append(tok)
        for w in writes:
            self.res[w] = [tok, []]

    def _waits(self, eng, toks):
        obs = self.obs[eng]
        need = {}
        for (sem, val, src, snap) in toks:
            if src == eng and (eng == "pe" or not SAME_ENGINE_SYNC):
                continue
            if obs.get(id(sem), 0) >= val:
                continue
            cur = need.get(id(sem))
            if cur is None or cur[1] < val:
                need[id(sem)] = (sem, val, snap)
        out = []
        for k, (sem, val, snap) in need.items():
            if obs.get(k, 0) >= val:
                continue
            out.append((sem, val))
            obs[k] = val
            for kk, vv in snap.items():
                if obs.get(kk, 0) < vv:
                    obs[kk] = vv
        return out

    def op(self, eng, name, *args, reads=(), writes=(), **kw):
        fn = (name, args, kw)
        psr = [r for r in reads if isinstance(r, tuple) and r[0] == "ps"]
        if psr:
            reads = [r for r in reads if not (isinstance(r, tuple) and r[0] == "ps")]
            writes = list(writes) + [r for r in psr if r not in writes]
        toks = self._deps(reads, writes)
        waits = self._waits(eng, toks)
        self.ecount[eng] += 1
        sem = self.esem[eng]
        tok = (sem, self.ecount[eng], eng, dict(self.obs[eng]))
        self.streams[eng].append((waits, fn, sem, 1))
        self._record(tok, reads, writes)

    def dma(self, queue, semkey, reads=(), writes=(), **kw):
        fn = ("dma_start", (), kw)
        if semkey not in self.dsem:
            self.dsem[semkey] = [self.nc.alloc_semaphore(name="ds_%d" % len(self.dsem)), 0, None]
        ent = self.dsem[semkey]
        toks = self._deps(reads, writes)
        if ent[2] is not None:
            toks.append(ent[2])
        waits = self._waits(queue, toks)
        ent[1] += 16
        tok = (ent[0], ent[1], "dma", dict(self.obs[queue]))
        ent[2] = tok
        self.last_dma[semkey] = tok
        self.streams[queue].append((waits, fn, ent[0], 16))
        self._record(tok, reads, writes)

    def barrier(self):
        toks = [(self.esem[k], self.ecount[k]) for k in self.ENG if self.ecount[k] > 0]
        toks += [(t[0], t[1]) for t in self.last_dma.values()]
        for e in self.ENG:
            obs = self.obs[e]
            waits = []
            for (sem, val) in toks:
                if obs.get(id(sem), 0) < val:
                    waits.append((sem, val))
                    obs[id(sem)] = val
            self.streams[e].append((waits, None, None, 0))
        self.res = {}

    def emit(self):
        nc = self.nc
        streams = self.streams

        def run(e, lst):
            for (waits, fn, sem, inc) in lst:
                for (s, v) in waits:
                    e.wait_ge(s, v)
                if fn is not None:
                    getattr(e, fn[0])(*fn[1], **fn[2]).then_inc(sem, inc)

        with nc.Block() as block:
            @block.tensor
            def _(e):
                run(e, streams["pe"])

            @block.scalar
            def _(e):
                run(e, streams["act"])

            @block.vector
            def _(e):
                run(e, streams["dve"])

            @block.gpsimd
            def _(e):
                run(e, streams["pool"])

            @block.sync
            def _(e):
                run(e, streams["sp"])


class Arena:
    def __init__(self, nc, nwords):
        self.t = nc.alloc_sbuf_tensor("arena", [128, nwords], F32)
        self.n = nwords
        self.top = 0

    def mark(self):
        return self.top

    def release(self, m):
        self.top = m

    def alloc(self, nelem, dtype=F32):
        words = nelem if dtype == F32 else (nelem + 1) // 2
        words = (words + 7) // 8 * 8
        a = self.top
        self.top += words
        assert self.top <= self.n, ("SBUF arena overflow", self.top, self.n)
        ap = self.t[:, a:a + words]
        if dtype != F32:
            ap = ap.bitcast(dtype)
        return ap[:, 0:nelem]


class Rot:
    def __init__(self, items):
        self.items = items
        self.i = 0

    def next(self):
        it = self.items[self.i % len(self.items)]
        self.i += 1
        return it


def v3(ap, a):
    return ap.rearrange("p (a b) -> p a b", a=a)


def build_program(debug=False, stop=None):
    nc = bass.Bass("TRN2", target_bir_lowering=False)

    def din(name, shape, dt=F32):
        return nc.dram_tensor(name, list(shape), dt, kind="ExternalInput").ap()

    xa = din("xa", [S, D])
    xo = din("xo", [SO, D])
    w_in = din("w_in", [D, 7168])
    w_rga = din("w_rga", [4, 256, 256])
    w_rgi = din("w_rgi", [4, 256, 256])
    w_ba = din("w_ba", [D, D])
    w_bl = din("w_bl", [D, D])
    w_out = din("w_out", [D, D])
    w_up = din("w_up", [D, 4096])
    w_down = din("w_down", [4096, D])
    vecs = din("vecs", [128, 64])
    grep = din("grep", [128, 4 * D])
    ropek = din("ropek", [2, 32, S])
    ropeq = din("ropeq", [2, 32, SO])
    gmask_d = din("gmask", [128, 16 * 32])
    cmask_d = din("cmask", [128, 2 * 2 * 256])
    psel_d = din("psel", [128, 2])
    out_d = nc.dram_tensor("out", [SO, D], F32, kind="ExternalOutput").ap()

    def dscr(name, shape, dt):
        return nc.dram_tensor(name, list(shape), dt, kind="ExternalOutput" if debug else "Internal").ap()

    Ks = dscr("Ks", [NH, 128, S], BF16)
    Vs = dscr("Vs", [NH, 128, 64 * 130], BF16)
    Qs = dscr("Qs", [NH, 128, SO], BF16)
    Hs = dscr("Hs", [8, 128, SO], BF16)
    YL = dscr("YL", [8, 128, SO], BF16)
    H1 = dscr("H1", [SO, D], F32)

    P = Prog(nc)
    A = Arena(nc, ARENA_WORDS)
    psb = [nc.alloc_psum_tensor("psb%d" % i, [128, 512], F32) for i in range(8)]

    def ps(i):
        return psb[i][:, :], ("ps", i)

    identf = A.alloc(128)
    ident = A.alloc(128, BF16)
    vec = A.alloc(64)
    c1 = A.alloc(8)
    c2 = A.alloc(8)
    tmp8 = [A.alloc(8) for _ in range(4)]
    psel = A.alloc(2)
    ksum = A.alloc(NH * NBLK)
    ksum_hi = A.alloc(NH * NBLK, BF16)
    ksum_lo = A.alloc(NH * NBLK, BF16)
    carryx = A.alloc(8 * 3)
    carryh = A.alloc(8)
    pmat = A.alloc(128, BF16)
    pmatf = A.alloc(128)

    def vcol(base, c):
        return vec[:, base + c:base + c + 1]

    P.dma("sp", "c_vec", out=vec, in_=vecs, writes=["vec"])
    P.dma("sp", "c_psel", out=psel, in_=psel_d, writes=["psel"])
    P.op("pool", "memset", identf, 0.0, writes=["identf"])
    P.op("pool", "affine_select", out=identf, in_=identf, pattern=[[-1, 128]], compare_op=ALU.not_equal,
                                           fill=1.0, base=0, channel_multiplier=1,
         reads=["identf"], writes=["identf"])
    P.op("dve", "tensor_copy", out=ident, in_=identf, reads=["identf"], writes=["ident"])
    P.op("pool", "memset", pmatf, 0.0, writes=["pmatf"])
    P.op("pool", "affine_select", out=pmatf[:, 0:16], in_=pmatf[:, 0:16], pattern=[[-1, 16]],
                                           compare_op=ALU.not_equal, fill=1.0, base=-16, channel_multiplier=1,
         reads=["pmatf"], writes=["pmatf"])
    P.op("pool", "affine_select", out=pmatf[:, 16:32], in_=pmatf[:, 16:32], pattern=[[-1, 16]],
                                           compare_op=ALU.not_equal, fill=1.0, base=0, channel_multiplier=1,
         reads=["pmatf"], writes=["pmatf"])
    P.op("dve", "tensor_copy", out=pmat, in_=pmatf, reads=["pmatf"], writes=["pmat"])
    P.op("pool", "memset", ksum, 0.0, writes=["ksum"])
    P.op("pool", "memset", carryx, 0.0, writes=["carryx"])
    P.op("pool", "memset", carryh, 0.0, writes=["carryh"])
    lam = vec[:, 56:64]
    t_abs, t_e, t_l, t_r = tmp8
    P.op("dve", "tensor_scalar", out=t_r, in0=lam, scalar1=-1.0, scalar2=None, op0=ALU.mult,
         reads=["vec"], writes=["t_r"])
    P.op("dve", "tensor_tensor", out=t_abs, in0=lam, in1=t_r, op=ALU.max,
         reads=["vec", "t_r"], writes=["t_abs"])
    P.op("act", "activation", out=t_e, in_=t_abs, func=AF.Exp, scale=-1.0, reads=["t_abs"], writes=["t_e"])
    P.op("act", "activation", out=t_l, in_=t_e, func=AF.Ln, bias=1.0, reads=["t_e"], writes=["t_l"])
    P.op("dve", "tensor_scalar", out=t_r, in0=t_r, scalar1=0.0, scalar2=None, op0=ALU.max,
         reads=["t_r"], writes=["t_r"])
    P.op("dve", "tensor_tensor", out=t_r, in0=t_r, in1=t_l, op=ALU.add, reads=["t_r", "t_l"], writes=["t_r"])
    P.op("dve", "tensor_scalar", out=c1, in0=t_r, scalar1=-8.0, scalar2=None, op0=ALU.mult,
         reads=["t_r"], writes=["c1"])
    P.op("dve", "tensor_scalar", out=c2, in0=t_r, scalar1=-16.0, scalar2=None, op0=ALU.mult,
         reads=["t_r"], writes=["c2"])

    base_mark = A.mark()

    wcnt = [0]

    def load_w(dst3, src, key=None, semkey=None):
        wcnt[0] += 1
        P.dma("pool", "wl%d" % (wcnt[0] % 2), out=dst3, in_=src, writes=[key])

    def norm_tile(xt, xkey, gain_rep, gkey, xs, xskey, junk, ss_rot, act_xs=False):
        ss, sskey = ss_rot.next()
        P.op("act", "activation", out=junk, in_=xt, func=AF.Square, accum_out=ss[:, 0:1],
             reads=[xkey], writes=["junk", sskey])
        P.op("dve", "tensor_scalar", out=ss[:, 1:2], in0=ss[:, 0:1], scalar1=1.0 / D, scalar2=EPS,
                                              op0=ALU.mult, op1=ALU.add, reads=[sskey], writes=[sskey])
        P.op("act", "activation", out=ss[:, 2:3], in_=ss[:, 1:2], func=AF.Sqrt, reads=[sskey], writes=[sskey])
        P.op("dve", "reciprocal", out=ss[:, 3:4], in_=ss[:, 2:3], reads=[sskey], writes=[sskey])
        P.op("dve", "scalar_tensor_tensor", out=xs, in0=xt, scalar=ss[:, 3:4], in1=gain_rep,
                                                     op0=ALU.mult, op1=ALU.mult,
             reads=[xkey, sskey, gkey], writes=[xskey])
        return ss, sskey

    def transpose_tile(xs, xskey, psT, pskey, uT3, ukey, col0, evac_eng):
        pT = psT.bitcast(BF16)
        for c in range(8):
            P.op("pe", "transpose", out=pT[:, c * 128:(c + 1) * 128], in_=xs[:, c * 128:(c + 1) * 128],
                                                  identity=ident, reads=[xskey, "ident"], writes=[pskey])
        src = v3(pT, 8)
        dst = uT3[:, :, col0:col0 + 128]
        if evac_eng == "act":
            P.op("act", "activation", out=dst, in_=src, func=AF.Copy, reads=[pskey], writes=[ukey])
        else:
            P.op("dve", "tensor_copy", out=dst, in_=src, reads=[pskey], writes=[ukey])

    def rope_evac(pk, pkkey, n, C32, S32, rkey, kT, kTkey, t1, t2, tkey, swp, swpkey, evac_eng="act"):
        if evac_eng == "act":
            P.op("act", "activation", out=kT, in_=pk, func=AF.Copy, reads=[pkkey], writes=[kTkey])
        else:
            P.op("dve", "tensor_copy", out=kT, in_=pk, reads=[pkkey], writes=[kTkey])
        if not DBG.get('Krope', 1):
            return
        if DBG.get('Kt1', 1):
            P.op("dve", "tensor_tensor", out=t1[0:32, :], in0=pk[0:32, :], in1=(C32 if DBG.get('Kc32', 1) else t2[0:32, :]), op=ALU.mult,
                 reads=[pkkey, rkey] + ([kTkey] if DBG.get('Kser', 0) else []), writes=[tkey + "1"])
        if DBG.get('Ksw', 1):
            P.op("pe", "matmul", swp[:, 0:n], lhsT=pmat, rhs=kT, start=True, stop=True,
                 reads=[kTkey, "pmat"], writes=[swpkey])
        if not DBG.get('Krope2', 1):
            return
        P.op("dve", "tensor_tensor", out=t2[0:32, :], in0=swp[0:32, 0:n], in1=S32, op=ALU.mult,
             reads=[swpkey, rkey], writes=[tkey + "2"])
        P.op("pool", "tensor_tensor", out=kT[0:32, :], in0=t1[0:32, :], in1=t2[0:32, :], op=ALU.add,
             reads=[tkey + "1", tkey + "2"], writes=[kTkey])

    NG = S // 512
    wkvx = A.alloc(8 * 3072, BF16)
    wkvx3 = v3(wkvx, 8)
    wrg = A.alloc(2 * 8 * 256, BF16)
    wrg4 = wrg.rearrange("p (a c j) -> p a c j", a=2, c=8)
    g_pre = A.alloc(D)
    xts = [(A.alloc(D), ("xt", j)) for j in range(4)]
    xs_rot = Rot([(A.alloc(D, BF16), ("xs", i)) for i in range(2)])
    junk = A.alloc(D, BF16)
    ss_rot = Rot([(A.alloc(4), ("ss", i)) for i in range(4)])
    uT_rot = Rot([(A.alloc(8 * 512, BF16), ("uT", i)) for i in range(2)])
    kT_rot = Rot([(A.alloc(512, BF16), ("kT", i)) for i in range(3)])
    t_rot = Rot([((A.alloc(512), A.alloc(512)), "tr%d" % i) for i in range(2)])
    rope_rot = Rot([(A.alloc(2 * 512), ("rope", i)) for i in range(2)])
    vst = A.alloc(8 * 4 * 130, BF16)
    vst4 = vst.rearrange("p (h j e) -> p h j e", h=8, j=4)
    xl_rot = Rot([(A.alloc(515), ("xl", i)) for i in range(2)])
    xc_all = A.alloc(8 * 512)
    xc3 = v3(xc_all, 8)
    xcb = A.alloc(8 * 512, BF16)
    xcb3 = v3(xcb, 8)
    lw_rot = Rot([(tuple(A.alloc(512) for _ in range(5)), "lw%d" % i) for i in range(3)])
    hsel_rot = Rot([(A.alloc(8 * 256, BF16), ("hsel", i)) for i in range(2)])
    hsel_tmp = A.alloc(256)

    for s_ in range(3):
        load_w(wkvx3[:, :, s_ * 1024:(s_ + 1) * 1024],
               w_in[:, 1024 + s_ * 1024:2048 + s_ * 1024].rearrange("(c p) n -> p c n", p=128),
               key=("wkvx", s_), semkey="w%d" % s_)
    load_w(wrg4[:, 0], w_rga.rearrange("g (k p) j -> p (g k) j", p=128), key="wrg_a", semkey="w3")
    load_w(wrg4[:, 1], w_rgi.rearrange("g (k p) j -> p (g k) j", p=128), key="wrg_i", semkey="w4")
    P.dma("sp", "c_gain", out=g_pre, in_=grep[:, 0:D], writes=["g_pre"])
    P.op("pool", "memset", vst4[:, :, :, 128:130], 1.0, writes=["vst"])

    def load_x_1a(g):
        for j in range(4):
            xt, xkey = xts[j]
            r0 = g * 512 + j * 128
            P.dma("sp", "x%d" % j, out=xt, in_=xa[r0:r0 + 128, :], writes=[xkey])

    load_x_1a(0)
    mm_banks = Rot([4, 5, 6, 7])

    xs4 = [(A.alloc(D, BF16), ("xs4", i)) for i in range(4)]

    def norm_part_1a(g):
        for j in range(4):
            xt, xkey = xts[j]
            xs, xskey = xs4[j]
            norm_tile(xt, xkey, g_pre, "g_pre", xs, xskey, junk, ss_rot)
        if g + 1 < NG:
            load_x_1a(g + 1)

    def tr_part_1a(g):
        uT, ukey = uT_rot.next()
        uT3 = v3(uT, 8)
        for j in range(4):
            xs, xskey = xs4[j]
            psT, pskey = ps(j % 2)
            transpose_tile(xs, xskey, psT, pskey, uT3, ukey, j * 128, "act")
        return uT3, ukey

    NG1 = DBG.get('ng', NG)
    norm_part_1a(0)
    cur = tr_part_1a(0)
    for g in range(NG1):
        t0 = g * 512
        uT3, ukey = cur
        rp, rpkey = rope_rot.next()
        rp3 = v3(rp, 2)
        P.dma("sp", "rope%d" % (g % 2),
              out=rp3[0:32, :, :], in_=ropek[:, :, t0:t0 + 512].rearrange("a r t -> r a t"), writes=[rpkey])
        C32, S32 = rp3[0:32, 0, :], rp3[0:32, 1, :]
        for c in range(8):
            b = mm_banks.next()
            px, pxkey = ps(b)
            for k in range(8):
                P.op("pe", "matmul",
                     px, lhsT=wkvx3[:, k, 2048 + c * 128:2048 + (c + 1) * 128], rhs=uT3[:, k, :],
                     start=(k == 0), stop=(k == 7), reads=[ukey, ("wkvx", 2)], writes=[pxkey])
            xl, xlkey = xl_rot.next()
            P.op("pool", "tensor_copy", out=xl[:, 0:3], in_=carryx[:, 3 * c:3 * c + 3],
                 reads=["carryx"], writes=[xlkey])
            P.op("act", "activation", out=xl[:, 3:515], in_=px, func=AF.Copy,
                 reads=[pxkey], writes=[xlkey])
            P.op("pool", "tensor_copy", out=carryx[:, 3 * c:3 * c + 3], in_=xl[:, 512:515],
                 reads=[xlkey], writes=["carryx"])
            xcc = xc3[:, c, :]
            P.op("dve", "tensor_scalar",
                 out=xcc, in0=xl[:, 0:512], scalar1=vcol(0, c), scalar2=vcol(32, c), op0=ALU.mult, op1=ALU.add,
                 reads=[xlkey, "vec"], writes=[("xc", c)])
            for jj in range(1, 4):
                P.op("dve", "scalar_tensor_tensor",
                     out=xcc, in0=xl[:, jj:jj + 512], scalar=vcol(8 * jj, c), in1=xcc, op0=ALU.mult, op1=ALU.add,
                     reads=[xlkey, "vec", ("xc", c)], writes=[("xc", c)])
            P.op("act", "activation", out=xcb3[:, c, :], in_=xcc, func=AF.Copy,
                 reads=[("xc", c)], writes=[("xcb", c)])

        if g + 1 < NG1:
            norm_part_1a(g + 1)

        def k_part2(h, kT, kTkey, t1, t2, tkey, swp, swpkey):
            P.op("pe", "matmul", swp[:, 0:512], lhsT=pmat, rhs=kT, start=True, stop=True,
                 reads=[kTkey, "pmat"], writes=[swpkey])
            P.op("dve", "tensor_tensor", out=t2[0:32, :], in0=swp[0:32, 0:512], in1=S32, op=ALU.mult,
                 reads=[swpkey, rpkey], writes=[tkey + "2"])
            P.op("pool", "tensor_tensor", out=kT[0:32, :], in0=t1[0:32, :], in1=t2[0:32, :], op=ALU.add,
                 reads=[tkey + "1", tkey + "2"], writes=[kTkey])
            P.op("dve", "tensor_reduce",
                 out=ksum[:, h * 32 + 2 * g:h * 32 + 2 * g + 2], in_=v3(kT, 2), axis=AX.X, op=ALU.add,
                 reads=[kTkey], writes=["ksum"])
            P.dma("sp", "kst%d" % ((g * NH + h) % 3),
                  out=Ks[h, :, t0:t0 + 512], in_=kT, reads=[kTkey], writes=[("Ks", h)])

        pend = None
        for h in range(NH):
            b = mm_banks.next()
            pk, pkkey = ps(b)
            for c in range(8):
                P.op("pe", "matmul", pk, lhsT=wkvx3[:, c, h * 128:(h + 1) * 128], rhs=uT3[:, c, :],
                     start=(c == 0), stop=(c == 7), reads=[ukey, ("wkvx", 0)], writes=[pkkey])
            kT, kTkey = kT_rot.next()
            (t1, t2), tkey = t_rot.next()
            swp, swpkey = ps(2 + (h % 2))
            P.op("act", "activation", out=kT, in_=pk, func=AF.Copy, reads=[pkkey], writes=[kTkey])
            P.op("dve", "tensor_tensor", out=t1[0:32, :], in0=pk[0:32, :], in1=C32, op=ALU.mult,
                 reads=[pkkey, rpkey], writes=[tkey + "1"])
            if pend is not None:
                k_part2(*pend)
            pend = (h, kT, kTkey, t1, t2, tkey, swp, swpkey)
        k_part2(*pend)

        for j in range(4):
            for hh in range(2):
                b = mm_banks.next()
                pv, pvkey = ps(b)
                for c in range(8):
                    P.op("pe", "matmul",
                         pv, lhsT=uT3[:, c, j * 128:(j + 1) * 128], rhs=wkvx3[:, c, 1024 + hh * 512:1024 + (hh + 1) * 512],
                         start=(c == 0), stop=(c == 7), reads=[ukey, ("wkvx", 1)], writes=[pvkey])
                P.op("act", "activation",
                     out=vst4[:, hh * 4:(hh + 1) * 4, j, 0:128], in_=v3(pv, 4), func=AF.Copy,
                     reads=[pvkey], writes=["vst"])
        P.dma("sp", "vst",
              out=Vs.rearrange("h p (j e) -> p h j e", e=130)[:, :, 4 * g:4 * g + 4, :], in_=vst4,
              reads=["vst"], writes=["Vs"])

        if g + 1 < NG1:
            cur = tr_part_1a(g + 1)

        hsel, hselkey = hsel_rot.next()
        hsel3 = v3(hsel, 8)
        for op_ in range(0, 8, 2):
            pair = []
            for oc in (op_, op_ + 1):
                gi, jc = oc // 2, oc % 2
                (r_, i_, a_, m_, h_), lwkey = lw_rot.next()
                br = mm_banks.next()
                pr, prkey = ps(br)
                bi = mm_banks.next()
                pi, pikey = ps(bi)
                for which, pp, ppkey in ((0, pr, prkey), (1, pi, pikey)):
                    for kc in range(2):
                        P.op("pe", "matmul",
                             pp, lhsT=wrg4[:, which, gi * 2 + kc, jc * 128:(jc + 1) * 128], rhs=xcb3[:, gi * 2 + kc, :],
                             start=(kc == 0), stop=(kc == 1),
                             reads=[("xcb", gi * 2), ("xcb", gi * 2 + 1), "wrg_a", "wrg_i"], writes=[ppkey])
                pair.append((oc, r_, i_, a_, m_, h_, lwkey, pr, prkey, pi, pikey))
            for (oc, r_, i_, a_, m_, h_, lwkey, pr, prkey, pi, pikey) in pair:
                P.op("act", "activation", out=r_, in_=pr, func=AF.Sigmoid, bias=vcol(40, oc),
                     reads=[prkey, "vec"], writes=[lwkey + "r"])
                P.op("act", "activation", out=i_, in_=pi, func=AF.Sigmoid, bias=vcol(48, oc),
                     reads=[pikey, "vec"], writes=[lwkey + "i"])
            for (oc, r_, i_, a_, m_, h_, lwkey, pr, prkey, pi, pikey) in pair:
                P.op("act", "activation", out=a_, in_=r_, func=AF.Exp, scale=c1[:, oc:oc + 1],
                     reads=[lwkey + "r", "c1"], writes=[lwkey + "a"])
                P.op("act", "activation", out=m_, in_=r_, func=AF.Exp, scale=c2[:, oc:oc + 1],
                     reads=[lwkey + "r", "c2"], writes=[lwkey + "m"])
            for (oc, r_, i_, a_, m_, h_, lwkey, pr, prkey, pi, pikey) in pair:
                P.op("act", "activation", out=m_, in_=m_, func=AF.Sqrt, scale=-1.0, bias=1.0,
                     reads=[lwkey + "m"], writes=[lwkey + "m"])
            for (oc, r_, i_, a_, m_, h_, lwkey, pr, prkey, pi, pikey) in pair:
                P.op("dve", "tensor_tensor", out=i_, in0=i_, in1=xc3[:, oc, :], op=ALU.mult,
                     reads=[lwkey + "i", ("xc", oc)], writes=[lwkey + "i"])
                P.op("pool", "tensor_tensor", out=i_, in0=i_, in1=m_, op=ALU.mult,
                     reads=[lwkey + "i", lwkey + "m"], writes=[lwkey + "i"])
                P.op("dve", "tensor_tensor_scan",
                     out=h_, data0=a_, data1=i_, initial=carryh[:, oc:oc + 1], op0=ALU.mult, op1=ALU.add,
                     reads=[lwkey + "a", lwkey + "i", "carryh"], writes=[lwkey + "h"])
                P.op("dve", "tensor_copy", out=carryh[:, oc:oc + 1], in_=h_[:, 511:512],
                     reads=[lwkey + "h"], writes=["carryh"])
                P.op("pool", "tensor_scalar", out=hsel_tmp, in0=h_[:, 256:512], scalar1=psel[:, 1:2],
                     scalar2=None, op0=ALU.mult,
                     reads=[lwkey + "h", "psel"], writes=["hsel_tmp"])
                P.op("dve", "scalar_tensor_tensor",
                     out=hsel3[:, oc, :], in0=h_[:, 0:256], scalar=psel[:, 0:1], in1=hsel_tmp, op0=ALU.mult, op1=ALU.add,
                     reads=[lwkey + "h", "psel", "hsel_tmp"], writes=[hselkey])
        P.dma("sp", "hst%d" % (g % 2),
              out=Hs[:, :, g * 256:(g + 1) * 256].rearrange("c p t -> p c t"), in_=hsel3,
              reads=[hselkey], writes=["Hs"])

    P.op("dve", "tensor_copy", out=ksum_hi, in_=ksum, reads=["ksum"], writes=["ksum_hi"])
    P.op("dve", "tensor_tensor", out=ksum, in0=ksum, in1=ksum_hi, op=ALU.subtract,
         reads=["ksum", "ksum_hi"], writes=["ksum"])
    P.op("dve", "tensor_copy", out=ksum_lo, in_=ksum, reads=["ksum"], writes=["ksum_lo"])

    if stop == "1a":
        P.barrier()
        P.emit()
        return nc
    P.barrier()
    A.release(base_mark)
    NGO = SO // 512
    wq = A.alloc(8 * 1024, BF16)
    wq3 = v3(wq, 8)
    wgl = A.alloc(8 * 1024, BF16)
    wgl3 = v3(wgl, 8)
    wgt = A.alloc(8 * 1024, BF16)
    wgt3 = v3(wgt, 8)
    wbl = A.alloc(8 * 1024, BF16)
    wbl3 = v3(wbl, 8)
    g_pre = A.alloc(D)
    xts = [(A.alloc(D), ("xt", j)) for j in range(4)]
    xs_rot = Rot([(A.alloc(D, BF16), ("xs", i)) for i in range(2)])
    junk = A.alloc(D, BF16)
    ss_rot = Rot([(A.alloc(4), ("ss", i)) for i in range(4)])
    uT_rot = Rot([(A.alloc(8 * 512, BF16), ("uT", i)) for i in range(2)])
    kT_rot = Rot([(A.alloc(512, BF16), ("kT", i)) for i in range(3)])
    t_rot = Rot([((A.alloc(512), A.alloc(512)), "tr%d" % i) for i in range(2)])
    rope_rot = Rot([(A.alloc(2 * 512), ("rope", i)) for i in range(2)])
    hs_rot = Rot([(A.alloc(8 * 512, BF16), ("hs", i)) for i in range(2)])
    gw_rot = Rot([(tuple(A.alloc(512) for _ in range(3)), "gw%d" % i) for i in range(2)])
    hg_rot = Rot([(A.alloc(8 * 512, BF16), ("hg", i)) for i in range(2)])
    sg_rot = Rot([(A.alloc(512), ("sg", i)) for i in range(2)])
    yl_rot = Rot([(A.alloc(8 * 512, BF16), ("yl", i)) for i in range(2)])

    load_w(wq3, w_in[:, 0:1024].rearrange("(c p) n -> p c n", p=128), key="wq", semkey="w0")
    load_w(wgl3, w_in[:, 4096:5120].rearrange("(c p) n -> p c n", p=128), key="wgl", semkey="w1")
    load_w(wgt3, w_in[:, 6144:7168].rearrange("(c p) n -> p c n", p=128), key="wgt", semkey="w2")
    load_w(wbl3, w_bl.rearrange("(c p) n -> p c n", p=128), key="wbl", semkey="w3")
    P.dma("sp", "c_gain", out=g_pre, in_=grep[:, 0:D], writes=["g_pre"])

    def load_x_own(g, ntile):
        for j in range(ntile):
            xt, xkey = xts[j]
            r0 = g * ntile * 128 + j * 128
            P.dma("sp", "x%d" % j, out=xt, in_=xo[r0:r0 + 128, :], writes=[xkey])

    load_x_own(0, 4)
    for g in range(NGO):
        t0 = g * 512
        rp, rpkey = rope_rot.next()
        rp3 = v3(rp, 2)
        P.dma("sp", "rope%d" % (g % 2),
            out=rp3[0:32, :, :], in_=ropeq[:, :, t0:t0 + 512].rearrange("a r t -> r a t"), writes=[rpkey])
        hs, hskey = hs_rot.next()
        hs3 = v3(hs, 8)
        P.dma("sp", "hld%d" % (g % 2),
            out=hs3, in_=Hs[:, :, t0:t0 + 512].rearrange("c p t -> p c t"), reads=["Hs"], writes=[hskey])
        uT, ukey = uT_rot.next()
        uT3 = v3(uT, 8)
        for j in range(4):
            xt, xkey = xts[j]
            xs, xskey = xs_rot.next()
            norm_tile(xt, xkey, g_pre, "g_pre", xs, xskey, junk, ss_rot)
            psT, pskey = ps(j % 2)
            transpose_tile(xs, xskey, psT, pskey, uT3, ukey, j * 128, "act")
        if g + 1 < NGO:
            load_x_own(g + 1, 4)
        for h in range(NH):
            b = mm_banks.next()
            pk, pkkey = ps(b)
            for c in range(8):
                P.op("pe", "matmul", pk, lhsT=wq3[:, c, h * 128:(h + 1) * 128], rhs=uT3[:, c, :],
                                                               start=(c == 0), stop=(c == 7),
                     reads=[ukey, "wq"], writes=[pkkey])
            kT, kTkey = kT_rot.next()
            (t1, t2), tkey = t_rot.next()
            swp, swpkey = ps(2 + (h % 2))
            rope_evac(pk, pkkey, 512, rp3[0:32, 0, :], rp3[0:32, 1, :], rpkey, kT, kTkey, t1, t2, tkey, swp, swpkey)
            P.dma("sp", "kst%d" % ((g * NH + h) % 3),
                out=Qs[h, :, t0:t0 + 512], in_=kT, reads=[kTkey], writes=[("Qs", h)])
        hg, hgkey = hg_rot.next()
        hg3 = v3(hg, 8)
        for oc in range(8):
            b = mm_banks.next()
            pg, pgkey = ps(b)
            for c in range(8):
                P.op("pe", "matmul", pg, lhsT=wgl3[:, c, oc * 128:(oc + 1) * 128], rhs=uT3[:, c, :],
                                                                 start=(c == 0), stop=(c == 7),
                     reads=[ukey, "wgl"], writes=[pgkey])
            (gx, gq, gs), gwkey = gw_rot.next()
            P.op("act", "activation", out=gx, in_=pg, func=AF.Copy, reads=[pgkey], writes=[gwkey + "x"])
            P.op("act", "activation", out=gq, in_=pg, func=AF.Square, reads=[pgkey], writes=[gwkey + "q"])
            P.op("dve", "tensor_scalar", out=gq, in0=gq, scalar1=0.044715, scalar2=1.0, op0=ALU.mult, op1=ALU.add,
                 reads=[gwkey + "q"], writes=[gwkey + "q"])
            P.op("pool", "tensor_tensor", out=gq, in0=gq, in1=gx, op=ALU.mult,
                 reads=[gwkey + "q", gwkey + "x"], writes=[gwkey + "q"])
            P.op("act", "activation", out=gs, in_=gq, func=AF.Sigmoid, scale=1.5957691216057308,
                 reads=[gwkey + "q"], writes=[gwkey + "s"])
            P.op("pool", "tensor_tensor", out=gs, in0=gs, in1=gx, op=ALU.mult,
                 reads=[gwkey + "s", gwkey + "x"], writes=[gwkey + "s"])
            P.op("dve", "tensor_tensor", out=hg3[:, oc, :], in0=gs, in1=hs3[:, oc, :], op=ALU.mult,
                 reads=[gwkey + "s", hskey], writes=[hgkey])
        yl, ylkey = yl_rot.next()
        yl3 = v3(yl, 8)
        for oc in range(8):
            b = mm_banks.next()
            pg, pgkey = ps(b)
            for c in range(8):
                P.op("pe", "matmul", pg, lhsT=wgt3[:, c, oc * 128:(oc + 1) * 128], rhs=uT3[:, c, :],
                                                                 start=(c == 0), stop=(c == 7),
                     reads=[ukey, "wgt"], writes=[pgkey])
            sg, sgkey = sg_rot.next()
            P.op("act", "activation", out=sg, in_=pg, func=AF.Sigmoid, reads=[pgkey], writes=[sgkey])
            b2 = mm_banks.next()
            py, pykey = ps(b2)
            for c in range(8):
                P.op("pe", "matmul", py, lhsT=wbl3[:, c, oc * 128:(oc + 1) * 128], rhs=hg3[:, c, :],
                                                                 start=(c == 0), stop=(c == 7),
                     reads=[hgkey, "wbl"], writes=[pykey])
            P.op("dve", "tensor_tensor", out=yl3[:, oc, :], in0=py, in1=sg, op=ALU.mult,
                 reads=[pykey, sgkey], writes=[ylkey])
        P.dma("sp", "yst%d" % (g % 2),
            out=YL[:, :, t0:t0 + 512].rearrange("c p t -> p c t"), in_=yl3, reads=[ylkey], writes=["YL"])

    if stop == "1b":
        P.barrier()
        P.emit()
        return nc
    P.barrier()
    A.release(base_mark)
    attnT = A.alloc(NH * SO, BF16)
    attnT3 = v3(attnT, NH)
    p2_mark = A.mark()
    kv_rot = Rot([((A.alloc(S, BF16), A.alloc(64 * 130, BF16), A.alloc(SO, BF16)), "kv%d" % i) for i in range(2)])
    gmask = A.alloc(16 * 32)
    cmaskf = A.alloc(1024)
    cmask = A.alloc(1024, BF16)
    cmask3 = v3(cmask, 2)
    pt_rot = Rot([(A.alloc(512, BF16), ("pt", i)) for i in range(3)])
    acc_rot = Rot([(A.alloc(2 * 130), ("acc", i)) for i in range(2)])
    sel_rot = Rot([(A.alloc(2 * 32), ("sel", i)) for i in range(2)])
    g1_rot = Rot([(A.alloc(32 + 8 + 1), ("g1", i)) for i in range(4)])
    on_rot = Rot([(A.alloc(2 * 128, BF16), ("on", i)) for i in range(2)])
    rinv = A.alloc(2)

    P.dma("sp", "c_gmask", out=gmask, in_=gmask_d, writes=["gmask"])
    P.dma("sp", "c_cmask", out=cmaskf, in_=cmask_d, writes=["cmaskf"])
    P.op("dve", "tensor_copy", out=cmask, in_=cmaskf, reads=["cmaskf"], writes=["cmask"])

    def load_head(h):
        (KT, VV, QT), kvkey = kv_rot.next()
        P.dma("sp", kvkey + "k", out=KT, in_=Ks[h], reads=[("Ks", h)], writes=[kvkey + "k"])
        P.dma("sp", kvkey + "v", out=VV, in_=Vs[h], reads=["Vs"], writes=[kvkey + "v"])
        P.dma("sp", kvkey + "q", out=QT, in_=Qs[h], reads=[("Qs", h)], writes=[kvkey + "q"])
        return (KT, v3(VV, 64), QT), kvkey

    s_banks = Rot([0, 1, 2])
    o_banks = Rot([3, 4])
    nxt = load_head(0)
    for h in range(NH):
        (KT, VV3, QT), kvkey = nxt
        if h + 1 < NH:
            nxt = load_head(h + 1)
        units = [(i, j) for i in range(16) for j in range(2 * i + 2)]
        state = {}

        def emit_S(u):
            i, j = units[u]
            b = s_banks.next()
            sp_, spkey = ps(b)
            for kt in range(2):
                P.op("pe", "matmul",
                    sp_[:, kt * 256:(kt + 1) * 256], lhsT=KT[:, (2 * j + kt) * 128:(2 * j + kt + 1) * 128],
                    rhs=QT[:, i * 256:(i + 1) * 256], start=True, stop=True,
                    reads=[kvkey + "k", kvkey + "q"], writes=[spkey])
            state[u] = (sp_, spkey)

        def emit_gate(i):
            pgt, pgtkey = ps(5)
            acc, acckey = acc_rot.next()
            sel, selkey = sel_rot.next()
            for qt in range(2):
                qcols = QT[:, i * 256 + qt * 128:i * 256 + (qt + 1) * 128]
                P.op("pe", "matmul", pgt[:, qt * 32:(qt + 1) * 32], lhsT=qcols,
                                                                  rhs=ksum_hi[:, h * 32:(h + 1) * 32], start=True, stop=False,
                     reads=[kvkey + "q", "ksum_hi"], writes=[pgtkey])
                P.op("pe", "matmul", pgt[:, qt * 32:(qt + 1) * 32], lhsT=qcols,
                                                                  rhs=ksum_lo[:, h * 32:(h + 1) * 32], start=False, stop=True,
                     reads=[kvkey + "q", "ksum_lo"], writes=[pgtkey])
            for qt in range(2):
                g1, g1key = g1_rot.next()
                P.op("dve", "tensor_tensor", out=g1[:, 0:32], in0=pgt[:, qt * 32:(qt + 1) * 32],
                                                                    in1=gmask[:, i * 32:(i + 1) * 32], op=ALU.add,
                     reads=[pgtkey, "gmask"], writes=[g1key])
                P.op("dve", "max", out=g1[:, 32:40], in_=g1[:, 0:32], reads=[g1key], writes=[g1key])
                P.op("dve", "tensor_scalar", out=g1[:, 40:41], in0=g1[:, 35:36], scalar1=-1e29, scalar2=None,
                                                             op0=ALU.max, reads=[g1key], writes=[g1key])
                P.op("dve", "tensor_scalar", out=sel[:, qt * 32:(qt + 1) * 32], in0=g1[:, 0:32],
                                                                             scalar1=g1[:, 40:41], scalar2=None, op0=ALU.is_ge,
                     reads=[g1key], writes=[selkey])
            return acc, acckey, sel, selkey

        blk = {}

        def emit_rest(u):
            i, j = units[u]
            sp_, spkey = state.pop(u)
            acc, acckey, sel, selkey = blk[i]
            pt, ptkey = pt_rot.next()
            P.op("act", "activation", out=pt, in_=sp_, func=AF.Exp, scale=SCALE, reads=[spkey], writes=[ptkey])
            if j >= 2 * i:
                P.op("pool", "tensor_tensor", out=pt, in0=pt, in1=cmask3[:, j - 2 * i, :], op=ALU.mult,
                     reads=[ptkey, "cmask"], writes=[ptkey])
            b = o_banks.next()
            po, pokey = ps(b)
            po3 = po[:, 0:260].rearrange("p (a b) -> p a b", a=2)
            for qt in range(2):
                for kt in range(2):
                    P.op("pe", "matmul",
                        po3[:, qt, :], lhsT=pt[:, kt * 256 + qt * 128:kt * 256 + (qt + 1) * 128], rhs=VV3[:, 2 * j + kt, :],
                        start=(kt == 0), stop=(kt == 1), reads=[ptkey, kvkey + "v"], writes=[pokey])
            acc3 = v3(acc, 2)
            for qt in range(2):
                sc = sel[:, qt * 32 + j:qt * 32 + j + 1]
                if j == 0:
                    P.op("dve", "tensor_scalar", out=acc3[:, qt, :], in0=po3[:, qt, :], scalar1=sc,
                                                                        scalar2=None, op0=ALU.mult,
                         reads=[pokey, selkey], writes=[acckey])
                else:
                    P.op("dve", "scalar_tensor_tensor", out=acc3[:, qt, :], in0=po3[:, qt, :], scalar=sc,
                                                                               in1=acc3[:, qt, :], op0=ALU.mult, op1=ALU.add,
                         reads=[pokey, selkey, acckey], writes=[acckey])

        pending = []

        def emit_norm(i):
            acc, acckey, sel, selkey = blk[i]
            acc3 = v3(acc, 2)
            on, onkey = on_rot.next()
            on3 = v3(on, 2)
            P.op("dve", "reciprocal", out=rinv, in_=acc3[:, :, 128], reads=[acckey], writes=["rinv"])
            for qt in range(2):
                P.op("dve", "tensor_scalar", out=on3[:, qt, :], in0=acc3[:, qt, 0:128], scalar1=rinv[:, qt:qt + 1],
                                                             scalar2=None, op0=ALU.mult,
                     reads=[acckey, "rinv"], writes=[onkey])
            pending.append((i, on3, onkey))

        def emit_fin():
            i, on3, onkey = pending.pop(0)
            pT_, pTkey = ps(6)
            pTb = pT_.bitcast(BF16)
            for qt in range(2):
                P.op("pe", "transpose", out=pTb[:, qt * 128:(qt + 1) * 128], in_=on3[:, qt, :], identity=ident,
                     reads=[onkey, "ident"], writes=[pTkey])
            P.op("dve", "tensor_copy", out=attnT3[:, h, i * 256:(i + 1) * 256], in_=pTb[:, 0:256],
                 reads=[pTkey], writes=["attnT"])

        U = len(units)
        blk[0] = emit_gate(0)
        emit_S(0)
        for u in range(U):
            i, j = units[u]
            if u + 1 < U:
                ni, nj = units[u + 1]
                if nj == 0:
                    blk[ni] = emit_gate(ni)
                emit_S(u + 1)
            emit_rest(u)
            if j == 1 and pending:
                emit_fin()
            if j == 2 * i + 1:
                emit_norm(i)
        while pending:
            emit_fin()

    if debug:
        ATT = nc.dram_tensor("ATT", [NH, 128, SO], BF16, kind="ExternalOutput").ap()
        P.dma("sp", "dbg_att", out=ATT.rearrange("h p t -> p h t"), in_=attnT3, reads=["attnT"], writes=["ATT"])
    if stop == "2":
        P.barrier()
        P.emit()
        return nc
    P.barrier()
    A.release(p2_mark)
    NG3 = SO // 256
    wga = A.alloc(8 * 1024, BF16)
    wga3 = v3(wga, 8)
    wba = A.alloc(8 * 1024, BF16)
    wba3 = v3(wba, 8)
    wo = A.alloc(8 * 1024, BF16)
    wo3 = v3(wo, 8)
    g_pre = A.alloc(D)
    g_post = A.alloc(D)
    xts = [(A.alloc(D), ("xt", j)) for j in range(2)]
    xs_rot = Rot([(A.alloc(D, BF16), ("xs", i)) for i in range(2)])
    junk = A.alloc(D, BF16)
    ss_rot = Rot([(A.alloc(4), ("ss", i)) for i in range(4)])
    uT_rot = Rot([(A.alloc(8 * 256, BF16), ("uT", i)) for i in range(2)])
    sg_rot = Rot([(A.alloc(256), ("sg", i)) for i in range(2)])
    yld_rot = Rot([(A.alloc(8 * 256, BF16), ("yld", i)) for i in range(2)])
    mT_rot = Rot([(A.alloc(8 * 256, BF16), ("mT", i)) for i in range(2)])
    mix_rot = Rot([(A.alloc(D), ("mix", i)) for i in range(2)])

    load_w(wga3, w_in[:, 5120:6144].rearrange("(c p) n -> p c n", p=128), key="wga", semkey="w0")
    load_w(wba3, w_ba.rearrange("(c p) n -> p c n", p=128), key="wba", semkey="w1")
    load_w(wo3, w_out.rearrange("(c p) n -> p c n", p=128), key="wo", semkey="w2")
    P.dma("sp", "c_gain", out=g_pre, in_=grep[:, 0:D], writes=["g_pre"])
    P.dma("sp", "c_gain2", out=g_post, in_=grep[:, D:2 * D], writes=["g_post"])

    load_x_own(0, 2)
    for g in range(NG3):
        t0 = g * 256
        yld, yldkey = yld_rot.next()
        yld3 = v3(yld, 8)
        P.dma("sp", "yld%d" % (g % 2),
            out=yld3, in_=YL[:, :, t0:t0 + 256].rearrange("c p t -> p c t"), reads=["YL"], writes=[yldkey])
        uT, ukey = uT_rot.next()
        uT3 = v3(uT, 8)
        mix_list = []
        for j in range(2):
            xt, xkey = xts[j]
            xs, xskey = xs_rot.next()
            norm_tile(xt, xkey, g_pre, "g_pre", xs, xskey, junk, ss_rot)
            psT, pskey = ps(j % 2)
            transpose_tile(xs, xskey, psT, pskey, uT3, ukey, j * 128, "act")
        mT, mTkey = mT_rot.next()
        mT3 = v3(mT, 8)
        for oc in range(8):
            b = mm_banks.next()
            pg, pgkey = ps(b)
            for c in range(8):
                P.op("pe", "matmul", pg[:, 0:256], lhsT=wga3[:, c, oc * 128:(oc + 1) * 128], rhs=uT3[:, c, :],
                                                                 start=(c == 0), stop=(c == 7),
                     reads=[ukey, "wga"], writes=[pgkey])
            sg, sgkey = sg_rot.next()
            P.op("act", "activation", out=sg, in_=pg[:, 0:256], func=AF.Sigmoid, reads=[pgkey], writes=[sgkey])
            b2 = mm_banks.next()
            py, pykey = ps(b2)
            for c in range(8):
                P.op("pe", "matmul", py[:, 0:256], lhsT=wba3[:, c, oc * 128:(oc + 1) * 128],
                                                                 rhs=attnT3[:, c, t0:t0 + 256], start=(c == 0), stop=(c == 7),
                     reads=["attnT", "wba"], writes=[pykey])
            P.op("dve", "tensor_tensor", out=sg, in0=py[:, 0:256], in1=sg, op=ALU.mult,
                 reads=[pykey, sgkey], writes=[sgkey])
            P.op("pool", "tensor_tensor", out=mT3[:, oc, :], in0=sg, in1=yld3[:, oc, :], op=ALU.add,
                 reads=[sgkey, yldkey], writes=[mTkey])
        for j in range(2):
            xt, xkey = xts[j]
            mix, mixkey = mix_rot.next()
            for hh in range(2):
                b = mm_banks.next()
                pm, pmkey = ps(b)
                for c in range(8):
                    P.op("pe", "matmul", pm, lhsT=mT3[:, c, j * 128:(j + 1) * 128],
                                                                          rhs=wo3[:, c, hh * 512:(hh + 1) * 512],
                                                                          start=(c == 0), stop=(c == 7),
                         reads=[mTkey, "wo"], writes=[pmkey])
                P.op("act", "activation", out=mix[:, hh * 512:(hh + 1) * 512], in_=pm, func=AF.Copy,
                     reads=[pmkey], writes=[mixkey])
            ss, sskey = ss_rot.next()
            P.op("act", "activation", out=junk, in_=mix, func=AF.Square, accum_out=ss[:, 0:1],
                 reads=[mixkey], writes=["junk", sskey])
            P.op("dve", "tensor_scalar", out=ss[:, 1:2], in0=ss[:, 0:1], scalar1=1.0 / D, scalar2=EPS,
                                                         op0=ALU.mult, op1=ALU.add, reads=[sskey], writes=[sskey])
            P.op("act", "activation", out=ss[:, 2:3], in_=ss[:, 1:2], func=AF.Sqrt, reads=[sskey], writes=[sskey])
            P.op("dve", "reciprocal", out=ss[:, 3:4], in_=ss[:, 2:3], reads=[sskey], writes=[sskey])
            P.op("dve", "scalar_tensor_tensor", out=mix, in0=mix, scalar=ss[:, 3:4], in1=g_post,
                                                                         op0=ALU.mult, op1=ALU.mult,
                 reads=[mixkey, sskey, "g_post"], writes=[mixkey])
            P.op("pool", "tensor_tensor", out=mix, in0=mix, in1=xt, op=ALU.add,
                 reads=[mixkey, xkey], writes=[mixkey])
            r0 = t0 + j * 128
            P.dma("sp", "h1st%d" % j, out=H1[r0:r0 + 128, :], in_=mix,
                  reads=[mixkey], writes=["H1"])
        if g + 1 < NG3:
            load_x_own(g + 1, 2)

    if stop == "3a":
        P.barrier()
        P.emit()
        return nc
    P.barrier()
    A.release(base_mark)
    wup = A.alloc(8 * 4096, BF16)
    wup3 = v3(wup, 8)
    wdn = A.alloc(32 * 1024, BF16)
    wdn3 = v3(wdn, 32)
    g_pre = A.alloc(D)
    g_post = A.alloc(D)
    xts = [(A.alloc(D), ("xt", j)) for j in range(2)]
    xs_rot = Rot([(A.alloc(D, BF16), ("xs", i)) for i in range(1)])
    junk = A.alloc(D, BF16)
    ss_rot = Rot([(A.alloc(4), ("ss", i)) for i in range(4)])
    uT_rot = Rot([(A.alloc(8 * 256, BF16), ("uT", i)) for i in range(1)])
    aT = A.alloc(32 * 256, BF16)
    aT3 = v3(aT, 32)
    rl_rot = Rot([(A.alloc(256), ("rl", i)) for i in range(2)])
    mo_rot = Rot([(A.alloc(D), ("mo", i)) for i in range(1)])

    for s_ in range(4):
        load_w(wup3[:, :, s_ * 1024:(s_ + 1) * 1024], w_up[:, s_ * 1024:(s_ + 1) * 1024].rearrange("(c p) n -> p c n", p=128),
               key=("wup", s_), semkey="w%d" % s_)
    for s_ in range(4):
        load_w(wdn3[:, s_ * 8:(s_ + 1) * 8, :], w_down[s_ * 1024:(s_ + 1) * 1024, :].rearrange("(c p) n -> p c n", p=128),
               key=("wdn", s_), semkey="w%d" % (4 + s_) if s_ < 1 else "w%d" % s_)
    P.dma("sp", "c_gain", out=g_pre, in_=grep[:, 2 * D:3 * D], writes=["g_pre"])
    P.dma("sp", "c_gain2", out=g_post, in_=grep[:, 3 * D:4 * D], writes=["g_post"])

    def load_h1(g):
        for j in range(2):
            xt, xkey = xts[j]
            r0 = g * 256 + j * 128
            P.dma("sp", "x%d" % j, out=xt, in_=H1[r0:r0 + 128, :],
                  reads=["H1"], writes=[xkey])

    load_h1(0)
    for g in range(NG3):
        t0 = g * 256
        uT, ukey = uT_rot.next()
        uT3 = v3(uT, 8)
        for j in range(2):
            xt, xkey = xts[j]
            xs, xskey = xs_rot.next()
            norm_tile(xt, xkey, g_pre, "g_pre", xs, xskey, junk, ss_rot)
            psT, pskey = ps(j % 2)
            transpose_tile(xs, xskey, psT, pskey, uT3, ukey, j * 128, "act")
        for fc in range(32):
            b = mm_banks.next()
            pu, pukey = ps(b)
            for c in range(8):
                P.op("pe", "matmul", pu[:, 0:256], lhsT=wup3[:, c, fc * 128:(fc + 1) * 128], rhs=uT3[:, c, :],
                                                                 start=(c == 0), stop=(c == 7),
                     reads=[ukey, ("wup", fc // 8)], writes=[pukey])
            rl, rlkey = rl_rot.next()
            P.op("act", "activation", out=rl, in_=pu[:, 0:256], func=AF.Relu, reads=[pukey], writes=[rlkey])
            eng = "dve" if fc % 2 == 0 else "pool"
            P.op(eng, "tensor_tensor", out=aT3[:, fc, :], in0=rl, in1=rl, op=ALU.mult,
                 reads=[rlkey], writes=[("aT", fc)])
        for j in range(2):
            xt, xkey = xts[j]
            mo, mokey = mo_rot.next()
            for hh in range(2):
                b = mm_banks.next()
                pm, pmkey = ps(b)
                for fc in range(32):
                    P.op("pe", "matmul", pm, lhsT=aT3[:, fc, j * 128:(j + 1) * 128],
                                                                            rhs=wdn3[:, fc, hh * 512:(hh + 1) * 512],
                                                                            start=(fc == 0), stop=(fc == 31),
                         reads=[("aT", fc), ("wdn", fc // 8)], writes=[pmkey])
                P.op("act", "activation", out=mo[:, hh * 512:(hh + 1) * 512], in_=pm, func=AF.Copy,
                     reads=[pmkey], writes=[mokey])
            ss, sskey = ss_rot.next()
            P.op("act", "activation", out=junk, in_=mo, func=AF.Square, accum_out=ss[:, 0:1],
                 reads=[mokey], writes=["junk", sskey])
            P.op("dve", "tensor_scalar", out=ss[:, 1:2], in0=ss[:, 0:1], scalar1=1.0 / D, scalar2=EPS,
                                                         op0=ALU.mult, op1=ALU.add, reads=[sskey], writes=[sskey])
            P.op("act", "activation", out=ss[:, 2:3], in_=ss[:, 1:2], func=AF.Sqrt, reads=[sskey], writes=[sskey])
            P.op("dve", "reciprocal", out=ss[:, 3:4], in_=ss[:, 2:3], reads=[sskey], writes=[sskey])
            P.op("dve", "scalar_tensor_tensor", out=mo, in0=mo, scalar=ss[:, 3:4], in1=g_post,
                                                                       op0=ALU.mult, op1=ALU.mult,
                 reads=[mokey, sskey, "g_post"], writes=[mokey])
            P.op("pool", "tensor_tensor", out=mo, in0=mo, in1=xt, op=ALU.add,
                 reads=[mokey, xkey], writes=[mokey])
            r0 = t0 + j * 128
            P.dma("sp", "ost", out=out_d[r0:r0 + 128, :], in_=mo,
                  reads=[mokey], writes=["out"])
        if g + 1 < NG3:
            load_h1(g + 1)

    P.barrier()
    P.emit()
    return nc


def _fm(v):
    return np.ascontiguousarray(np.asarray(v, np.float32).reshape(8, 128).T)


def _rope_tables(pos):
    inv_freq = (np.float32(500000.0) ** (-(np.arange(0, 32, 2, dtype=np.float32)) / np.float32(32))).astype(np.float32)
    ang = (pos.astype(np.float32)[:, None] * inv_freq[None, :]).astype(np.float32)
    cos = np.cos(ang.astype(np.float64)).astype(np.float32).T
    sin = np.sin(ang.astype(np.float64)).astype(np.float32).T
    c32 = np.concatenate([cos, cos], axis=0)
    s32 = np.concatenate([-sin, sin], axis=0)
    return np.ascontiguousarray(np.stack([c32, s32], axis=0))


_NC_CACHE = {}
DBG = {}


def kernel(x, attn_pre_norm, attn_post_norm, w_in, conv_w, conv_b, w_rg_a, b_rg_a, w_rg_i, b_rg_i, lru_lambda,
           w_branch_attn, w_branch_lru, w_out, mlp_pre_norm, mlp_post_norm, w_mlp_up, w_mlp_down):
    x = np.asarray(x, np.float32)
    f = lambda a: np.ascontiguousarray(np.asarray(a, np.float32))
    vec_cols = [_fm(conv_w[0][j]) for j in range(4)] + [_fm(conv_b[0]), _fm(b_rg_a[0]), _fm(b_rg_i[0]), _fm(lru_lambda[0])]
    vecs = np.ascontiguousarray(np.concatenate(vec_cols, axis=1))
    grep = np.ascontiguousarray(np.broadcast_to(np.concatenate(
        [f(attn_pre_norm[0]), f(attn_post_norm[0]), f(mlp_pre_norm[0]), f(mlp_post_norm[0])])[None, :], (128, 4096)))
    ropek = _rope_tables(np.arange(S))
    shared = {
        "w_in": f(w_in[0]), "w_rga": f(w_rg_a[0]), "w_rgi": f(w_rg_i[0]), "w_ba": f(w_branch_attn[0]),
        "w_bl": f(w_branch_lru[0]), "w_out": f(w_out[0]), "w_up": f(w_mlp_up[0]), "w_down": f(w_mlp_down[0]),
        "vecs": vecs, "grep": grep, "ropek": ropek,
    }
    tri = (np.arange(256)[:, None] <= np.arange(256)[None, :]).astype(np.float32)
    in_maps = []
    own_rows = []
    for c in range(8):
        b, p = c // 2, c % 2
        blocks = np.arange(16) * 2 + p
        rows = (blocks[:, None] * 256 + np.arange(256)[None, :]).reshape(-1)
        own_rows.append((b, rows))
        gm = np.zeros((16, 32), np.float32)
        for i in range(16):
            gb = 2 * i + p
            gm[i, gb] = 1e30
            gm[i, gb + 1:] = -1e30
        gmask = np.ascontiguousarray(np.broadcast_to(gm.reshape(1, 512), (128, 512)))
        ma = tri if p == 0 else np.ones((256, 256), np.float32)
        mb = np.zeros((256, 256), np.float32) if p == 0 else tri
        cm = np.stack([ma.reshape(2, 128, 256).transpose(1, 0, 2), mb.reshape(2, 128, 256).transpose(1, 0, 2)], axis=1)
        cmask = np.ascontiguousarray(cm.reshape(128, 1024))
        psel = np.ascontiguousarray(np.broadcast_to(np.array([1.0 - p, float(p)], np.float32)[None, :], (128, 2)))
        m = dict(shared)
        m.update({"xa": np.ascontiguousarray(x[b]), "xo": np.ascontiguousarray(x[b][rows]),
                  "ropeq": _rope_tables(rows), "gmask": gmask, "cmask": cmask, "psel": psel})
        in_maps.append(m)
    if _NC_CACHE.get("prep_only"):
        return in_maps, own_rows
    if "nc" not in _NC_CACHE:
        _NC_CACHE["nc"] = build_program()
    res = run_bass_kernel_spmd(_NC_CACHE["nc"], in_maps, core_ids=list(range(8)))
    out = np.empty((4, S, D), np.float32)
    for c in range(8):
        b, rows = own_rows[c]
        out[b, rows] = res.results[c]["out"]
    return out
```

```python
import numpy as np
import concourse.bass as bass
import concourse.mybir as mybir
from concourse.bass_utils import run_bass_kernel_spmd

F32 = mybir.dt.float32
BF16 = mybir.dt.bfloat16
AF = mybir.ActivationFunctionType
ALU = mybir.AluOpType
AX = mybir.AxisListType

SAME_ENGINE_SYNC = True
D = 1024
S = 8192
SO = 4096
NH = 8
NBLK = 32
ARENA_WORDS = 53100
SCALE = 128 ** -0.5
EPS = 1e-6


class Prog:
    ENG = ("pe", "act", "dve", "pool", "sp")

    def __init__(self, nc):
        self.nc = nc
        self.streams = {k: [] for k in self.ENG}
        self.esem = {k: nc.alloc_semaphore(name="es_" + k) for k in self.ENG}
        self.ecount = {k: 0 for k in self.ENG}
        self.obs = {k: {} for k in self.ENG}
        self.res = {}
        self.dsem = {}
        self.last_dma = {}

    def _deps(self, reads, writes):
        toks = []
        for r in reads:
            st = self.res.get(r)
            if st is not None and st[0] is not None:
                toks.append(st[0])
        for w in writes:
            st = self.res.get(w)
            if st is not None:
                if st[0] is not None:
                    toks.append(st[0])
                toks.extend(st[1])
        return toks

    def _record(self, tok, reads, writes):
        for r in reads:
            st = self.res.setdefault(r, [None, []])
            st[1].append(tok)
        for w in writes:
            self.res[w] = [tok, []]

    def _waits(self, eng, toks):
        obs = self.obs[eng]
        need = {}
        for (sem, val, src, snap) in toks:
            if src == eng and (eng == "pe" or not SAME_ENGINE_SYNC):
                continue
            if obs.get(id(sem), 0) >= val:
                continue
            cur = need.get(id(sem))
            if cur is None or cur[1] < val:
                need[id(sem)] = (sem, val, snap)
        out = []
        for k, (sem, val, snap) in need.items():
            if obs.get(k, 0) >= val:
                continue
            out.append((sem, val))
            obs[k] = val
            for kk, vv in snap.items():
                if obs.get(kk, 0) < vv:
                    obs[kk] = vv
        return out

    def op(self, eng, name, *args, reads=(), writes=(), **kw):
        fn = (name, args, kw)
        psr = [r for r in reads if isinstance(r, tuple) and r[0] == "ps"]
        if psr:
            reads = [r for r in reads if not (isinstance(r, tuple) and r[0] == "ps")]
            writes = list(writes) + [r for r in psr if r not in writes]
        toks = self._deps(reads, writes)
        waits = self._waits(eng, toks)
        self.ecount[eng] += 1
        sem = self.esem[eng]
        tok = (sem, self.ecount[eng], eng, dict(self.obs[eng]))
        self.streams[eng].append((waits, fn, sem, 1))
        self._record(tok, reads, writes)

    def dma(self, queue, semkey, reads=(), writes=(), **kw):
        fn = ("dma_start", (), kw)
        if semkey not in self.dsem:
            self.dsem[semkey] = [self.nc.alloc_semaphore(name="ds_%d" % len(self.dsem)), 0, None]
        ent = self.dsem[semkey]
        toks = self._deps(reads, writes)
        if ent[2] is not None:
            toks.append(ent[2])
        waits = self._waits(queue, toks)
        ent[1] += 16
        tok = (ent[0], ent[1], "dma", dict(self.obs[queue]))
        ent[2] = tok
        self.last_dma[semkey] = tok
        self.streams[queue].append((waits, fn, ent[0], 16))
        self._record(tok, reads, writes)

    def barrier(self):
        toks = [(self.esem[k], self.ecount[k]) for k in self.ENG if self.ecount[k] > 0]
        toks += [(t[0], t[1]) for t in self.last_dma.values()]
        for e in self.ENG:
            obs = self.obs[e]
            waits = []
            for (sem, val) in toks:
                if obs.get(id(sem), 0) < val:
                    waits.append((sem, val))
                    obs[id(sem)] = val
            self.streams[e].append((waits, None, None, 0))
        self.res = {}

    def emit(self):
        nc = self.nc
        streams = self.streams

        def run(e, lst):
            for (waits, fn, sem, inc) in lst:
                for (s, v) in waits:
                    e.wait_ge(s, v)
                if fn is not None:
                    getattr(e, fn[0])(*fn[1], **fn[2]).then_inc(sem, inc)

        with nc.Block() as block:
            @block.tensor
            def _(e):
                run(e, streams["pe"])

            @block.scalar
            def _(e):
                run(e, streams["act"])

            @block.vector
            def _(e):
                run(e, streams["dve"])

            @block.gpsimd
            def _(e):
                run(e, streams["pool"])

            @block.sync
            def _(e):
                run(e, streams["sp"])


class Arena:
    def __init__(self, nc, nwords):
        self.t = nc.alloc_sbuf_tensor("arena", [128, nwords], F32)
        self.n = nwords
        self.top = 0

    def mark(self):
        return self.top

    def release(self, m):
        self.top = m

    def alloc(self, nelem, dtype=F32):
        words = nelem if dtype == F32 else (nelem + 1) // 2
        words = (words + 7) // 8 * 8
        a = self.top
        self.top += words
        assert self.top <= self.n, ("SBUF arena overflow", self.top, self.n)
        ap = self.t[:, a:a + words]
        if dtype != F32:
            ap = ap.bitcast(dtype)
        return ap[:, 0:nelem]


class Rot:
    def __init__(self, items):
        self.items = items
        self.i = 0

    def next(self):
        it = self.items[self.i % len(self.items)]
        self.i += 1
        return it


def v3(ap, a):
    return ap.rearrange("p (a b) -> p a b", a=a)


def build_program(debug=False, stop=None):
    nc = bass.Bass("TRN2", target_bir_lowering=False)

    def din(name, shape, dt=F32):
        return nc.dram_tensor(name, list(shape), dt, kind="ExternalInput").ap()

    xa = din("xa", [S, D])
    xo = din("xo", [SO, D])
    w_in = din("w_in", [D, 7168])
    w_rga = din("w_rga", [4, 256, 256])
    w_rgi = din("w_rgi", [4, 256, 256])
    w_ba = din("w_ba", [D, D])
    w_bl = din("w_bl", [D, D])
    w_out = din("w_out", [D, D])
    w_up = din("w_up", [D, 4096])
    w_down = din("w_down", [4096, D])
    vecs = din("vecs", [128, 64])
    grep = din("grep", [128, 4 * D])
    ropek = din("ropek", [2, 32, S])
    ropeq = din("ropeq", [2, 32, SO])
    gmask_d = din("gmask", [128, 16 * 32])
    cmask_d = din("cmask", [128, 2 * 2 * 256])
    psel_d = din("psel", [128, 2])
    out_d = nc.dram_tensor("out", [SO, D], F32, kind="ExternalOutput").ap()

    def dscr(name, shape, dt):
        return nc.dram_tensor(name, list(shape), dt, kind="ExternalOutput" if debug else "Internal").ap()

    Ks = dscr("Ks", [NH, 128, S], BF16)
    Vs = dscr("Vs", [NH, 128, 64 * 130], BF16)
    Qs = dscr("Qs", [NH, 128, SO], BF16)
    Hs = dscr("Hs", [8, 128, SO], BF16)
    YL = dscr("YL", [8, 128, SO], BF16)
    H1 = dscr("H1", [SO, D], F32)

    P = Prog(nc)
    A = Arena(nc, ARENA_WORDS)
    psb = [nc.alloc_psum_tensor("psb%d" % i, [128, 512], F32) for i in range(8)]

    def ps(i):
        return psb[i][:, :], ("ps", i)

    identf = A.alloc(128)
    ident = A.alloc(128, BF16)
    vec = A.alloc(64)
    c1 = A.alloc(8)
    c2 = A.alloc(8)
    tmp8 = [A.alloc(8) for _ in range(4)]
    psel = A.alloc(2)
    ksum = A.alloc(NH * NBLK)
    ksum_hi = A.alloc(NH * NBLK, BF16)
    ksum_lo = A.alloc(NH * NBLK, BF16)
    carryx = A.alloc(8 * 3)
    carryh = A.alloc(8)
    pmat = A.alloc(128, BF16)
    pmatf = A.alloc(128)

    def vcol(base, c):
        return vec[:, base + c:base + c + 1]

    P.dma("sp", "c_vec", out=vec, in_=vecs, writes=["vec"])
    P.dma("sp", "c_psel", out=psel, in_=psel_d, writes=["psel"])
    P.op("pool", "memset", identf, 0.0, writes=["identf"])
    P.op("pool", "affine_select", out=identf, in_=identf, pattern=[[-1, 128]], compare_op=ALU.not_equal,
                                           fill=1.0, base=0, channel_multiplier=1,
         reads=["identf"], writes=["identf"])
    P.op("dve", "tensor_copy", out=ident, in_=identf, reads=["identf"], writes=["ident"])
    P.op("pool", "memset", pmatf, 0.0, writes=["pmatf"])
    P.op("pool", "affine_select", out=pmatf[:, 0:16], in_=pmatf[:, 0:16], pattern=[[-1, 16]],
                                           compare_op=ALU.not_equal, fill=1.0, base=-16, channel_multiplier=1,
         reads=["pmatf"], writes=["pmatf"])
    P.op("pool", "affine_select", out=pmatf[:, 16:32], in_=pmatf[:, 16:32], pattern=[[-1, 16]],
                                           compare_op=ALU.not_equal, fill=1.0, base=0, channel_multiplier=1,
         reads=["pmatf"], writes=["pmatf"])
    P.op("dve", "tensor_copy", out=pmat, in_=pmatf, reads=["pmatf"], writes=["pmat"])
    P.op("pool", "memset", ksum, 0.0, writes=["ksum"])
    P.op("pool", "memset", carryx, 0.0, writes=["carryx"])
    P.op("pool", "memset", carryh, 0.0, writes=["carryh"])
    lam = vec[:, 56:64]
    t_abs, t_e, t_l, t_r = tmp8
    P.op("dve", "tensor_scalar", out=t_r, in0=lam, scalar1=-1.0, scalar2=None, op0=ALU.mult,
         reads=["vec"], writes=["t_r"])
    P.op("dve", "tensor_tensor", out=t_abs, in0=lam, in1=t_r, op=ALU.max,
         reads=["vec", "t_r"], writes=["t_abs"])
    P.op("act", "activation", out=t_e, in_=t_abs, func=AF.Exp, scale=-1.0, reads=["t_abs"], writes=["t_e"])
    P.op("act", "activation", out=t_l, in_=t_e, func=AF.Ln, bias=1.0, reads=["t_e"], writes=["t_l"])
    P.op("dve", "tensor_scalar", out=t_r, in0=t_r, scalar1=0.0, scalar2=None, op0=ALU.max,
         reads=["t_r"], writes=["t_r"])
    P.op("dve", "tensor_tensor", out=t_r, in0=t_r, in1=t_l, op=ALU.add, reads=["t_r", "t_l"], writes=["t_r"])
    P.op("dve", "tensor_scalar", out=c1, in0=t_r, scalar1=-8.0, scalar2=None, op0=ALU.mult,
         reads=["t_r"], writes=["c1"])
    P.op("dve", "tensor_scalar", out=c2, in0=t_r, scalar1=-16.0, scalar2=None, op0=ALU.mult,
         reads=["t_r"], writes=["c2"])

    base_mark = A.mark()

    wcnt = [0]

    def load_w(dst3, src, key=None, semkey=None):
        wcnt[0] += 1
        P.dma("pool", "wl%d" % (wcnt[0] % 2), out=dst3, in_=src, writes=[key])

    def norm_tile(xt, xkey, gain_rep, gkey, xs, xskey, junk, ss_rot, act_xs=False):
        ss, sskey = ss_rot.next()
        P.op("act", "activation", out=junk, in_=xt, func=AF.Square, accum_out=ss[:, 0:1],
             reads=[xkey], writes=["junk", sskey])
        P.op("dve", "tensor_scalar", out=ss[:, 1:2], in0=ss[:, 0:1], scalar1=1.0 / D, scalar2=EPS,
                                              op0=ALU.mult, op1=ALU.add, reads=[sskey], writes=[sskey])
        P.op("act", "activation", out=ss[:, 2:3], in_=ss[:, 1:2], func=AF.Sqrt, reads=[sskey], writes=[sskey])
        P.op("dve", "reciprocal", out=ss[:, 3:4], in_=ss[:, 2:3], reads=[sskey], writes=[sskey])
        P.op("dve", "scalar_tensor_tensor", out=xs, in0=xt, scalar=ss[:, 3:4], in1=gain_rep,
                                                     op0=ALU.mult, op1=ALU.mult,
             reads=[xkey, sskey, gkey], writes=[xskey])
        return ss, sskey

    def transpose_tile(xs, xskey, psT, pskey, uT3, ukey, col0, evac_eng):
        pT = psT.bitcast(BF16)
        for c in range(8):
            P.op("pe", "transpose", out=pT[:, c * 128:(c + 1) * 128], in_=xs[:, c * 128:(c + 1) * 128],
                                                  identity=ident, reads=[xskey, "ident"], writes=[pskey])
        src = v3(pT, 8)
        dst = uT3[:, :, col0:col0 + 128]
        if evac_eng == "act":
            P.op("act", "activation", out=dst, in_=src, func=AF.Copy, reads=[pskey], writes=[ukey])
        else:
            P.op("dve", "tensor_copy", out=dst, in_=src, reads=[pskey], writes=[ukey])

    def rope_evac(pk, pkkey, n, C32, S32, rkey, kT, kTkey, t1, t2, tkey, swp, swpkey, evac_eng="act"):
        if evac_eng == "act":
            P.op("act", "activation", out=kT, in_=pk, func=AF.Copy, reads=[pkkey], writes=[kTkey])
        else:
            P.op("dve", "tensor_copy", out=kT, in_=pk, reads=[pkkey], writes=[kTkey])
        if not DBG.get('Krope', 1):
            return
        if DBG.get('Kt1', 1):
            P.op("dve", "tensor_tensor", out=t1[0:32, :], in0=pk[0:32, :], in1=(C32 if DBG.get('Kc32', 1) else t2[0:32, :]), op=ALU.mult,
                 reads=[pkkey, rkey] + ([kTkey] if DBG.get('Kser', 0) else []), writes=[tkey + "1"])
        if DBG.get('Ksw', 1):
            P.op("pe", "matmul", swp[:, 0:n], lhsT=pmat, rhs=kT, start=True, stop=True,
                 reads=[kTkey, "pmat"], writes=[swpkey])
        if not DBG.get('Krope2', 1):
            return
        P.op("dve", "tensor_tensor", out=t2[0:32, :], in0=swp[0:32, 0:n], in1=S32, op=ALU.mult,
             reads=[swpkey, rkey], writes=[tkey + "2"])
        P.op("pool", "tensor_tensor", out=kT[0:32, :], in0=t1[0:32, :], in1=t2[0:32, :], op=ALU.add,
             reads=[tkey + "1", tkey + "2"], writes=[kTkey])

    NG = S // 512
    wkvx = A.alloc(8 * 3072, BF16)
    wkvx3 = v3(wkvx, 8)
    wrg = A.alloc(2 * 8 * 256, BF16)
    wrg4 = wrg.rearrange("p (a c j) -> p a c j", a=2, c=8)
    g_pre = A.alloc(D)
    xts = [(A.alloc(D), ("xt", j)) for j in range(4)]
    xs_rot = Rot([(A.alloc(D, BF16), ("xs", i)) for i in range(2)])
    junk = A.alloc(D, BF16)
    ss_rot = Rot([(A.alloc(4), ("ss", i)) for i in range(4)])
    uT_rot = Rot([(A.alloc(8 * 512, BF16), ("uT", i)) for i in range(2)])
    kT_rot = Rot([(A.alloc(512, BF16), ("kT", i)) for i in range(3)])
    t_rot = Rot([((A.alloc(512), A.alloc(512)), "tr%d" % i) for i in range(2)])
    rope_rot = Rot([(A.alloc(2 * 512), ("rope", i)) for i in range(2)])
    vst = A.alloc(8 * 4 * 130, BF16)
    vst4 = vst.rearrange("p (h j e) -> p h j e", h=8, j=4)
    xl_rot = Rot([(A.alloc(515), ("xl", i)) for i in range(2)])
    xc_all = A.alloc(8 * 512)
    xc3 = v3(xc_all, 8)
    xcb = A.alloc(8 * 512, BF16)
    xcb3 = v3(xcb, 8)
    lw_rot = Rot([(tuple(A.alloc(512) for _ in range(5)), "lw%d" % i) for i in range(3)])
    hsel_rot = Rot([(A.alloc(8 * 256, BF16), ("hsel", i)) for i in range(2)])
    hsel_tmp = A.alloc(256)

    for s_ in range(3):
        load_w(wkvx3[:, :, s_ * 1024:(s_ + 1) * 1024],
               w_in[:, 1024 + s_ * 1024:2048 + s_ * 1024].rearrange("(c p) n -> p c n", p=128),
               key=("wkvx", s_), semkey="w%d" % s_)
    load_w(wrg4[:, 0], w_rga.rearrange("g (k p) j -> p (g k) j", p=128), key="wrg_a", semkey="w3")
    load_w(wrg4[:, 1], w_rgi.rearrange("g (k p) j -> p (g k) j", p=128), key="wrg_i", semkey="w4")
    P.dma("sp", "c_gain", out=g_pre, in_=grep[:, 0:D], writes=["g_pre"])
    P.op("pool", "memset", vst4[:, :, :, 128:130], 1.0, writes=["vst"])

    def load_x_1a(g):
        for j in range(4):
            xt, xkey = xts[j]
            r0 = g * 512 + j * 128
            P.dma("sp", "x%d" % j, out=xt, in_=xa[r0:r0 + 128, :], writes=[xkey])

    load_x_1a(0)
    mm_banks = Rot([4, 5, 6, 7])

    xs4 = [(A.alloc(D, BF16), ("xs4", i)) for i in range(4)]

    def norm_part_1a(g):
        for j in range(4):
            xt, xkey = xts[j]
            xs, xskey = xs4[j]
            norm_tile(xt, xkey, g_pre, "g_pre", xs, xskey, junk, ss_rot)
        if g + 1 < NG:
            load_x_1a(g + 1)

    def tr_part_1a(g):
        uT, ukey = uT_rot.next()
        uT3 = v3(uT, 8)
        for j in range(4):
            xs, xskey = xs4[j]
            psT, pskey = ps(j % 2)
            transpose_tile(xs, xskey, psT, pskey, uT3, ukey, j * 128, "act")
        return uT3, ukey

    NG1 = DBG.get('ng', NG)
    norm_part_1a(0)
    cur = tr_part_1a(0)
    for g in range(NG1):
        t0 = g * 512
        uT3, ukey = cur
        rp, rpkey = rope_rot.next()
        rp3 = v3(rp, 2)
        P.dma("sp", "rope%d" % (g % 2),
              out=rp3[0:32, :, :], in_=ropek[:, :, t0:t0 + 512].rearrange("a r t -> r a t"), writes=[rpkey])
        C32, S32 = rp3[0:32, 0, :], rp3[0:32, 1, :]
        for c in range(8):
            b = mm_banks.next()
            px, pxkey = ps(b)
            for k in range(8):
                P.op("pe", "matmul",
                     px, lhsT=wkvx3[:, k, 2048 + c * 128:2048 + (c + 1) * 128], rhs=uT3[:, k, :],
                     start=(k == 0), stop=(k == 7), reads=[ukey, ("wkvx", 2)], writes=[pxkey])
            xl, xlkey = xl_rot.next()
            P.op("pool", "tensor_copy", out=xl[:, 0:3], in_=carryx[:, 3 * c:3 * c + 3],
                 reads=["carryx"], writes=[xlkey])
            P.op("act", "activation", out=xl[:, 3:515], in_=px, func=AF.Copy,
                 reads=[pxkey], writes=[xlkey])
            P.op("pool", "tensor_copy", out=carryx[:, 3 * c:3 * c + 3], in_=xl[:, 512:515],
                 reads=[xlkey], writes=["carryx"])
            xcc = xc3[:, c, :]
            P.op("dve", "tensor_scalar",
                 out=xcc, in0=xl[:, 0:512], scalar1=vcol(0, c), scalar2=vcol(32, c), op0=ALU.mult, op1=ALU.add,
                 reads=[xlkey, "vec"], writes=[("xc", c)])
            for jj in range(1, 4):
                P.op("dve", "scalar_tensor_tensor",
                     out=xcc, in0=xl[:, jj:jj + 512], scalar=vcol(8 * jj, c), in1=xcc, op0=ALU.mult, op1=ALU.add,
                     reads=[xlkey, "vec", ("xc", c)], writes=[("xc", c)])
            P.op("act", "activation", out=xcb3[:, c, :], in_=xcc, func=AF.Copy,
                 reads=[("xc", c)], writes=[("xcb", c)])

        if g + 1 < NG1:
            norm_part_1a(g + 1)

        def k_part2(h, kT, kTkey, t1, t2, tkey, swp, swpkey):
            P.op("pe", "matmul", swp[:, 0:512], lhsT=pmat, rhs=kT, start=True, stop=True,
                 reads=[kTkey, "pmat"], writes=[swpkey])
            P.op("dve", "tensor_tensor", out=t2[0:32, :], in0=swp[0:32, 0:512], in1=S32, op=ALU.mult,
                 reads=[swpkey, rpkey], writes=[tkey + "2"])
            P.op("pool", "tensor_tensor", out=kT[0:32, :], in0=t1[0:32, :], in1=t2[0:32, :], op=ALU.add,
                 reads=[tkey + "1", tkey + "2"], writes=[kTkey])
            P.op("dve", "tensor_reduce",
                 out=ksum[:, h * 32 + 2 * g:h * 32 + 2 * g + 2], in_=v3(kT, 2), axis=AX.X, op=ALU.add,
                 reads=[kTkey], writes=["ksum"])
            P.dma("sp", "kst%d" % ((g * NH + h) % 3),
                  out=Ks[h, :, t0:t0 + 512], in_=kT, reads=[kTkey], writes=[("Ks", h)])

        pend = None
        for h in range(NH):
            b = mm_banks.next()
            pk, pkkey = ps(b)
            for c in range(8):
                P.op("pe", "matmul", pk, lhsT=wkvx3[:, c, h * 128:(h + 1) * 128], rhs=uT3[:, c, :],
                     start=(c == 0), stop=(c == 7), reads=[ukey, ("wkvx", 0)], writes=[pkkey])
            kT, kTkey = kT_rot.next()
            (t1, t2), tkey = t_rot.next()
            swp, swpkey = ps(2 + (h % 2))
            P.op("act", "activation", out=kT, in_=pk, func=AF.Copy, reads=[pkkey], writes=[kTkey])
            P.op("dve", "tensor_tensor", out=t1[0:32, :], in0=pk[0:32, :], in1=C32, op=ALU.mult,
                 reads=[pkkey, rpkey], writes=[tkey + "1"])
            if pend is not None:
                k_part2(*pend)
            pend = (h, kT, kTkey, t1, t2, tkey, swp, swpkey)
        k_part2(*pend)

        for j in range(4):
            for hh in range(2):
                b = mm_banks.next()
                pv, pvkey = ps(b)
                for c in range(8):
                    P.op("pe", "matmul",
                         pv, lhsT=uT3[:, c, j * 128:(j + 1) * 128], rhs=wkvx3[:, c, 1024 + hh * 512:1024 + (hh + 1) * 512],
                         start=(c == 0), stop=(c == 7), reads=[ukey, ("wkvx", 1)], writes=[pvkey])
                P.op("act", "activation",
                     out=vst4[:, hh * 4:(hh + 1) * 4, j, 0:128], in_=v3(pv, 4), func=AF.Copy,
                     reads=[pvkey], writes=["vst"])
        P.dma("sp", "vst",
              out=Vs.rearrange("h p (j e) -> p h j e", e=130)[:, :, 4 * g:4 * g + 4, :], in_=vst4,
              reads=["vst"], writes=["Vs"])

        if g + 1 < NG1:
            cur = tr_part_1a(g + 1)

        hsel, hselkey = hsel_rot.next()
        hsel3 = v3(hsel, 8)
        for op_ in range(0, 8, 2):
            pair = []
            for oc in (op_, op_ + 1):
                gi, jc = oc // 2, oc % 2
                (r_, i_, a_, m_, h_), lwkey = lw_rot.next()
                br = mm_banks.next()
                pr, prkey = ps(br)
                bi = mm_banks.next()
                pi, pikey = ps(bi)
                for which, pp, ppkey in ((0, pr, prkey), (1, pi, pikey)):
                    for kc in range(2):
                        P.op("pe", "matmul",
                             pp, lhsT=wrg4[:, which, gi * 2 + kc, jc * 128:(jc + 1) * 128], rhs=xcb3[:, gi * 2 + kc, :],
                             start=(kc == 0), stop=(kc == 1),
                             reads=[("xcb", gi * 2), ("xcb", gi * 2 + 1), "wrg_a", "wrg_i"], writes=[ppkey])
                pair.append((oc, r_, i_, a_, m_, h_, lwkey, pr, prkey, pi, pikey))
            for (oc, r_, i_, a_, m_, h_, lwkey, pr, prkey, pi, pikey) in pair:
                P.op("act", "activation", out=r_, in_=pr, func=AF.Sigmoid, bias=vcol(40, oc),
                     reads=[prkey, "vec"], writes=[lwkey + "r"])
                P.op("act", "activation", out=i_, in_=pi, func=AF.Sigmoid, bias=vcol(48, oc),
                     reads=[pikey, "vec"], writes=[lwkey + "i"])
            for (oc, r_, i_, a_, m_, h_, lwkey, pr, prkey, pi, pikey) in pair:
                P.op("act", "activation", out=a_, in_=r_, func=AF.Exp, scale=c1[:, oc:oc + 1],
                     reads=[lwkey + "r", "c1"], writes=[lwkey + "a"])
                P.op("act", "activation", out=m_, in_=r_, func=AF.Exp, scale=c2[:, oc:oc + 1],
                     reads=[lwkey + "r", "c2"], writes=[lwkey + "m"])
            for (oc, r_, i_, a_, m_, h_, lwkey, pr, prkey, pi, pikey) in pair:
                P.op("act", "activation", out=m_, in_=m_, func=AF.Sqrt, scale=-1.0, bias=1.0,
                     reads=[lwkey + "m"], writes=[lwkey + "m"])
            for (oc, r_, i_, a_, m_, h_, lwkey, pr, prkey, pi, pikey) in pair:
                P.op("dve", "tensor_tensor", out=i_, in0=i_, in1=xc3[:, oc, :], op=ALU.mult,
                     reads=[lwkey + "i", ("xc", oc)], writes=[lwkey + "i"])
                P.op("pool", "tensor_tensor", out=i_, in0=i_, in1=m_, op=ALU.mult,
                     reads=[lwkey + "i", lwkey + "m"], writes=[lwkey + "i"])
                P.op("dve", "tensor_tensor_scan",
                     out=h_, data0=a_, data1=i_, initial=carryh[:, oc:oc + 1], op0=ALU.mult, op1=ALU.add,
                     reads=[lwkey + "a", lwkey + "i", "carryh"], writes=[lwkey + "h"])
                P.op("dve", "tensor_copy", out=carryh[:, oc:oc + 1], in_=h_[:, 511:512],
                     reads=[lwkey + "h"], writes=["carryh"])
                P.op("pool", "tensor_scalar", out=hsel_tmp, in0=h_[:, 256:512], scalar1=psel[:, 1:2],
                     scalar2=None, op0=ALU.mult,
                     reads=[lwkey + "h", "psel"], writes=["hsel_tmp"])
                P.op("dve", "scalar_tensor_tensor",
                     out=hsel3[:, oc, :], in0=h_[:, 0:256], scalar=psel[:, 0:1], in1=hsel_tmp, op0=ALU.mult, op1=ALU.add,
                     reads=[lwkey + "h", "psel", "hsel_tmp"], writes=[hselkey])
        P.dma("sp", "hst%d" % (g % 2),
              out=Hs[:, :, g * 256:(g + 1) * 256].rearrange("c p t -> p c t"), in_=hsel3,
              reads=[hselkey], writes=["Hs"])

    P.op("dve", "tensor_copy", out=ksum_hi, in_=ksum, reads=["ksum"], writes=["ksum_hi"])
    P.op("dve", "tensor_tensor", out=ksum, in0=ksum, in1=ksum_hi, op=ALU.subtract,
         reads=["ksum", "ksum_hi"], writes=["ksum"])
    P.op("dve", "tensor_copy", out=ksum_lo, in_=ksum, reads=["ksum"], writes=["ksum_lo"])

    if stop == "1a":
        P.barrier()
        P.emit()
        return nc
    P.barrier()
    A.release(base_mark)
    NGO = SO // 512
    wq = A.alloc(8 * 1024, BF16)
    wq3 = v3(wq, 8)
    wgl = A.alloc(8 * 1024, BF16)
    wgl3 = v3(wgl, 8)
    wgt = A.alloc(8 * 1024, BF16)
    wgt3 = v3(wgt, 8)
    wbl = A.alloc(8 * 1024, BF16)
    wbl3 = v3(wbl, 8)
    g_pre = A.alloc(D)
    xts = [(A.alloc(D), ("xt", j)) for j in range(4)]
    xs_rot = Rot([(A.alloc(D, BF16), ("xs", i)) for i in range(2)])
    junk = A.alloc(D, BF16)
    ss_rot = Rot([(A.alloc(4), ("ss", i)) for i in range(4)])
    uT_rot = Rot([(A.alloc(8 * 512, BF16), ("uT", i)) for i in range(2)])
    kT_rot = Rot([(A.alloc(512, BF16), ("kT", i)) for i in range(3)])
    t_rot = Rot([((A.alloc(512), A.alloc(512)), "tr%d" % i) for i in range(2)])
    rope_rot = Rot([(A.alloc(2 * 512), ("rope", i)) for i in range(2)])
    hs_rot = Rot([(A.alloc(8 * 512, BF16), ("hs", i)) for i in range(2)])
    gw_rot = Rot([(tuple(A.alloc(512) for _ in range(3)), "gw%d" % i) for i in range(2)])
    hg_rot = Rot([(A.alloc(8 * 512, BF16), ("hg", i)) for i in range(2)])
    sg_rot = Rot([(A.alloc(512), ("sg", i)) for i in range(2)])
    yl_rot = Rot([(A.alloc(8 * 512, BF16), ("yl", i)) for i in range(2)])

    load_w(wq3, w_in[:, 0:1024].rearrange("(c p) n -> p c n", p=128), key="wq", semkey="w0")
    load_w(wgl3, w_in[:, 4096:5120].rearrange("(c p) n -> p c n", p=128), key="wgl", semkey="w1")
    load_w(wgt3, w_in[:, 6144:7168].rearrange("(c p) n -> p c n", p=128), key="wgt", semkey="w2")
    load_w(wbl3, w_bl.rearrange("(c p) n -> p c n", p=128), key="wbl", semkey="w3")
    P.dma("sp", "c_gain", out=g_pre, in_=grep[:, 0:D], writes=["g_pre"])

    def load_x_own(g, ntile):
        for j in range(ntile):
            xt, xkey = xts[j]
            r0 = g * ntile * 128 + j * 128
            P.dma("sp", "x%d" % j, out=xt, in_=xo[r0:r0 + 128, :], writes=[xkey])

    load_x_own(0, 4)
    for g in range(NGO):
        t0 = g * 512
        rp, rpkey = rope_rot.next()
        rp3 = v3(rp, 2)
        P.dma("sp", "rope%d" % (g % 2),
            out=rp3[0:32, :, :], in_=ropeq[:, :, t0:t0 + 512].rearrange("a r t -> r a t"), writes=[rpkey])
        hs, hskey = hs_rot.next()
        hs3 = v3(hs, 8)
        P.dma("sp", "hld%d" % (g % 2),
            out=hs3, in_=Hs[:, :, t0:t0 + 512].rearrange("c p t -> p c t"), reads=["Hs"], writes=[hskey])
        uT, ukey = uT_rot.next()
        uT3 = v3(uT, 8)
        for j in range(4):
            xt, xkey = xts[j]
            xs, xskey = xs_rot.next()
            norm_tile(xt, xkey, g_pre, "g_pre", xs, xskey, junk, ss_rot)
            psT, pskey = ps(j % 2)
            transpose_tile(xs, xskey, psT, pskey, uT3, ukey, j * 128, "act")
        if g + 1 < NGO:
            load_x_own(g + 1, 4)
        for h in range(NH):
            b = mm_banks.next()
            pk, pkkey = ps(b)
            for c in range(8):
                P.op("pe", "matmul", pk, lhsT=wq3[:, c, h * 128:(h + 1) * 128], rhs=uT3[:, c, :],
                                                               start=(c == 0), stop=(c == 7),
                     reads=[ukey, "wq"], writes=[pkkey])
            kT, kTkey = kT_rot.next()
            (t1, t2), tkey = t_rot.next()
            swp, swpkey = ps(2 + (h % 2))
            rope_evac(pk, pkkey, 512, rp3[0:32, 0, :], rp3[0:32, 1, :], rpkey, kT, kTkey, t1, t2, tkey, swp, swpkey)
            P.dma("sp", "kst%d" % ((g * NH + h) % 3),
                out=Qs[h, :, t0:t0 + 512], in_=kT, reads=[kTkey], writes=[("Qs", h)])
        hg, hgkey = hg_rot.next()
        hg3 = v3(hg, 8)
        for oc in range(8):
            b = mm_banks.next()
            pg, pgkey = ps(b)
            for c in range(8):
                P.op("pe", "matmul", pg, lhsT=wgl3[:, c, oc * 128:(oc + 1) * 128], rhs=uT3[:, c, :],
                                                                 start=(c == 0), stop=(c == 7),
                     reads=[ukey, "wgl"], writes=[pgkey])
            (gx, gq, gs), gwkey = gw_rot.next()
            P.op("act", "activation", out=gx, in_=pg, func=AF.Copy, reads=[pgkey], writes=[gwkey + "x"])
            P.op("act", "activation", out=gq, in_=pg, func=AF.Square, reads=[pgkey], writes=[gwkey + "q"])
            P.op("dve", "tensor_scalar", out=gq, in0=gq, scalar1=0.044715, scalar2=1.0, op0=ALU.mult, op1=ALU.add,
                 reads=[gwkey + "q"], writes=[gwkey + "q"])
            P.op("pool", "tensor_tensor", out=gq, in0=gq, in1=gx, op=ALU.mult,
                 reads=[gwkey + "q", gwkey + "x"], writes=[gwkey + "q"])
            P.op("act", "activation", out=gs, in_=gq, func=AF.Sigmoid, scale=1.5957691216057308,
                 reads=[gwkey + "q"], writes=[gwkey + "s"])
            P.op("pool", "tensor_tensor", out=gs, in0=gs, in1=gx, op=ALU.mult,
                 reads=[gwkey + "s", gwkey + "x"], writes=[gwkey + "s"])
            P.op("dve", "tensor_tensor", out=hg3[:, oc, :], in0=gs, in1=hs3[:, oc, :], op=ALU.mult,
                 reads=[gwkey + "s", hskey], writes=[hgkey])
        yl, ylkey = yl_rot.next()
        yl3 = v3(yl, 8)
        for oc in range(8):
            b = mm_banks.next()
            pg, pgkey = ps(b)
            for c in range(8):
                P.op("pe", "matmul", pg, lhsT=wgt3[:, c, oc * 128:(oc + 1) * 128], rhs=uT3[:, c, :],
                                                                 start=(c == 0), stop=(c == 7),
                     reads=[ukey, "wgt"], writes=[pgkey])
            sg, sgkey = sg_rot.next()
            P.op("act", "activation", out=sg, in_=pg, func=AF.Sigmoid, reads=[pgkey], writes=[sgkey])
            b2 = mm_banks.next()
            py, pykey = ps(b2)
            for c in range(8):
                P.op("pe", "matmul", py, lhsT=wbl3[:, c, oc * 128:(oc + 1) * 128], rhs=hg3[:, c, :],
                                                                 start=(c == 0), stop=(c == 7),
                     reads=[hgkey, "wbl"], writes=[pykey])
            P.op("dve", "tensor_tensor", out=yl3[:, oc, :], in0=py, in1=sg, op=ALU.mult,
                 reads=[pykey, sgkey], writes=[ylkey])
        P.dma("sp", "yst%d" % (g % 2),
            out=YL[:, :, t0:t0 + 512].rearrange("c p t -> p c t"), in_=yl3, reads=[ylkey], writes=["YL"])

    if stop == "1b":
        P.barrier()
        P.emit()
        return nc
    P.barrier()
    A.release(base_mark)
    attnT = A.alloc(NH * SO, BF16)
    attnT3 = v3(attnT, NH)
    p2_mark = A.mark()
    kv_rot = Rot([((A.alloc(S, BF16), A.alloc(64 * 130, BF16), A.alloc(SO, BF16)), "kv%d" % i) for i in range(2)])
    gmask = A.alloc(16 * 32)
    cmaskf = A.alloc(1024)
    cmask = A.alloc(1024, BF16)
    cmask3 = v3(cmask, 2)
    pt_rot = Rot([(A.alloc(512, BF16), ("pt", i)) for i in range(4)])
    acc_rot = Rot([(A.alloc(2 * 130), ("acc", i)) for i in range(3)])
    sel_rot = Rot([(A.alloc(2 * 32), ("sel", i)) for i in range(3)])
    g1_rot = Rot([(A.alloc(32 + 8 + 1), ("g1", i)) for i in range(4)])
    on_rot = Rot([(A.alloc(2 * 128, BF16), ("on", i)) for i in range(2)])
    rinv = A.alloc(2)

    P.dma("sp", "c_gmask", out=gmask, in_=gmask_d, writes=["gmask"])
    P.dma("sp", "c_cmask", out=cmaskf, in_=cmask_d, writes=["cmaskf"])
    P.op("dve", "tensor_copy", out=cmask, in_=cmaskf, reads=["cmaskf"], writes=["cmask"])

    def load_head(h):
        (KT, VV, QT), kvkey = kv_rot.next()
        P.dma("sp", kvkey + "k", out=KT, in_=Ks[h], reads=[("Ks", h)], writes=[kvkey + "k"])
        P.dma("sp", kvkey + "v", out=VV, in_=Vs[h], reads=["Vs"], writes=[kvkey + "v"])
        P.dma("sp", kvkey + "q", out=QT, in_=Qs[h], reads=[("Qs", h)], writes=[kvkey + "q"])
        return (KT, v3(VV, 64), QT), kvkey

    s_banks = Rot([0, 1, 2])
    o_banks = Rot([3, 4, 7])
    nxt = load_head(0)
    for h in range(NH):
        (KT, VV3, QT), kvkey = nxt
        if h + 1 < NH:
            nxt = load_head(h + 1)
        units = [(i, j) for i in range(16) for j in range(2 * i + 2)]
        state = {}

        def emit_S(u):
            i, j = units[u]
            b = s_banks.next()
            sp_, spkey = ps(b)
            for kt in range(2):
                P.op("pe", "matmul",
                    sp_[:, kt * 256:(kt + 1) * 256], lhsT=KT[:, (2 * j + kt) * 128:(2 * j + kt + 1) * 128],
                    rhs=QT[:, i * 256:(i + 1) * 256], start=True, stop=True,
                    reads=[kvkey + "k", kvkey + "q"], writes=[spkey])
            state[u] = (sp_, spkey)

        def emit_gate(i):
            pgt, pgtkey = ps(5)
            acc, acckey = acc_rot.next()
            sel, selkey = sel_rot.next()
            for qt in range(2):
                qcols = QT[:, i * 256 + qt * 128:i * 256 + (qt + 1) * 128]
                P.op("pe", "matmul", pgt[:, qt * 32:(qt + 1) * 32], lhsT=qcols,
                                                                  rhs=ksum_hi[:, h * 32:(h + 1) * 32], start=True, stop=False,
                     reads=[kvkey + "q", "ksum_hi"], writes=[pgtkey])
                P.op("pe", "matmul", pgt[:, qt * 32:(qt + 1) * 32], lhsT=qcols,
                                                                  rhs=ksum_lo[:, h * 32:(h + 1) * 32], start=False, stop=True,
                     reads=[kvkey + "q", "ksum_lo"], writes=[pgtkey])
            for qt in range(2):
                g1, g1key = g1_rot.next()
                P.op("dve", "tensor_tensor", out=g1[:, 0:32], in0=pgt[:, qt * 32:(qt + 1) * 32],
                                                                    in1=gmask[:, i * 32:(i + 1) * 32], op=ALU.add,
                     reads=[pgtkey, "gmask"], writes=[g1key])
                P.op("dve", "max", out=g1[:, 32:40], in_=g1[:, 0:32], reads=[g1key], writes=[g1key])
                P.op("dve", "tensor_scalar", out=g1[:, 40:41], in0=g1[:, 35:36], scalar1=-1e29, scalar2=None,
                                                             op0=ALU.max, reads=[g1key], writes=[g1key])
                P.op("dve", "tensor_scalar", out=sel[:, qt * 32:(qt + 1) * 32], in0=g1[:, 0:32],
                                                                             scalar1=g1[:, 40:41], scalar2=None, op0=ALU.is_ge,
                     reads=[g1key], writes=[selkey])
            return acc, acckey, sel, selkey

        blk = {}

        def emit_rest(u):
            i, j = units[u]
            sp_, spkey = state.pop(u)
            acc, acckey, sel, selkey = blk[i]
            pt, ptkey = pt_rot.next()
            P.op("act", "activation", out=pt, in_=sp_, func=AF.Exp, scale=SCALE, reads=[spkey], writes=[ptkey])
            if j >= 2 * i:
                P.op("pool", "tensor_tensor", out=pt, in0=pt, in1=cmask3[:, j - 2 * i, :], op=ALU.mult,
                     reads=[ptkey, "cmask"], writes=[ptkey])
            b = o_banks.next()
            po, pokey = ps(b)
            po3 = po[:, 0:260].rearrange("p (a b) -> p a b", a=2)
            for qt in range(2):
                for kt in range(2):
                    P.op("pe", "matmul",
                        po3[:, qt, :], lhsT=pt[:, kt * 256 + qt * 128:kt * 256 + (qt + 1) * 128], rhs=VV3[:, 2 * j + kt, :],
                        start=(kt == 0), stop=(kt == 1), reads=[ptkey, kvkey + "v"], writes=[pokey])
            acc3 = v3(acc, 2)
            for qt in range(2):
                sc = sel[:, qt * 32 + j:qt * 32 + j + 1]
                if j == 0:
                    P.op("dve", "tensor_scalar", out=acc3[:, qt, :], in0=po3[:, qt, :], scalar1=sc,
                                                                        scalar2=None, op0=ALU.mult,
                         reads=[pokey, selkey], writes=[acckey])
                else:
                    P.op("dve", "scalar_tensor_tensor", out=acc3[:, qt, :], in0=po3[:, qt, :], scalar=sc,
                                                                               in1=acc3[:, qt, :], op0=ALU.mult, op1=ALU.add,
                         reads=[pokey, selkey, acckey], writes=[acckey])

        pending = []

        def emit_norm(i):
            acc, acckey, sel, selkey = blk[i]
            acc3 = v3(acc, 2)
            on, onkey = on_rot.next()
            on3 = v3(on, 2)
            P.op("dve", "reciprocal", out=rinv, in_=acc3[:, :, 128], reads=[acckey], writes=["rinv"])
            for qt in range(2):
                P.op("dve", "tensor_scalar", out=on3[:, qt, :], in0=acc3[:, qt, 0:128], scalar1=rinv[:, qt:qt + 1],
                                                             scalar2=None, op0=ALU.mult,
                     reads=[acckey, "rinv"], writes=[onkey])
            pending.append((i, on3, onkey))

        def emit_fin():
            i, on3, onkey = pending.pop(0)
            pT_, pTkey = ps(6)
            pTb = pT_.bitcast(BF16)
            for qt in range(2):
                P.op("pe", "transpose", out=pTb[:, qt * 128:(qt + 1) * 128], in_=on3[:, qt, :], identity=ident,
                     reads=[onkey, "ident"], writes=[pTkey])
            P.op("dve", "tensor_copy", out=attnT3[:, h, i * 256:(i + 1) * 256], in_=pTb[:, 0:256],
                 reads=[pTkey], writes=["attnT"])

        U = len(units)
        LA = 2
        blk[0] = emit_gate(0)
        for u0 in range(min(LA, U)):
            ni, nj = units[u0]
            if nj == 0 and ni not in blk:
                blk[ni] = emit_gate(ni)
            emit_S(u0)
        for u in range(U):
            i, j = units[u]
            if u + LA < U:
                ni, nj = units[u + LA]
                if nj == 0:
                    blk[ni] = emit_gate(ni)
                emit_S(u + LA)
            emit_rest(u)
            if j == 1 and pending:
                emit_fin()
            if j == 2 * i + 1:
                emit_norm(i)
        while pending:
            emit_fin()

    if debug:
        ATT = nc.dram_tensor("ATT", [NH, 128, SO], BF16, kind="ExternalOutput").ap()
        P.dma("sp", "dbg_att", out=ATT.rearrange("h p t -> p h t"), in_=attnT3, reads=["attnT"], writes=["ATT"])
    if stop == "2":
        P.barrier()
        P.emit()
        return nc
    P.barrier()
    A.release(p2_mark)
    NG3 = SO // 256
    wga = A.alloc(8 * 1024, BF16)
    wga3 = v3(wga, 8)
    wba = A.alloc(8 * 1024, BF16)
    wba3 = v3(wba, 8)
    wo = A.alloc(8 * 1024, BF16)
    wo3 = v3(wo, 8)
    g_pre = A.alloc(D)
    g_post = A.alloc(D)
    xts = [(A.alloc(D), ("xt", j)) for j in range(2)]
    xs_rot = Rot([(A.alloc(D, BF16), ("xs", i)) for i in range(2)])
    junk = A.alloc(D, BF16)
    ss_rot = Rot([(A.alloc(4), ("ss", i)) for i in range(4)])
    uT_rot = Rot([(A.alloc(8 * 256, BF16), ("uT", i)) for i in range(2)])
    sg_rot = Rot([(A.alloc(256), ("sg", i)) for i in range(2)])
    yld_rot = Rot([(A.alloc(8 * 256, BF16), ("yld", i)) for i in range(2)])
    mT_rot = Rot([(A.alloc(8 * 256, BF16), ("mT", i)) for i in range(2)])
    mix_rot = Rot([(A.alloc(D), ("mix", i)) for i in range(2)])

    load_w(wga3, w_in[:, 5120:6144].rearrange("(c p) n -> p c n", p=128), key="wga", semkey="w0")
    load_w(wba3, w_ba.rearrange("(c p) n -> p c n", p=128), key="wba", semkey="w1")
    load_w(wo3, w_out.rearrange("(c p) n -> p c n", p=128), key="wo", semkey="w2")
    P.dma("sp", "c_gain", out=g_pre, in_=grep[:, 0:D], writes=["g_pre"])
    P.dma("sp", "c_gain2", out=g_post, in_=grep[:, D:2 * D], writes=["g_post"])

    load_x_own(0, 2)
    for g in range(NG3):
        t0 = g * 256
        yld, yldkey = yld_rot.next()
        yld3 = v3(yld, 8)
        P.dma("sp", "yld%d" % (g % 2),
            out=yld3, in_=YL[:, :, t0:t0 + 256].rearrange("c p t -> p c t"), reads=["YL"], writes=[yldkey])
        uT, ukey = uT_rot.next()
        uT3 = v3(uT, 8)
        mix_list = []
        for j in range(2):
            xt, xkey = xts[j]
            xs, xskey = xs_rot.next()
            norm_tile(xt, xkey, g_pre, "g_pre", xs, xskey, junk, ss_rot)
            psT, pskey = ps(j % 2)
            transpose_tile(xs, xskey, psT, pskey, uT3, ukey, j * 128, "act")
        mT, mTkey = mT_rot.next()
        mT3 = v3(mT, 8)
        for oc in range(8):
            b = mm_banks.next()
            pg, pgkey = ps(b)
            for c in range(8):
                P.op("pe", "matmul", pg[:, 0:256], lhsT=wga3[:, c, oc * 128:(oc + 1) * 128], rhs=uT3[:, c, :],
                                                                 start=(c == 0), stop=(c == 7),
                     reads=[ukey, "wga"], writes=[pgkey])
            sg, sgkey = sg_rot.next()
            P.op("act", "activation", out=sg, in_=pg[:, 0:256], func=AF.Sigmoid, reads=[pgkey], writes=[sgkey])
            b2 = mm_banks.next()
            py, pykey = ps(b2)
            for c in range(8):
                P.op("pe", "matmul", py[:, 0:256], lhsT=wba3[:, c, oc * 128:(oc + 1) * 128],
                                                                 rhs=attnT3[:, c, t0:t0 + 256], start=(c == 0), stop=(c == 7),
                     reads=["attnT", "wba"], writes=[pykey])
            P.op("dve", "tensor_tensor", out=sg, in0=py[:, 0:256], in1=sg, op=ALU.mult,
                 reads=[pykey, sgkey], writes=[sgkey])
            P.op("pool", "tensor_tensor", out=mT3[:, oc, :], in0=sg, in1=yld3[:, oc, :], op=ALU.add,
                 reads=[sgkey, yldkey], writes=[mTkey])
        for j in range(2):
            xt, xkey = xts[j]
            mix, mixkey = mix_rot.next()
            for hh in range(2):
                b = mm_banks.next()
                pm, pmkey = ps(b)
                for c in range(8):
                    P.op("pe", "matmul", pm, lhsT=mT3[:, c, j * 128:(j + 1) * 128],
                                                                          rhs=wo3[:, c, hh * 512:(hh + 1) * 512],
                                                                          start=(c == 0), stop=(c == 7),
                         reads=[mTkey, "wo"], writes=[pmkey])
                P.op("act", "activation", out=mix[:, hh * 512:(hh + 1) * 512], in_=pm, func=AF.Copy,
                     reads=[pmkey], writes=[mixkey])
            ss, sskey = ss_rot.next()
            P.op("act", "activation", out=junk, in_=mix, func=AF.Square, accum_out=ss[:, 0:1],
                 reads=[mixkey], writes=["junk", sskey])
            P.op("dve", "tensor_scalar", out=ss[:, 1:2], in0=ss[:, 0:1], scalar1=1.0 / D, scalar2=EPS,
                                                         op0=ALU.mult, op1=ALU.add, reads=[sskey], writes=[sskey])
            P.op("act", "activation", out=ss[:, 2:3], in_=ss[:, 1:2], func=AF.Sqrt, reads=[sskey], writes=[sskey])
            P.op("dve", "reciprocal", out=ss[:, 3:4], in_=ss[:, 2:3], reads=[sskey], writes=[sskey])
            P.op("dve", "scalar_tensor_tensor", out=mix, in0=mix, scalar=ss[:, 3:4], in1=g_post,
                                                                         op0=ALU.mult, op1=ALU.mult,
                 reads=[mixkey, sskey, "g_post"], writes=[mixkey])
            P.op("pool", "tensor_tensor", out=mix, in0=mix, in1=xt, op=ALU.add,
                 reads=[mixkey, xkey], writes=[mixkey])
            r0 = t0 + j * 128
            P.dma("sp", "h1st%d" % j, out=H1[r0:r0 + 128, :], in_=mix,
                  reads=[mixkey], writes=["H1"])
        if g + 1 < NG3:
            load_x_own(g + 1, 2)

    if stop == "3a":
        P.barrier()
        P.emit()
        return nc
    P.barrier()
    A.release(base_mark)
    wup = A.alloc(8 * 4096, BF16)
    wup3 = v3(wup, 8)
    wdn = A.alloc(32 * 1024, BF16)
    wdn3 = v3(wdn, 32)
    g_pre = A.alloc(D)
    g_post = A.alloc(D)
    xts = [(A.alloc(D), ("xt", j)) for j in range(2)]
    xs_rot = Rot([(A.alloc(D, BF16), ("xs", i)) for i in range(1)])
    junk = A.alloc(D, BF16)
    ss_rot = Rot([(A.alloc(4), ("ss", i)) for i in range(4)])
    uT_rot = Rot([(A.alloc(8 * 256, BF16), ("uT", i)) for i in range(1)])
    aT = A.alloc(32 * 256, BF16)
    aT3 = v3(aT, 32)
    rl_rot = Rot([(A.alloc(256), ("rl", i)) for i in range(2)])
    mo_rot = Rot([(A.alloc(D), ("mo", i)) for i in range(1)])

    for s_ in range(4):
        load_w(wup3[:, :, s_ * 1024:(s_ + 1) * 1024], w_up[:, s_ * 1024:(s_ + 1) * 1024].rearrange("(c p) n -> p c n", p=128),
               key=("wup", s_), semkey="w%d" % s_)
    for s_ in range(4):
        load_w(wdn3[:, s_ * 8:(s_ + 1) * 8, :], w_down[s_ * 1024:(s_ + 1) * 1024, :].rearrange("(c p) n -> p c n", p=128),
               key=("wdn", s_), semkey="w%d" % (4 + s_) if s_ < 1 else "w%d" % s_)
    P.dma("sp", "c_gain", out=g_pre, in_=grep[:, 2 * D:3 * D], writes=["g_pre"])
    P.dma("sp", "c_gain2", out=g_post, in_=grep[:, 3 * D:4 * D], writes=["g_post"])

    def load_h1(g):
        for j in range(2):
            xt, xkey = xts[j]
            r0 = g * 256 + j * 128
            P.dma("sp", "x%d" % j, out=xt, in_=H1[r0:r0 + 128, :],
                  reads=["H1"], writes=[xkey])

    load_h1(0)
    for g in range(NG3):
        t0 = g * 256
        uT, ukey = uT_rot.next()
        uT3 = v3(uT, 8)
        for j in range(2):
            xt, xkey = xts[j]
            xs, xskey = xs_rot.next()
            norm_tile(xt, xkey, g_pre, "g_pre", xs, xskey, junk, ss_rot)
            psT, pskey = ps(j % 2)
            transpose_tile(xs, xskey, psT, pskey, uT3, ukey, j * 128, "act")
        for fc in range(32):
            b = mm_banks.next()
            pu, pukey = ps(b)
            for c in range(8):
                P.op("pe", "matmul", pu[:, 0:256], lhsT=wup3[:, c, fc * 128:(fc + 1) * 128], rhs=uT3[:, c, :],
                                                                 start=(c == 0), stop=(c == 7),
                     reads=[ukey, ("wup", fc // 8)], writes=[pukey])
            rl, rlkey = rl_rot.next()
            P.op("act", "activation", out=rl, in_=pu[:, 0:256], func=AF.Relu, reads=[pukey], writes=[rlkey])
            eng = "dve" if fc % 2 == 0 else "pool"
            P.op(eng, "tensor_tensor", out=aT3[:, fc, :], in0=rl, in1=rl, op=ALU.mult,
                 reads=[rlkey], writes=[("aT", fc)])
        for j in range(2):
            xt, xkey = xts[j]
            mo, mokey = mo_rot.next()
            for hh in range(2):
                b = mm_banks.next()
                pm, pmkey = ps(b)
                for fc in range(32):
                    P.op("pe", "matmul", pm, lhsT=aT3[:, fc, j * 128:(j + 1) * 128],
                                                                            rhs=wdn3[:, fc, hh * 512:(hh + 1) * 512],
                                                                            start=(fc == 0), stop=(fc == 31),
                         reads=[("aT", fc), ("wdn", fc // 8)], writes=[pmkey])
                P.op("act", "activation", out=mo[:, hh * 512:(hh + 1) * 512], in_=pm, func=AF.Copy,
                     reads=[pmkey], writes=[mokey])
            ss, sskey = ss_rot.next()
            P.op("act", "activation", out=junk, in_=mo, func=AF.Square, accum_out=ss[:, 0:1],
                 reads=[mokey], writes=["junk", sskey])
            P.op("dve", "tensor_scalar", out=ss[:, 1:2], in0=ss[:, 0:1], scalar1=1.0 / D, scalar2=EPS,
                                                         op0=ALU.mult, op1=ALU.add, reads=[sskey], writes=[sskey])
            P.op("act", "activation", out=ss[:, 2:3], in_=ss[:, 1:2], func=AF.Sqrt, reads=[sskey], writes=[sskey])
            P.op("dve", "reciprocal", out=ss[:, 3:4], in_=ss[:, 2:3], reads=[sskey], writes=[sskey])
            P.op("dve", "scalar_tensor_tensor", out=mo, in0=mo, scalar=ss[:, 3:4], in1=g_post,
                                                                       op0=ALU.mult, op1=ALU.mult,
                 reads=[mokey, sskey, "g_post"], writes=[mokey])
            P.op("pool", "tensor_tensor", out=mo, in0=mo, in1=xt, op=ALU.add,
                 reads=[mokey, xkey], writes=[mokey])
            r0 = t0 + j * 128
            P.dma("sp", "ost", out=out_d[r0:r0 + 128, :], in_=mo,
                  reads=[mokey], writes=["out"])
        if g + 1 < NG3:
            load_h1(g + 1)

    P.barrier()
    P.emit()
    return nc


def _fm(v):
    return np.ascontiguousarray(np.asarray(v, np.float32).reshape(8, 128).T)


def _rope_tables(pos):
    inv_freq = (np.float32(500000.0) ** (-(np.arange(0, 32, 2, dtype=np.float32)) / np.float32(32))).astype(np.float32)
    ang = (pos.astype(np.float32)[:, None] * inv_freq[None, :]).astype(np.float32)
    cos = np.cos(ang.astype(np.float64)).astype(np.float32).T
    sin = np.sin(ang.astype(np.float64)).astype(np.float32).T
    c32 = np.concatenate([cos, cos], axis=0)
    s32 = np.concatenate([-sin, sin], axis=0)
    return np.ascontiguousarray(np.stack([c32, s32], axis=0))


_NC_CACHE = {}
DBG = {}


def kernel(x, attn_pre_norm, attn_post_norm, w_in, conv_w, conv_b, w_rg_a, b_rg_a, w_rg_i, b_rg_i, lru_lambda,
           w_branch_attn, w_branch_lru, w_out, mlp_pre_norm, mlp_post_norm, w_mlp_up, w_mlp_down):
    x = np.asarray(x, np.float32)
    f = lambda a: np.ascontiguousarray(np.asarray(a, np.float32))
    vec_cols = [_fm(conv_w[0][j]) for j in range(4)] + [_fm(conv_b[0]), _fm(b_rg_a[0]), _fm(b_rg_i[0]), _fm(lru_lambda[0])]
    vecs = np.ascontiguousarray(np.concatenate(vec_cols, axis=1))
    grep = np.ascontiguousarray(np.broadcast_to(np.concatenate(
        [f(attn_pre_norm[0]), f(attn_post_norm[0]), f(mlp_pre_norm[0]), f(mlp_post_norm[0])])[None, :], (128, 4096)))
    ropek = _rope_tables(np.arange(S))
    shared = {
        "w_in": f(w_in[0]), "w_rga": f(w_rg_a[0]), "w_rgi": f(w_rg_i[0]), "w_ba": f(w_branch_attn[0]),
        "w_bl": f(w_branch_lru[0]), "w_out": f(w_out[0]), "w_up": f(w_mlp_up[0]), "w_down": f(w_mlp_down[0]),
        "vecs": vecs, "grep": grep, "ropek": ropek,
    }
    tri = (np.arange(256)[:, None] <= np.arange(256)[None, :]).astype(np.float32)
    in_maps = []
    own_rows = []
    for c in range(8):
        b, p = c // 2, c % 2
        blocks = np.arange(16) * 2 + p
        rows = (blocks[:, None] * 256 + np.arange(256)[None, :]).reshape(-1)
        own_rows.append((b, rows))
        gm = np.zeros((16, 32), np.float32)
        for i in range(16):
            gb = 2 * i + p
            gm[i, gb] = 1e30
            gm[i, gb + 1:] = -1e30
        gmask = np.ascontiguousarray(np.broadcast_to(gm.reshape(1, 512), (128, 512)))
        ma = tri if p == 0 else np.ones((256, 256), np.float32)
        mb = np.zeros((256, 256), np.float32) if p == 0 else tri
        cm = np.stack([ma.reshape(2, 128, 256).transpose(1, 0, 2), mb.reshape(2, 128, 256).transpose(1, 0, 2)], axis=1)
        cmask = np.ascontiguousarray(cm.reshape(128, 1024))
        psel = np.ascontiguousarray(np.broadcast_to(np.array([1.0 - p, float(p)], np.float32)[None, :], (128, 2)))
        m = dict(shared)
        m.update({"xa": np.ascontiguousarray(x[b]), "xo": np.ascontiguousarray(x[b][rows]),
                  "ropeq": _rope_tables(rows), "gmask": gmask, "cmask": cmask, "psel": psel})
        in_maps.append(m)
    if _NC_CACHE.get("prep_only"):
        return in_maps, own_rows
    if "nc" not in _NC_CACHE:
        _NC_CACHE["nc"] = build_program()
    res = run_bass_kernel_spmd(_NC_CACHE["nc"], in_maps, core_ids=list(range(8)))
    out = np.empty((4, S, D), np.float32)
    for c in range(8):
        b, rows = own_rows[c]
        out[b, rows] = res.results[c]["out"]
    return out
```
